# Optimizing a Trainium2 kernel written in Bass

```python
import math
import jax, jax.numpy as jnp
from jax import lax
import numpy as np

D_MODEL = 1024
BATCH = 8
SEQ = 4096
DEPTH = 4

N_MIXERS = 2
N_SSM_LAYERS = (DEPTH + 1) // 2
N_ATT_LAYERS = DEPTH // 2
SSM_WIDTH = D_MODEL // 2
SSM_CH = 16
SSM_GROUPS = SSM_WIDTH // SSM_CH
SSM_STATE = 64
DT_MIN, DT_MAX = 1e-3, 1e-1
HEAD_DIM = 64
N_HEADS = D_MODEL // HEAD_DIM
N_KV_HEADS = N_HEADS // 4
KV_GROUP = N_HEADS // N_KV_HEADS
IDX_HEADS = max(4, D_MODEL // 128)
IDX_DIM = 64
TOPK_MAX = 256
Q_BLOCK = 128
ROPE_THETA = 10000.0
ATT_SPLITS = (N_HEADS * HEAD_DIM, N_KV_HEADS * HEAD_DIM, N_KV_HEADS * HEAD_DIM,
              IDX_HEADS * IDX_DIM, IDX_DIM, IDX_HEADS)
ATT_IN_COLS = sum(ATT_SPLITS)
D_FF = 4 * D_MODEL
NORM_EPS = 1e-6

kernel_name = "hybrid_s5_dsa_sandwich_trunk"


def rms_norm(x, g):
    xf = x.astype(jnp.float32)
    y = xf * lax.rsqrt(jnp.mean(xf * xf, axis=-1, keepdims=True) + NORM_EPS)
    return (y * g.astype(jnp.float32)).astype(x.dtype)


def rope_tables(seq_len, dim, dtype):
    half = dim // 2
    inv_freq = ROPE_THETA ** (-jnp.arange(half, dtype=jnp.float32) * 2.0 / dim)
    ang = jnp.arange(seq_len, dtype=jnp.float32)[:, None] * inv_freq[None, :]
    cos = jnp.concatenate([jnp.cos(ang), jnp.cos(ang)], axis=-1)
    sin = jnp.concatenate([jnp.sin(ang), jnp.sin(ang)], axis=-1)
    return cos.astype(dtype), sin.astype(dtype)


def apply_rope(x, cos, sin):
    half = x.shape[-1] // 2
    rot = jnp.concatenate([-x[..., half:], x[..., :half]], axis=-1)
    return x * cos[None, :, None, :] + rot * sin[None, :, None, :]


def s5_mixer(h, w_in, lam_re, lam_im, log_dt, b_re, b_im, c_re, c_im, d_skip, w_glu, w_out):
    bsz, seq, _ = h.shape
    dt_ = h.dtype
    u = (h @ w_in).reshape(bsz, seq, SSM_GROUPS, SSM_CH)
    lr = lam_re.astype(jnp.float32); li = lam_im.astype(jnp.float32)
    dt = jnp.exp(log_dt.astype(jnp.float32))[:, None]
    mag = jnp.exp(lr * dt)
    abar_re = mag * jnp.cos(li * dt)
    abar_im = mag * jnp.sin(li * dt)
    den = lr * lr + li * li
    nr = abar_re - 1.0
    ni = abar_im
    fr = (nr * lr + ni * li) / den
    fi = (ni * lr - nr * li) / den
    br = b_re.astype(jnp.float32); bi = b_im.astype(jnp.float32)
    bbar_re = (fr[..., None] * br - fi[..., None] * bi).astype(dt_)
    bbar_im = (fr[..., None] * bi + fi[..., None] * br).astype(dt_)
    bu_re = jnp.einsum('bsgc,gpc->bsgp', u, bbar_re)
    bu_im = jnp.einsum('bsgc,gpc->bsgp', u, bbar_im)
    a_re = jnp.broadcast_to(abar_re.astype(dt_), bu_re.shape)
    a_im = jnp.broadcast_to(abar_im.astype(dt_), bu_im.shape)

    def combine(left, right):
        a1r, a1i, b1r, b1i = left
        a2r, a2i, b2r, b2i = right
        return (a2r * a1r - a2i * a1i,
                a2r * a1i + a2i * a1r,
                a2r * b1r - a2i * b1i + b2r,
                a2r * b1i + a2i * b1r + b2i)

    _, _, xs_re, xs_im = lax.associative_scan(combine, (a_re, a_im, bu_re, bu_im), axis=1)
    y = (jnp.einsum('bsgp,gcp->bsgc', xs_re, c_re)
         - jnp.einsum('bsgp,gcp->bsgc', xs_im, c_im)
         + d_skip[None, None] * u)
    z = jax.nn.gelu(y.reshape(bsz, seq, SSM_WIDTH))
    z = z * jax.nn.sigmoid(z @ w_glu)
    return z @ w_out


def dsa_mixer(h, w_in, w_out):
    bsz, seq, _ = h.shape
    dt_ = h.dtype
    proj = h @ w_in
    offs = np.cumsum(ATT_SPLITS)[:-1].tolist()
    q, k, v, qi, ki, wi = jnp.split(proj, offs, axis=-1)
    cos, sin = rope_tables(seq, HEAD_DIM, dt_)
    q = apply_rope(q.reshape(bsz, seq, N_HEADS, HEAD_DIM), cos, sin)
    k = apply_rope(k.reshape(bsz, seq, N_KV_HEADS, HEAD_DIM), cos, sin)
    v = v.reshape(bsz, seq, N_KV_HEADS, HEAD_DIM)
    qi = apply_rope(qi.reshape(bsz, seq, IDX_HEADS, IDX_DIM), cos, sin)
    ki = apply_rope(ki[:, :, None, :], cos, sin)[:, :, 0, :]
    wi = wi * (IDX_HEADS ** -0.5 * IDX_DIM ** -0.5)

    k_sel = min(TOPK_MAX, seq // 4)
    n_blk = seq // Q_BLOCK
    to_blocks = lambda a: jnp.swapaxes(a.reshape((bsz, n_blk, Q_BLOCK) + a.shape[2:]), 0, 1)
    q_b, qi_b, wi_b = to_blocks(q), to_blocks(qi), to_blocks(wi)
    starts = jnp.arange(n_blk, dtype=jnp.int32) * Q_BLOCK
    key_pos = jnp.arange(seq, dtype=jnp.int32)
    b_idx = jnp.arange(bsz)[:, None, None]
    scale = HEAD_DIM ** -0.5

    def block(args):
        q_blk, qi_blk, wi_blk, t0 = args
        t = t0 + jnp.arange(Q_BLOCK, dtype=jnp.int32)
        rel = jax.nn.relu(jnp.einsum('bthd,bsd->bths', qi_blk, ki).astype(jnp.float32))
        score = jnp.einsum('bths,bth->bts', rel, wi_blk.astype(jnp.float32))
        causal = key_pos[None, :] <= t[:, None]
        score = jnp.where(causal[None], score, -jnp.inf)
        _, idx = lax.top_k(score, k_sel)
        kg = k[b_idx, idx]
        vg = v[b_idx, idx]
        qg = q_blk.reshape(bsz, Q_BLOCK, N_KV_HEADS, KV_GROUP, HEAD_DIM)
        att = jnp.einsum('btngd,btknd->btngk', qg, kg).astype(jnp.float32) * scale
        valid = idx <= t[None, :, None]
        att = jnp.where(valid[:, :, None, None, :], att, -jnp.inf)
        p = jax.nn.softmax(att, axis=-1).astype(dt_)
        o = jnp.einsum('btngk,btknd->btngd', p, vg)
        return o.reshape(bsz, Q_BLOCK, N_HEADS * HEAD_DIM)

    out = lax.map(block, (q_b, qi_b, wi_b, starts))
    out = jnp.swapaxes(out, 0, 1).reshape(bsz, seq, N_HEADS * HEAD_DIM)
    return out @ w_out


def sq_relu_mlp(h, w1, w2):
    a = jax.nn.relu(h @ w1)
    return (a * a) @ w2


def setup_inputs(seed: int = 0) -> dict:
    key = jax.random.key(seed)
    ks = jax.random.split(key, 20)
    nA, nB = N_SSM_LAYERS, N_ATT_LAYERS
    G, P, C, E = SSM_GROUPS, SSM_STATE, SSM_CH, SSM_WIDTH
    nrm = lambda k, shape, fan: jax.random.normal(k, shape, jnp.float32) * (fan ** -0.5)
    x = jax.random.normal(ks[0], (BATCH, SEQ, D_MODEL), jnp.float32)
    norm_g = 1.0 + 0.01 * jax.random.normal(ks[1], (DEPTH, 4, D_MODEL), jnp.float32)
    mlp_w1 = nrm(ks[2], (DEPTH, D_MODEL, D_FF), D_MODEL)
    mlp_w2 = nrm(ks[3], (DEPTH, D_FF, D_MODEL), D_FF)
    ssm_w_in = nrm(ks[4], (nA, D_MODEL, E), D_MODEL)
    n_idx = jnp.arange(P, dtype=jnp.float32)
    ssm_lam_re = -0.5 + 0.01 * jax.random.normal(ks[5], (nA, G, P), jnp.float32)
    ssm_lam_im = math.pi * n_idx[None, None, :] + 0.01 * jax.random.normal(ks[6], (nA, G, P), jnp.float32)
    ssm_log_dt = jax.random.uniform(ks[7], (nA, G), jnp.float32, math.log(DT_MIN), math.log(DT_MAX))
    ssm_b_re = nrm(ks[8], (nA, G, P, C), 2 * C)
    ssm_b_im = nrm(ks[9], (nA, G, P, C), 2 * C)
    ssm_c_re = nrm(ks[10], (nA, G, C, P), 2 * P)
    ssm_c_im = nrm(ks[11], (nA, G, C, P), 2 * P)
    ssm_d = jax.random.normal(ks[12], (nA, G, C), jnp.float32)
    ssm_w_glu = nrm(ks[13], (nA, E, E), E)
    ssm_w_out = nrm(ks[14], (nA, E, D_MODEL), E)
    att_w_in = nrm(ks[15], (nB, D_MODEL, ATT_IN_COLS), D_MODEL)
    att_w_out = nrm(ks[16], (nB, N_HEADS * HEAD_DIM, D_MODEL), N_HEADS * HEAD_DIM)
    return {"x": x, "norm_g": norm_g, "mlp_w1": mlp_w1, "mlp_w2": mlp_w2,
            "ssm_w_in": ssm_w_in, "ssm_lam_re": ssm_lam_re, "ssm_lam_im": ssm_lam_im,
            "ssm_log_dt": ssm_log_dt, "ssm_b_re": ssm_b_re, "ssm_b_im": ssm_b_im,
            "ssm_c_re": ssm_c_re, "ssm_c_im": ssm_c_im, "ssm_d": ssm_d,
            "ssm_w_glu": ssm_w_glu, "ssm_w_out": ssm_w_out,
            "att_w_in": att_w_in, "att_w_out": att_w_out}


def reference(x, norm_g, mlp_w1, mlp_w2, ssm_w_in, ssm_lam_re, ssm_lam_im, ssm_log_dt,
              ssm_b_re, ssm_b_im, ssm_c_re, ssm_c_im, ssm_d, ssm_w_glu, ssm_w_out,
              att_w_in, att_w_out):
    h = x
    for i in range(DEPTH):
        j = i // N_MIXERS
        pre = rms_norm(h, norm_g[i, 0])
        if i % N_MIXERS == 0:
            mix = s5_mixer(pre, ssm_w_in[j], ssm_lam_re[j], ssm_lam_im[j], ssm_log_dt[j],
                           ssm_b_re[j], ssm_b_im[j], ssm_c_re[j], ssm_c_im[j], ssm_d[j],
                           ssm_w_glu[j], ssm_w_out[j])
        else:
            mix = dsa_mixer(pre, att_w_in[j], att_w_out[j])
        h = h + rms_norm(mix, norm_g[i, 1])
        ff = sq_relu_mlp(rms_norm(h, norm_g[i, 2]), mlp_w1[i], mlp_w2[i])
        h = h + rms_norm(ff, norm_g[i, 3])
    return h
```

```python
import math
import numpy as np
from contextlib import ExitStack
import concourse.bass as bass
import concourse.mybir as mybir
from concourse.bass_utils import run_bass_kernel_spmd

F32 = mybir.dt.float32
BF16 = mybir.dt.bfloat16
I32 = mybir.dt.int32
ALU = mybir.AluOpType
AF = mybir.ActivationFunctionType
AX = mybir.AxisListType

D = 1024
DFF = 4096
EPS = 1e-6
NCORES = 8


class Buf:
    __slots__ = ("w", "r")

    def __init__(self):
        self.w = None
        self.r = {}


class T:
    __slots__ = ("ap", "b")

    def __init__(self, ap, b=None):
        self.ap = ap
        self.b = b if b is not None else Buf()


class Sched:
    ENGS = ("pe", "dve", "act", "pool", "sp")
    QUEUES = ("sp", "pool", "act")

    def __init__(self, nc, st, nslots=12):
        self.nc = nc
        self.sem = {e: st.enter_context(nc.semaphore("c_" + e)) for e in self.ENGS}
        self.cnt = {e: 0 for e in self.ENGS}
        self.seen = {e: {} for e in self.ENGS}
        self.ops = {e: [] for e in self.ENGS}
        self.slots = {}
        self.slot_i = {}
        for q in self.QUEUES:
            self.slots[q] = []
            self.slot_i[q] = 0
            for i in range(nslots):
                key = "d_%s%d" % (q, i)
                self.sem[key] = st.enter_context(nc.semaphore(key))
                self.slots[q].append([key, 0])

    def _deps(self, reads, writes):
        deps = {}

        def add(k, v):
            if deps.get(k, 0) < v:
                deps[k] = v

        for b in reads:
            if b.w is not None:
                add(*b.w)
        for b in writes:
            if b.w is not None:
                add(*b.w)
            for k, v in b.r.items():
                add(k, v)
        return deps

    def _waits(self, eng, deps):
        waits = []
        for k, v in deps.items():
            if k == eng and eng == "pe":
                continue
            if self.seen[eng].get(k, 0) < v:
                self.seen[eng][k] = v
                waits.append((k, v))
        return waits

    def op(self, eng, meth, kw, reads=(), writes=(), inc=True):
        fn = (meth, kw)
        waits = self._waits(eng, self._deps(reads, writes))
        if inc:
            self.cnt[eng] += 1
            tv = self.cnt[eng]
        else:
            tv = self.cnt[eng] + 1
        self.ops[eng].append((waits, fn, eng if inc else None, 1))
        for b in reads:
            if b.r.get(eng, 0) < tv:
                b.r[eng] = tv
        for b in writes:
            b.w = (eng, tv)
            b.r = {}

    def dma(self, q, out, in_, reads=(), writes=(), **kw):
        slot = self.slots[q][self.slot_i[q]]
        self.slot_i[q] = (self.slot_i[q] + 1) % len(self.slots[q])
        deps = self._deps(reads, writes)
        if slot[1] > 0 and deps.get(slot[0], 0) < slot[1]:
            deps[slot[0]] = slot[1]
        waits = self._waits(q, deps)
        slot[1] += 16
        tv = slot[1]
        key = slot[0]
        kw = dict(kw)
        kw["out"] = out
        kw["in_"] = in_
        self.ops[q].append((waits, ("dma_start", kw), key, 16))
        for b in reads:
            if b.r.get(key, 0) < tv:
                b.r[key] = tv
        for b in writes:
            b.w = (key, tv)
            b.r = {}

    def barrier(self):
        allv = {e: self.cnt[e] for e in self.ENGS}
        for q in self.QUEUES:
            for key, v in self.slots[q]:
                if v > 0:
                    allv[key] = v
        for e in self.ENGS:
            waits = []
            for k, v in allv.items():
                if v > 0 and self.seen[e].get(k, 0) < v:
                    self.seen[e][k] = v
                    waits.append((k, v))
            if waits:
                self.ops[e].append((waits, None, None, 0))

    def emit(self):
        names = {"pe": "tensor", "dve": "vector", "act": "scalar", "pool": "gpsimd", "sp": "sync"}
        with self.nc.Block() as block:
            for e in self.ENGS:
                ops = self.ops[e]

                def body(eng, ops=ops):
                    for waits, fn, inckey, incv in ops:
                        for k, v in waits:
                            eng.wait_ge(self.sem[k], v)
                        if fn is None:
                            continue
                        ins = getattr(eng, fn[0])(**fn[1])
                        if inckey is not None:
                            ins.then_inc(self.sem[inckey], incv)

                getattr(block, names[e])(body)


class Arena:
    def __init__(self, base_ap, nbytes):
        self.base = base_ap
        self.nbytes = nbytes
        self.off = 0
        self.marks = []

    def alloc(self, cols, dtype=F32, parts=128):
        esz = mybir.dt.size(dtype)
        nb = (cols * esz + 31) // 32 * 32
        assert self.off + nb <= self.nbytes, "arena overflow %d + %d > %d" % (self.off, nb, self.nbytes)
        v = self.base[0:parts, self.off // 4:(self.off + nb) // 4]
        if dtype != F32:
            v = v.bitcast(dtype)
        v = v[:, 0:cols]
        self.off += nb
        return T(v)

    def mark(self):
        self.marks.append(self.off)

    def release(self):
        self.off = self.marks.pop()


class Prog:
    def __init__(self, S, arena_bytes=200 * 1024):
        self.S = S
        self.st = ExitStack()
        st = self.st
        nc = bass.Bass("TRN2", target_bir_lowering=False)
        self.nc = nc
        self.dram = {}
        self.sch = Sched(nc, st)
        base = st.enter_context(nc.sbuf_tensor("arena", [128, arena_bytes // 4], F32))
        self.ar = Arena(base[:, :], arena_bytes)
        self.psum = []
        for i in range(8):
            t = st.enter_context(nc.psum_tensor("ps%d" % i, [128, 512], F32))
            self.psum.append(T(t[:, :]))
        self.ident_bf = self.ar.alloc(128, BF16)
        self.ident_f = self.ar.alloc(128, F32)
        self.eps = self.ar.alloc(1, F32)
        idd = self.din("ident", [128, 128])
        self.sch.dma("sp", self.ident_f.ap, idd, writes=[self.ident_f.b])
        self.sch.op("dve", "tensor_copy", dict(out=self.ident_bf.ap, in_=self.ident_f.ap),
                    reads=[self.ident_f.b], writes=[self.ident_bf.b])
        self.sch.op("dve", "memset", dict(ap=self.eps.ap, constant=EPS), writes=[self.eps.b])

    def din(self, name, shape, dtype=F32):
        t = self.nc.dram_tensor(name, list(shape), dtype, kind="ExternalInput").ap()
        self.dram[name] = t
        return t

    def dout(self, name, shape, dtype=F32):
        t = self.nc.dram_tensor(name, list(shape), dtype, kind="ExternalOutput").ap()
        self.dram[name] = t
        return t

    def dscratch(self, name, shape, dtype=F32):
        t = self.nc.dram_tensor(name, list(shape), dtype, kind="Internal").ap()
        self.dram[name] = t
        return t

    def phase_begin(self):
        self.ar.mark()

    def phase_end(self):
        self.sch.barrier()
        self.ar.release()

    def rstd(self, out, ss, n):
        sch = self.sch
        P = ss.ap.shape[0]
        sch.op("act", "activation", dict(out=out.ap, in_=ss.ap, func=AF.Sqrt, bias=self.eps.ap[0:P, :], scale=1.0 / n),
               reads=[ss.b, self.eps.b], writes=[out.b])
        sch.op("dve", "reciprocal", dict(out=out.ap, in_=out.ap), reads=[out.b], writes=[out.b])


def mlp_phase(P, src, dst, w1d, w2d, gin_row, gout_row):
    sch, ar, S = P.sch, P.ar, P.S
    TT = 256
    P.phase_begin()
    w1 = [ar.alloc(8 * 512, BF16) for _ in range(8)]
    w2 = [ar.alloc(4 * 1024, BF16) for _ in range(8)]
    w1src = w1d.rearrange("(c p) f -> p c f", p=128)
    w2src = w2d.rearrange("(c p) d -> p c d", p=128)
    for i in range(8):
        sch.dma("pool", w1[i].ap.rearrange("p (c f) -> p c f", f=512), w1src[:, :, i * 512:(i + 1) * 512],
                writes=[w1[i].b])
    for i in range(8):
        sch.dma("pool", w2[i].ap.rearrange("p (c d) -> p c d", d=1024), w2src[:, 4 * i:4 * i + 4, :],
                writes=[w2[i].b])
    gin = ar.alloc(D)
    gout = ar.alloc(D)
    sch.dma("sp", gin.ap, gin_row.partition_broadcast(128), writes=[gin.b])
    sch.dma("sp", gout.ap, gout_row.partition_broadcast(128), writes=[gout.b])
    NB = 2
    hs = [[ar.alloc(D) for _ in range(2)] for _ in range(NB)]
    hn = [ar.alloc(D, BF16) for _ in range(2)]
    hnT = [ar.alloc(8 * TT, BF16) for _ in range(NB)]
    ss = [ar.alloc(2) for _ in range(NB)]
    rs = [ar.alloc(2) for _ in range(NB)]
    rs2 = [ar.alloc(2) for _ in range(NB)]
    junk = ar.alloc(D, BF16)
    rr = [ar.alloc(TT) for _ in range(2)]
    aT = [ar.alloc(TT, BF16) for _ in range(2)]
    yt = ar.alloc(D)
    acc = P.psum[0:4]
    pm = P.psum[4:6]
    pt = P.psum[6:8]
    ntile = S // TT

    def load_tile(t):
        for sub in range(2):
            r0 = t * TT + sub * 128
            sch.dma("sp", hs[t % NB][sub].ap, src[r0:r0 + 128, :], writes=[hs[t % NB][sub].b])

    load_tile(0)
    for t in range(ntile):
        pb = t % NB
        h = hs[pb]
        for sub in range(2):
            sch.op("act", "activation", dict(out=junk.ap, in_=h[sub].ap, func=AF.Square,
                                             accum_out=ss[pb].ap[:, sub:sub + 1]),
                   reads=[h[sub].b], writes=[junk.b, ss[pb].b])
        P.rstd(rs[pb], ss[pb], D)
        for sub in range(2):
            sch.op("dve", "scalar_tensor_tensor", dict(out=hn[sub].ap, in0=h[sub].ap, scalar=rs[pb].ap[:, sub:sub + 1],
                                                       in1=gin.ap, op0=ALU.mult, op1=ALU.mult),
                   reads=[h[sub].b, rs[pb].b, gin.b], writes=[hn[sub].b])
        for kc in range(8):
            ptb = pt[kc % 2]
            ptv = ptb.ap.bitcast(BF16)
            for sub in range(2):
                sch.op("pe", "transpose", dict(out=ptv[:, sub * 128:(sub + 1) * 128],
                                               in_=hn[sub].ap[:, kc * 128:(kc + 1) * 128], identity=P.ident_bf.ap),
                       reads=[hn[sub].b, P.ident_bf.b], writes=[ptb.b], inc=(sub == 1))
            if kc % 2 == 0:
                sch.op("act", "copy", dict(out=hnT[pb].ap[:, kc * TT:(kc + 1) * TT], in_=ptv[:, 0:TT]),
                       reads=[ptb.b], writes=[hnT[pb].b])
            else:
                sch.op("dve", "tensor_copy", dict(out=hnT[pb].ap[:, kc * TT:(kc + 1) * TT], in_=ptv[:, 0:TT]),
                       reads=[ptb.b], writes=[hnT[pb].b])
        if t + 1 < ntile:
            load_tile(t + 1)

        def mm1(f):
            pmb = pm[f % 2]
            wt = w1[f // 4]
            c0 = (f % 4) * 128
            for kc in range(8):
                sch.op("pe", "matmul", dict(out=pmb.ap[:, 0:TT], lhsT=wt.ap[:, kc * 512 + c0:kc * 512 + c0 + 128],
                                            rhs=hnT[pb].ap[:, kc * TT:(kc + 1) * TT], start=(kc == 0), stop=(kc == 7)),
                       reads=[wt.b, hnT[pb].b], writes=[pmb.b], inc=(kc == 7))
            sch.op("act", "activation", dict(out=rr[f % 2].ap, in_=pmb.ap[:, 0:TT], func=AF.Relu),
                   reads=[pmb.b], writes=[rr[f % 2].b])
            sch.op("dve", "tensor_tensor", dict(out=aT[f % 2].ap, in0=rr[f % 2].ap, in1=rr[f % 2].ap, op=ALU.mult),
                   reads=[rr[f % 2].b], writes=[aT[f % 2].b])

        def mm2(f):
            wt = w2[f // 4]
            o = (f % 4) * 1024
            for sub in range(2):
                for half in range(2):
                    a = acc[sub * 2 + half]
                    sch.op("pe", "matmul", dict(out=a.ap[:, 0:512], lhsT=aT[f % 2].ap[:, sub * 128:(sub + 1) * 128],
                                                rhs=wt.ap[:, o + half * 512:o + (half + 1) * 512],
                                                start=(f == 0), stop=(f == 31)),
                           reads=[aT[f % 2].b, wt.b], writes=[a.b], inc=(f == 31 or (sub == 1 and half == 1)))

        mm1(0)
        for f in range(32):
            if f + 1 < 32:
                mm1(f + 1)
            mm2(f)
        for sub in range(2):
            for half in range(2):
                a = acc[sub * 2 + half]
                sch.op("act", "activation", dict(out=junk.ap[:, 0:512], in_=a.ap[:, 0:512], func=AF.Square,
                                                 accum_out=rs2[pb].ap[:, half:half + 1]),
                       reads=[a.b], writes=[junk.b, rs2[pb].b])
            sch.op("dve", "tensor_tensor", dict(out=rs2[pb].ap[:, 0:1], in0=rs2[pb].ap[:, 0:1],
                                                in1=rs2[pb].ap[:, 1:2], op=ALU.add),
                   reads=[rs2[pb].b], writes=[rs2[pb].b])
            r1 = T(rs2[pb].ap[:, 0:1], rs2[pb].b)
            P.rstd(r1, r1, D)
            for half in range(2):
                a = acc[sub * 2 + half]
                sch.op("dve", "scalar_tensor_tensor", dict(
                    out=yt.ap[:, half * 512:(half + 1) * 512], in0=a.ap[:, 0:512], scalar=rs2[pb].ap[:, 0:1],
                    in1=gout.ap[:, half * 512:(half + 1) * 512], op0=ALU.mult, op1=ALU.mult),
                    reads=[a.b, rs2[pb].b, gout.b], writes=[yt.b])
            sch.op("pool", "tensor_tensor", dict(out=h[sub].ap, in0=yt.ap, in1=h[sub].ap, op=ALU.add),
                   reads=[yt.b, h[sub].b], writes=[h[sub].b])
            r0 = t * TT + sub * 128
            sch.dma("sp", dst[r0:r0 + 128, :], h[sub].ap, reads=[h[sub].b])
    P.phase_end()


G, PST, CH, E = 32, 64, 16, 512
TWO_PI_S = 6.2831845


def s5_host_layout(lam_re, lam_im, log_dt, b_re, b_im, c_re, c_im, d_skip):
    lr = np.concatenate([lam_re.T, lam_re.T], 0)
    li = np.concatenate([lam_im.T, lam_im.T], 0)
    ld = np.broadcast_to(log_dt[None, :], (128, G))
    bre = b_re.transpose(1, 0, 2)
    bim = b_im.transpose(1, 0, 2)
    cre = c_re.transpose(2, 0, 1)
    cim = c_im.transpose(2, 0, 1)
    B1 = np.concatenate([bre, bim], 0).reshape(128, G * CH)
    B2 = np.concatenate([bim, bre], 0).reshape(128, G * CH)
    C1 = np.concatenate([cre, cim], 0).reshape(128, G * CH)
    C2 = np.concatenate([cim, cre], 0).reshape(128, G * CH)
    dcol = np.tile(d_skip.T, (8, 1))
    small = np.concatenate([lr, li, ld, dcol], 1)
    big = np.concatenate([B1, B2, C1, C2], 1)
    return np.ascontiguousarray(small, dtype=np.float32), np.ascontiguousarray(big, dtype=np.float32)


def s5_consts():
    sg = np.ones((128, 2), np.float32)
    sg[64:, 0] = -1.0
    sg[:64, 1] = -1.0
    j = np.arange(128) // 16
    mask = (j[None, :] >= j[:, None]).astype(np.float32)
    return np.concatenate([sg, mask], 1)


def s5_phase(P, src, dst, small_d, big_d, cst_d, w_in_d, w_glu_d, w_out_d, gin_row, gout_row, NI=2):
    sch, ar, S = P.sch, P.ar, P.S
    KP = S // 8
    NM = KP // NI
    NKB = KP // 128
    P.phase_begin()
    small = ar.alloc(128)
    big = ar.alloc(2048)
    cst = ar.alloc(130)
    sch.dma("sp", small.ap, small_d, writes=[small.b])
    sch.dma("sp", big.ap, big_d, writes=[big.b])
    sch.dma("sp", cst.ap, cst_d, writes=[cst.b])
    gin = ar.alloc(D)
    gout = ar.alloc(D)
    sch.dma("sp", gin.ap, gin_row.partition_broadcast(128), writes=[gin.b])
    sch.dma("sp", gout.ap, gout_row.partition_broadcast(128), writes=[gout.b])
    sg = cst.ap[:, 0:1]
    nsg = cst.ap[:, 1:2]
    cmask = cst.ap[:, 2:130]
    lr = small.ap[:, 0:32]
    li = small.ap[:, 32:64]
    ld = small.ap[:, 64:96]
    dcol = small.ap[:, 96:128]
    B1 = big.ap[:, 0:512].rearrange("p (g c) -> p g c", c=CH)
    B2 = big.ap[:, 512:1024].rearrange("p (g c) -> p g c", c=CH)
    C1 = big.ap[:, 1024:1536].rearrange("p (g c) -> p g c", c=CH)
    C2 = big.ap[:, 1536:2048].rearrange("p (g c) -> p g c", c=CH)

    EC = list(range(-7, 8 * NI + 1))
    EB = [8 * (NI - 1 - i) + 7 - j for i in range(NI) for j in range(8)]
    EX = EC + EB
    NE = len(EX)
    NC_ = len(EC)
    tb = T(None)
    def tl(cols, dt=F32):
        t = ar.alloc(cols, dt)
        return t.ap
    Pr = tl(32 * NE); Pi = tl(32 * NE); nsPi = tl(32 * NE); sPi = tl(32 * NE)
    Bb1 = tl(512); Bb2 = tl(512)
    zT = ar.alloc(4 * 8 * KP, BF16)
    zTv = zT.ap.rearrange("p (e j k) -> p e j k", j=8, k=KP)
    off_after_zT = ar.off
    U = ar.alloc(G * KP, BF16)
    Uv = U.ap.rearrange("p (g k) -> p g k", k=KP)
    ar.mark()
    dt_ = tl(32); lrdt = tl(32); lidt = tl(32)
    argm = tl(32 * NE); arga = tl(32 * NE); argi = tl(32 * NE, I32); argf = tl(32 * NE)
    m1 = tl(32 * NE)
    R = [small.b, big.b, cst.b, tb.b]
    W = [tb.b]
    def dve(meth, **kw):
        sch.op("dve", meth, kw, reads=R, writes=W)
    def act(**kw):
        sch.op("act", "activation", kw, reads=R, writes=W)
    act(out=dt_, in_=ld, func=AF.Exp)
    dve("tensor_tensor", out=lrdt, in0=lr, in1=dt_, op=ALU.mult)
    dve("tensor_tensor", out=lidt, in0=li, in1=dt_, op=ALU.mult)
    v3 = lambda a: a.rearrange("p (g n) -> p g n", n=NE)
    for i, n in enumerate(EX):
        dve("tensor_scalar", out=v3(argm)[:, :, i], in0=lrdt, scalar1=float(n), scalar2=None, op0=ALU.mult)
        dve("tensor_scalar", out=v3(arga)[:, :, i], in0=lidt, scalar1=float(n) / (2 * math.pi), scalar2=None, op0=ALU.mult)
    act(out=argm, in_=argm, func=AF.Exp)
    dve("tensor_copy", out=argi, in_=arga)
    dve("tensor_copy", out=argf, in_=argi)
    dve("tensor_tensor", out=arga, in0=arga, in1=argf, op=ALU.subtract)
    def wrap(y):
        dve("tensor_scalar", out=m1, in0=y, scalar1=0.5, scalar2=None, op0=ALU.is_gt)
        dve("tensor_tensor", out=y, in0=y, in1=m1, op=ALU.subtract)
        dve("tensor_scalar", out=m1, in0=y, scalar1=-0.5, scalar2=None, op0=ALU.is_lt)
        dve("tensor_tensor", out=y, in0=y, in1=m1, op=ALU.add)
    wrap(arga)
    act(out=Pi, in_=arga, func=AF.Sin, scale=TWO_PI_S)
    dve("tensor_scalar", out=arga, in0=arga, scalar1=0.25, scalar2=None, op0=ALU.add)
    wrap(arga)
    act(out=Pr, in_=arga, func=AF.Sin, scale=TWO_PI_S)
    dve("tensor_tensor", out=Pr, in0=Pr, in1=argm, op=ALU.mult)
    dve("tensor_tensor", out=Pi, in0=Pi, in1=argm, op=ALU.mult)
    dve("tensor_scalar", out=nsPi, in0=Pi, scalar1=nsg, scalar2=None, op0=ALU.mult)
    dve("tensor_scalar", out=sPi, in0=Pi, scalar1=sg, scalar2=None, op0=ALU.mult)
    i1 = EC.index(1)
    nr = tl(32); den = tl(32); fr = tl(32); fi = tl(32); t32 = tl(32); nsfi = tl(32); sfi = tl(32)
    dve("tensor_scalar", out=nr, in0=v3(Pr)[:, :, i1], scalar1=-1.0, scalar2=None, op0=ALU.add)
    ni_ = v3(Pi)[:, :, i1]
    dve("tensor_tensor", out=den, in0=lr, in1=lr, op=ALU.mult)
    dve("tensor_tensor", out=t32, in0=li, in1=li, op=ALU.mult)
    dve("tensor_tensor", out=den, in0=den, in1=t32, op=ALU.add)
    dve("reciprocal", out=den, in_=den)
    dve("tensor_tensor", out=fr, in0=nr, in1=lr, op=ALU.mult)
    dve("tensor_tensor", out=t32, in0=ni_, in1=li, op=ALU.mult)
    dve("tensor_tensor", out=fr, in0=fr, in1=t32, op=ALU.add)
    dve("tensor_tensor", out=fr, in0=fr, in1=den, op=ALU.mult)
    dve("tensor_tensor", out=fi, in0=ni_, in1=lr, op=ALU.mult)
    dve("tensor_tensor", out=t32, in0=nr, in1=li, op=ALU.mult)
    dve("tensor_tensor", out=fi, in0=fi, in1=t32, op=ALU.subtract)
    dve("tensor_tensor", out=fi, in0=fi, in1=den, op=ALU.mult)
    dve("tensor_scalar", out=nsfi, in0=fi, scalar1=nsg, scalar2=None, op0=ALU.mult)
    dve("tensor_scalar", out=sfi, in0=fi, scalar1=sg, scalar2=None, op0=ALU.mult)
    tB = tl(512)
    g3 = lambda a: a.rearrange("p (g c) -> p g c", c=CH)
    bc = lambda a: a.unsqueeze(2).broadcast_to([128, 32, CH])
    dve("tensor_tensor", out=g3(Bb1), in0=B1, in1=bc(fr), op=ALU.mult)
    dve("tensor_tensor", out=g3(tB), in0=B2, in1=bc(nsfi), op=ALU.mult)
    dve("tensor_tensor", out=Bb1, in0=Bb1, in1=tB, op=ALU.add)
    dve("tensor_tensor", out=g3(Bb2), in0=B2, in1=bc(fr), op=ALU.mult)
    dve("tensor_tensor", out=g3(tB), in0=B1, in1=bc(sfi), op=ALU.mult)
    dve("tensor_tensor", out=Bb2, in0=Bb2, in1=tB, op=ALU.add)
    iA = EC.index(8 * NI)
    A1 = v3(Pr)[:, :, iA]
    A2 = v3(nsPi)[:, :, iA]
    A2w = v3(sPi)[:, :, iA]

    NB8 = NI * 8
    def ba_tables(g, outst, outsw, tmp):
        o4 = lambda a: a.rearrange("p (n c) -> p n c", c=CH)
        prb = v3(Pr)[:, g, NC_:NE].unsqueeze(2).broadcast_to([128, NB8, CH])
        nspb = v3(nsPi)[:, g, NC_:NE].unsqueeze(2).broadcast_to([128, NB8, CH])
        spb = v3(sPi)[:, g, NC_:NE].unsqueeze(2).broadcast_to([128, NB8, CH])
        b1 = g3(Bb1)[:, g, :].unsqueeze(1).broadcast_to([128, NB8, CH])
        b2 = g3(Bb2)[:, g, :].unsqueeze(1).broadcast_to([128, NB8, CH])
        rw = dict(reads=[tb.b], writes=[outst.b, outsw.b, tmp.b])
        sch.op("dve", "tensor_tensor", dict(out=o4(outst.ap), in0=prb, in1=b1, op=ALU.mult), **rw)
        sch.op("dve", "tensor_tensor", dict(out=o4(tmp.ap), in0=nspb, in1=b2, op=ALU.mult), **rw)
        sch.op("dve", "tensor_tensor", dict(out=outst.ap, in0=outst.ap, in1=tmp.ap, op=ALU.add), **rw)
        if outsw is not outst:
            sch.op("dve", "tensor_tensor", dict(out=o4(outsw.ap), in0=prb, in1=b2, op=ALU.mult), **rw)
            sch.op("dve", "tensor_tensor", dict(out=o4(tmp.ap), in0=spb, in1=b1, op=ALU.mult), **rw)
            sch.op("dve", "tensor_tensor", dict(out=outsw.ap, in0=outsw.ap, in1=tmp.ap, op=ALU.add), **rw)

    sch.barrier()
    ar.release()
    ar.mark()
    w_in = ar.alloc(8 * E, BF16)
    sch.dma("pool", w_in.ap.rearrange("p (c e) -> p c e", e=E), w_in_d.rearrange("(c p) e -> p c e", p=128), writes=[w_in.b])
    hblk = ar.alloc(8 * D)
    hnj = [ar.alloc(D, BF16) for _ in range(2)]
    hnT = [ar.alloc(8 * 128, BF16) for _ in range(2)]
    utok = ar.alloc(8 * E, BF16)
    utv = utok.ap.rearrange("p (g j c) -> p g j c", j=8, c=CH)
    ss = ar.alloc(8); rs = ar.alloc(8)
    junk = ar.alloc(D, BF16)
    pt = P.psum[6:8]
    pu = P.psum[4:6]
    for kb in range(NKB):
        r0 = kb * 1024
        sch.dma("sp", hblk.ap, src[r0:r0 + 1024, :].rearrange("(k j) d -> k (j d)", j=8), writes=[hblk.b])
        for j in range(8):
            sch.op("act", "activation", dict(out=junk.ap, in_=hblk.ap[:, j * D:(j + 1) * D], func=AF.Square,
                                             accum_out=ss.ap[:, j:j + 1]), reads=[hblk.b], writes=[junk.b, ss.b])
        P.rstd(rs, ss, D)
        for j in range(8):
            hn = hnj[j % 2]
            sch.op("dve", "scalar_tensor_tensor", dict(out=hn.ap, in0=hblk.ap[:, j * D:(j + 1) * D],
                                                       scalar=rs.ap[:, j:j + 1], in1=gin.ap, op0=ALU.mult, op1=ALU.mult),
                   reads=[hblk.b, rs.b, gin.b], writes=[hn.b])
            hT = hnT[j % 2]
            for kc2 in range(2):
                ptb = pt[kc2]
                ptv = ptb.ap.bitcast(BF16)
                for q in range(4):
                    kc = kc2 * 4 + q
                    sch.op("pe", "transpose", dict(out=ptv[:, q * 128:(q + 1) * 128],
                                                   in_=hn.ap[:, kc * 128:(kc + 1) * 128], identity=P.ident_bf.ap),
                           reads=[hn.b, P.ident_bf.b], writes=[ptb.b], inc=(q == 3))
                if kc2 == 0:
                    sch.op("act", "copy", dict(out=hT.ap[:, 0:512], in_=ptv[:, 0:512]), reads=[ptb.b], writes=[hT.b])
                else:
                    sch.op("dve", "tensor_copy", dict(out=hT.ap[:, 512:1024], in_=ptv[:, 0:512]), reads=[ptb.b], writes=[hT.b])
            pub = pu[j % 2]
            for kc in range(8):
                sch.op("pe", "matmul", dict(out=pub.ap[:, 0:E], lhsT=hT.ap[:, kc * 128:(kc + 1) * 128],
                                            rhs=w_in.ap[:, kc * E:(kc + 1) * E], start=(kc == 0), stop=(kc == 7)),
                       reads=[hT.b, w_in.b], writes=[pub.b], inc=(kc == 7))
            sch.op("act", "copy", dict(out=utv[:, :, j, :], in_=pub.ap[:, 0:E].rearrange("p (g c) -> p g c", c=CH)),
                   reads=[pub.b], writes=[utok.b])
        for g4 in range(8):
            ptb = pt[g4 % 2]
            ptv = ptb.ap.bitcast(BF16)
            for q in range(4):
                g = g4 * 4 + q
                sch.op("pe", "transpose", dict(out=ptv[:, q * 128:(q + 1) * 128], in_=utok.ap[:, g * 128:(g + 1) * 128],
                                               identity=P.ident_bf.ap),
                       reads=[utok.b, P.ident_bf.b], writes=[ptb.b], inc=(q == 3))
            eng = ("act", "copy") if g4 % 2 == 0 else ("dve", "tensor_copy")
            sch.op(eng[0], eng[1], dict(out=Uv[:, g4 * 4:g4 * 4 + 4, kb * 128:(kb + 1) * 128],
                                        in_=ptv[:, 0:512].rearrange("p (g k) -> p g k", k=128)),
                   reads=[ptb.b], writes=[U.b])

    sch.barrier()
    ar.release()
    Sst = ar.alloc(G * NM, BF16)
    Ssw = ar.alloc(G * NM, BF16)
    Sstv = Sst.ap.rearrange("p (g m) -> p g m", m=NM)
    Sswv = Ssw.ap.rearrange("p (g m) -> p g m", m=NM)
    bast = [ar.alloc(NB8 * CH) for _ in range(2)]
    basw = [ar.alloc(NB8 * CH) for _ in range(2)]
    btmp = ar.alloc(NB8 * CH)
    wsst = [ar.alloc(NI * 128, BF16) for _ in range(2)]
    wssw = [ar.alloc(NI * 128, BF16) for _ in range(2)]
    ps_s = P.psum[0:4]
    for g in range(G):
        pb = g % 2
        ba_tables(g, bast[pb], basw[pb], btmp)
        for which, (ba, ws) in enumerate(((bast[pb], wsst[pb]), (basw[pb], wssw[pb]))):
            ptb = pt[which]
            for i in range(NI):
                sch.op("pe", "transpose", dict(out=ptb.ap[:, i * 128:(i + 1) * 128], in_=ba.ap[:, i * 128:(i + 1) * 128],
                                               identity=P.ident_f.ap),
                       reads=[ba.b, P.ident_f.b], writes=[ptb.b], inc=(i == NI - 1))
            sch.op("act", "copy", dict(out=ws.ap, in_=ptb.ap[:, 0:NI * 128]), reads=[ptb.b], writes=[ws.b])
        for which, (ws, Sv, Sb) in enumerate(((wsst[pb], Sstv, Sst), (wssw[pb], Sswv, Ssw))):
            psb = ps_s[(g % 2) * 2 + which]
            for i in range(NI):
                rhs = Uv[:, g, :].rearrange("p (m i) -> p m i", i=NI)[:, :, i]
                sch.op("pe", "matmul", dict(out=psb.ap[:, 0:NM], lhsT=ws.ap[:, i * 128:(i + 1) * 128], rhs=rhs,
                                            start=(i == 0), stop=(i == NI - 1)),
                       reads=[ws.b, U.b], writes=[psb.b], inc=(i == NI - 1))
            sch.op("dve", "tensor_copy", dict(out=Sv[:, g, :], in_=psb.ap[:, 0:NM]), reads=[psb.b], writes=[Sb.b])

    Xs = ar.alloc(32); Xw = ar.alloc(32); t1 = ar.alloc(32); t2 = ar.alloc(32)
    sb = T(None)
    rw = dict(reads=[tb.b, sb.b, Sst.b, Ssw.b], writes=[sb.b])
    sch.op("dve", "memset", dict(ap=Xs.ap, constant=0.0), **rw)
    sch.op("dve", "memset", dict(ap=Xw.ap, constant=0.0), **rw)
    for m in range(NM):
        tt = lambda **kw: sch.op("dve", "tensor_tensor", kw, **rw)
        tt(out=t1.ap, in0=Xs.ap, in1=A1, op=ALU.mult)
        tt(out=t2.ap, in0=Xw.ap, in1=A2, op=ALU.mult)
        tt(out=t1.ap, in0=t1.ap, in1=t2.ap, op=ALU.add)
        tt(out=t2.ap, in0=Xw.ap, in1=A1, op=ALU.mult)
        tt(out=Xw.ap, in0=Xs.ap, in1=A2w, op=ALU.mult)
        tt(out=Xw.ap, in0=Xw.ap, in1=t2.ap, op=ALU.add)
        tt(out=Xw.ap, in0=Xw.ap, in1=Sswv[:, :, m], op=ALU.add)
        tt(out=Xs.ap, in0=t1.ap, in1=Sstv[:, :, m], op=ALU.add)
        sch.op("dve", "tensor_copy", dict(out=Sstv[:, :, m], in_=Xs.ap), reads=[sb.b], writes=[sb.b, Sst.b])

    cast = [ar.alloc((NC_) * CH) for _ in range(2)]
    ctmp = ar.alloc((NC_) * CH)
    L1 = [ar.alloc(128) for _ in range(2)]
    Tg = [ar.alloc(NI * 128, BF16) for _ in range(2)]
    WY = [ar.alloc(NI * 128, BF16) for _ in range(2)]
    tmask = ar.alloc(128)
    zg = [ar.alloc(KP, BF16) for _ in range(2)]
    gx2 = ar.alloc(KP); gt = ar.alloc(KP)
    psy = P.psum[0:2]
    pst = P.psum[2:4]
    for g in range(G):
        pb = g % 2
        ba_tables(g, bast[pb], bast[pb], btmp)
        o4 = lambda a: a.rearrange("p (n c) -> p n c", c=CH)
        prc = v3(Pr)[:, g, 0:NC_].unsqueeze(2).broadcast_to([128, NC_, CH])
        nspc = v3(nsPi)[:, g, 0:NC_].unsqueeze(2).broadcast_to([128, NC_, CH])
        c1 = C1[:, g, :].unsqueeze(1).broadcast_to([128, NC_, CH])
        c2 = C2[:, g, :].unsqueeze(1).broadcast_to([128, NC_, CH])
        rwc = dict(reads=[tb.b, big.b], writes=[cast[pb].b, ctmp.b])
        sch.op("dve", "tensor_tensor", dict(out=o4(cast[pb].ap), in0=prc, in1=c1, op=ALU.mult), **rwc)
        sch.op("dve", "tensor_tensor", dict(out=o4(ctmp.ap), in0=nspc, in1=c2, op=ALU.mult), **rwc)
        sch.op("dve", "tensor_tensor", dict(out=cast[pb].ap, in0=cast[pb].ap, in1=ctmp.ap, op=ALU.add), **rwc)
        sch.op("dve", "tensor_scalar", dict(out=L1[pb].ap, in0=bast[pb].ap[:, (NI - 1) * 128:NI * 128], scalar1=sg, scalar2=None, op0=ALU.mult),
               reads=[bast[pb].b, cst.b], writes=[L1[pb].b])
        ptb = pst[pb]
        for dl in range(NI):
            sch.op("pe", "matmul", dict(out=ptb.ap[:, dl * 128:(dl + 1) * 128], lhsT=L1[pb].ap,
                                        rhs=cast[pb].ap[:, dl * 128:(dl + 1) * 128], start=True, stop=True),
                   reads=[L1[pb].b, cast[pb].b], writes=[ptb.b], inc=(dl == NI - 1))
        sch.op("dve", "tensor_tensor", dict(out=tmask.ap, in0=ptb.ap[:, 0:128], in1=cmask, op=ALU.mult),
               reads=[ptb.b, cst.b], writes=[tmask.b])
        sch.op("dve", "scalar_tensor_tensor", dict(out=Tg[pb].ap[:, 0:128], in0=P.ident_f.ap, scalar=dcol[:, g:g + 1], in1=tmask.ap,
                                                   op0=ALU.mult, op1=ALU.add),
               reads=[tmask.b, P.ident_f.b, small.b], writes=[Tg[pb].b])
        if NI > 1:
            sch.op("act", "copy", dict(out=Tg[pb].ap[:, 128:NI * 128], in_=ptb.ap[:, 128:NI * 128]), reads=[ptb.b], writes=[Tg[pb].b])
        sch.op("dve", "tensor_scalar", dict(out=WY[pb].ap, in0=cast[pb].ap[:, 128:128 + NI * 128], scalar1=sg, scalar2=None, op0=ALU.mult),
               reads=[cast[pb].b, cst.b], writes=[WY[pb].b])
        yb = psy[pb]
        yv = yb.ap[:, 0:KP].rearrange("p (m i) -> p m i", i=NI)
        uv = Uv[:, g, :].rearrange("p (m i) -> p m i", i=NI)
        for dl in range(NI):
            sch.op("pe", "matmul", dict(out=yv[:, :, dl:NI], lhsT=Tg[pb].ap[:, dl * 128:(dl + 1) * 128], rhs=uv[:, :, 0:NI - dl],
                                        start=(dl == 0), stop=False),
                   reads=[Tg[pb].b, U.b], writes=[yb.b], inc=False)
        for i in range(NI):
            sch.op("pe", "matmul", dict(out=yv[:, 1:NM, i], lhsT=WY[pb].ap[:, i * 128:(i + 1) * 128], rhs=Sstv[:, g, 0:NM - 1],
                                        start=False, stop=(i == NI - 1)),
                   reads=[WY[pb].b, Sst.b], writes=[yb.b], inc=(i == NI - 1))
        y = yb.ap[:, 0:KP]
        sch.op("act", "activation", dict(out=gx2.ap, in_=y, func=AF.Square), reads=[yb.b], writes=[gx2.b])
        sch.op("dve", "tensor_scalar", dict(out=gx2.ap, in0=gx2.ap, scalar1=0.044715, scalar2=1.0, op0=ALU.mult, op1=ALU.add),
               reads=[gx2.b], writes=[gx2.b])
        sch.op("dve", "tensor_tensor", dict(out=gt.ap, in0=y, in1=gx2.ap, op=ALU.mult), reads=[yb.b, gx2.b], writes=[gt.b])
        sch.op("act", "activation", dict(out=gt.ap, in_=gt.ap, func=AF.Sigmoid, scale=1.5957691216057308), reads=[gt.b], writes=[gt.b])
        sch.op("dve", "tensor_tensor", dict(out=zg[pb].ap, in0=y, in1=gt.ap, op=ALU.mult), reads=[yb.b, gt.b], writes=[zg[pb].b])
        ech, gi = g // 8, g % 8
        for j in range(8):
            sch.dma("sp" if j % 2 == 0 else "pool", zTv[gi * 16:(gi + 1) * 16, ech, j, :], zg[pb].ap[j * 16:(j + 1) * 16, :],
                    reads=[zg[pb].b], writes=[zT.b])

    sch.barrier()
    ar.off = off_after_zT
    w_glu = ar.alloc(4 * E, BF16)
    w_out = ar.alloc(4 * D, BF16)
    sch.dma("pool", w_glu.ap.rearrange("p (c e) -> p c e", e=E), w_glu_d.rearrange("(c p) e -> p c e", p=128), writes=[w_glu.b])
    sch.dma("pool", w_out.ap.rearrange("p (c e) -> p c e", e=D), w_out_d.rearrange("(c p) e -> p c e", p=128), writes=[w_out.b])
    junk = ar.alloc(D, BF16)
    z2 = [ar.alloc(4 * 512, BF16) for _ in range(2)]
    sgm = ar.alloc(512)
    hres = [ar.alloc(D) for _ in range(2)]
    yt = ar.alloc(D)
    rs2 = ar.alloc(2)
    pg = P.psum[4:6]
    po = P.psum[0:4]
    NKC = KP // 512 if KP >= 512 else 1
    KW = min(KP, 512)
    cnt = 0
    for j in range(8):
        for kc5 in range(NKC):
            k0 = kc5 * KW
            zz = z2[cnt % 2]
            for eo in range(4):
                pgb = pg[eo % 2]
                for ei in range(4):
                    sch.op("pe", "matmul", dict(out=pgb.ap[:, 0:KW], lhsT=w_glu.ap[:, ei * E + eo * 128:ei * E + (eo + 1) * 128],
                                                rhs=zTv[:, ei, j, k0:k0 + KW], start=(ei == 0), stop=(ei == 3)),
                           reads=[w_glu.b, zT.b], writes=[pgb.b], inc=(ei == 3))
                sch.op("act", "activation", dict(out=sgm.ap[:, 0:KW], in_=pgb.ap[:, 0:KW], func=AF.Sigmoid), reads=[pgb.b], writes=[sgm.b])
                sch.op("dve", "tensor_tensor", dict(out=zz.ap[:, eo * 512:eo * 512 + KW], in0=sgm.ap[:, 0:KW], in1=zTv[:, eo, j, k0:k0 + KW], op=ALU.mult),
                       reads=[sgm.b, zT.b], writes=[zz.b])
            for kq in range(KW // 128):
                kk = k0 + kq * 128
                hb = hres[kq % 2]
                rows = src.rearrange("(k j) d -> j k d", j=8)[j, kk:kk + 128, :]
                orows = dst.rearrange("(k j) d -> j k d", j=8)[j, kk:kk + 128, :]
                sch.dma("sp", hb.ap, rows, writes=[hb.b])
                for half in range(2):
                    pob = po[(kq % 2) * 2 + half]
                    for ei in range(4):
                        sch.op("pe", "matmul", dict(out=pob.ap[:, 0:512], lhsT=zz.ap[:, ei * 512 + kq * 128:ei * 512 + (kq + 1) * 128],
                                                    rhs=w_out.ap[:, ei * D + half * 512:ei * D + (half + 1) * 512],
                                                    start=(ei == 0), stop=(ei == 3)),
                               reads=[zz.b, w_out.b], writes=[pob.b], inc=(ei == 3))
                    sch.op("act", "activation", dict(out=junk.ap[:, 0:512], in_=pob.ap[:, 0:512], func=AF.Square,
                                                     accum_out=rs2.ap[:, half:half + 1]), reads=[pob.b], writes=[junk.b, rs2.b])
                sch.op("dve", "tensor_tensor", dict(out=rs2.ap[:, 0:1], in0=rs2.ap[:, 0:1], in1=rs2.ap[:, 1:2], op=ALU.add),
                       reads=[rs2.b], writes=[rs2.b])
                r1 = T(rs2.ap[:, 0:1], rs2.b)
                P.rstd(r1, r1, D)
                for half in range(2):
                    pob = po[(kq % 2) * 2 + half]
                    sch.op("dve", "scalar_tensor_tensor", dict(out=yt.ap[:, half * 512:(half + 1) * 512], in0=pob.ap[:, 0:512],
                                                               scalar=rs2.ap[:, 0:1], in1=gout.ap[:, half * 512:(half + 1) * 512],
                                                               op0=ALU.mult, op1=ALU.mult),
                           reads=[pob.b, rs2.b, gout.b], writes=[yt.b])
                sch.op("pool", "tensor_tensor", dict(out=hb.ap, in0=yt.ap, in1=hb.ap, op=ALU.add), reads=[yt.b, hb.b], writes=[hb.b])
                sch.dma("sp", orows, hb.ap, reads=[hb.b])
            cnt += 1
    P.phase_end()


HP = [0, 4, 1, 5, 2, 6, 3, 7, 8, 12, 9, 13, 10, 14, 11, 15]
QO, KO, QIO, KIO, VO, WIO, NCOL = 0, 1024, 1280, 1792, 1920, 2176, 2184
NEG = -1.0e30
NBIS = 22
import os
NO_INTERLEAVE = bool(int(os.environ.get('NO_INTERLEAVE', '0')))
NO_POOL = bool(int(os.environ.get('NO_POOL', '1')))


def dsa_host_layout(w_in, w_out):
    q = w_in[:, 0:1024].reshape(1024, 16, 64)[:, HP, :].reshape(1024, 1024)
    k = w_in[:, 1024:1280]
    v = w_in[:, 1280:1536]
    qi = w_in[:, 1536:2048]
    ki = w_in[:, 2048:2112]
    wi = w_in[:, 2112:2120]
    wip = np.concatenate([q, k, qi, ki, ki, v, wi], 1)
    wop = w_out.reshape(16, 64, 1024)[HP].reshape(1024, 1024)
    return np.ascontiguousarray(wip, dtype=np.float32), np.ascontiguousarray(wop, dtype=np.float32)


def dsa_consts():
    r = np.arange(128)
    negm = np.where(r[None, :] > r[:, None], NEG, 0.0).astype(np.float32)
    return negm


def dsa_phase(P, src, dst, w_in_d, w_out_d, negm_d, gin_row, gout_row):
    sch, ar, S = P.sch, P.ar, P.S
    NT = S // 128
    KSEL = min(256, S // 4)
    P.phase_begin()
    w_in = ar.alloc(8 * NCOL, BF16)
    w_out = ar.alloc(8 * D, BF16)
    wiv = w_in.ap.rearrange("p (c e) -> p c e", e=NCOL)
    wsrc = w_in_d.rearrange("(c p) e -> p c e", p=128)
    for c0 in range(0, NCOL, 512):
        c1 = min(NCOL, c0 + 512)
        sch.dma("pool", wiv[:, :, c0:c1], wsrc[:, :, c0:c1], writes=[w_in.b])
    sch.dma("pool", w_out.ap.rearrange("p (c e) -> p c e", e=D), w_out_d.rearrange("(c p) e -> p c e", p=128), writes=[w_out.b])
    gin = ar.alloc(D); gout = ar.alloc(D); negm = ar.alloc(128)
    sch.dma("sp", gin.ap, gin_row.partition_broadcast(128), writes=[gin.b])
    sch.dma("sp", gout.ap, gout_row.partition_broadcast(128), writes=[gout.b])
    sch.dma("sp", negm.ap, negm_d, writes=[negm.b])
    cosT = ar.alloc(NT * 32); sinT = ar.alloc(NT * 32)
    ar.mark()
    tb = T(None)
    posi = ar.alloc(NT, I32); posf = ar.alloc(NT); fi_ = ar.alloc(32, I32); invf = ar.alloc(32)
    ang = ar.alloc(NT * 32); angi = ar.alloc(NT * 32, I32); angf = ar.alloc(NT * 32); m1 = ar.alloc(NT * 32)
    rw = dict(reads=[tb.b], writes=[tb.b])
    sch.op("pool", "iota", dict(out=posi.ap, pattern=[[128, NT]], base=0, channel_multiplier=1), **rw)
    sch.op("pool", "iota", dict(out=fi_.ap, pattern=[[1, 32]], base=0, channel_multiplier=0), **rw)
    dve = lambda meth, **kw: sch.op("dve", meth, kw, **rw)
    dve("tensor_copy", out=posf.ap, in_=posi.ap)
    dve("tensor_copy", out=invf.ap, in_=fi_.ap)
    sch.op("act", "activation", dict(out=invf.ap, in_=invf.ap, func=AF.Exp, scale=-math.log(10000.0) / 32.0), **rw)
    dve("tensor_scalar", out=invf.ap, in0=invf.ap, scalar1=1.0 / (2 * math.pi), scalar2=None, op0=ALU.mult)
    a3 = lambda t: t.ap.rearrange("p (q i) -> p q i", i=32)
    dve("tensor_tensor", out=a3(ang), in0=posf.ap.unsqueeze(2).broadcast_to([128, NT, 32]),
        in1=invf.ap.unsqueeze(1).broadcast_to([128, NT, 32]), op=ALU.mult)
    dve("tensor_copy", out=angi.ap, in_=ang.ap)
    dve("tensor_copy", out=angf.ap, in_=angi.ap)
    dve("tensor_tensor", out=ang.ap, in0=ang.ap, in1=angf.ap, op=ALU.subtract)
    def wrap():
        dve("tensor_scalar", out=m1.ap, in0=ang.ap, scalar1=0.5, scalar2=None, op0=ALU.is_gt)
        dve("tensor_tensor", out=ang.ap, in0=ang.ap, in1=m1.ap, op=ALU.subtract)
        dve("tensor_scalar", out=m1.ap, in0=ang.ap, scalar1=-0.5, scalar2=None, op0=ALU.is_lt)
        dve("tensor_tensor", out=ang.ap, in0=ang.ap, in1=m1.ap, op=ALU.add)
    wrap()
    sch.op("act", "activation", dict(out=sinT.ap, in_=ang.ap, func=AF.Sin, scale=TWO_PI_S), reads=[tb.b], writes=[tb.b, sinT.b])
    dve("tensor_scalar", out=ang.ap, in0=ang.ap, scalar1=0.25, scalar2=None, op0=ALU.add)
    wrap()
    sch.op("act", "activation", dict(out=cosT.ap, in_=ang.ap, func=AF.Sin, scale=TWO_PI_S), reads=[tb.b], writes=[tb.b, cosT.b])
    sch.barrier()
    ar.release()
    cos3 = cosT.ap.rearrange("p (q i) -> p q i", i=32)
    sin3 = sinT.ap.rearrange("p (q i) -> p q i", i=32)

    kT = ar.alloc(2 * S, BF16)
    kTv = kT.ap.rearrange("p (b s) -> p b s", s=S)
    kiT = ar.alloc(S, BF16)
    V = ar.alloc(NT * 4 * 65, BF16)
    Vv = V.ap.rearrange("p (t n e) -> p t n e", n=4, e=65)
    kT_b = [Buf() for _ in range(NT)]
    kiT_b = [Buf() for _ in range(NT)]
    V_b = [Buf() for _ in range(NT)]
    sch.op("pool", "memset", dict(ap=V.ap, constant=1.0), writes=V_b)
    acc = ar.alloc(S)
    mask = ar.alloc(S, BF16)
    maskT = [ar.alloc(S, BF16) for _ in range(2)]
    hq = [ar.alloc(D) for _ in range(2)]
    qT = [ar.alloc(8 * 128, BF16) for _ in range(2)]
    hn = ar.alloc(D, BF16)
    hnT = ar.alloc(8 * 128, BF16)
    rp = ar.alloc(1920, BF16)
    tr = [ar.alloc(256) for _ in range(4)]
    wsb = ar.alloc(8); absw = ar.alloc(8); sgnw = ar.alloc(8)
    qiT = ar.alloc(4 * 128, BF16)
    rr = [ar.alloc(512) for _ in range(2)]
    pT = [ar.alloc(512, BF16) for _ in range(4)]
    osb = ar.alloc(D, BF16)
    oT = ar.alloc(8 * 128, BF16)
    yt = ar.alloc(D)
    junk = ar.alloc(D, BF16)
    junk2 = ar.alloc(512, BF16)
    ss = ar.alloc(1); rs = ar.alloc(1); rs2 = ar.alloc(2)
    lo = ar.alloc(1); hi = ar.alloc(1); mid = ar.alloc(1); cnt = ar.alloc(1); ge = ar.alloc(1); dd = ar.alloc(1)
    rden = ar.alloc(4)
    wt = ar.alloc(NBIS + 2); pow2 = ar.alloc(NBIS + 2); cntB = ar.alloc(1)
    junkA = ar.alloc(S - (int(S * 0.42) // 64) * 64 + 64, BF16)
    for k in range(NBIS + 2):
        sch.op("pool", "memset", dict(ap=pow2.ap[:, k:k + 1], constant=2.0 ** (-k)), writes=[pow2.b])
    pqk = P.psum[0:2]
    ppv = P.psum[2:4]
    pout = P.psum[0:2]
    pf = P.psum[4:7]
    ptf = P.psum[7]

    def rstd_ln(out, ssq):
        sch.op("act", "activation", dict(out=out.ap, in_=ssq.ap, func=AF.Ln, bias=P.eps.ap, scale=1.0 / D), reads=[ssq.b, P.eps.b], writes=[out.b])
        sch.op("act", "activation", dict(out=out.ap, in_=out.ap, func=AF.Exp, scale=-0.5), reads=[out.b], writes=[out.b])

    def front(qt):
        r0 = qt * 128
        L = (qt + 1) * 128
        h = hq[qt % 2]
        qTq = qT[qt % 2]
        mT = maskT[qt % 2]
        sch.dma("sp", h.ap, src[r0:r0 + 128, :], writes=[h.b])
        sch.op("act", "activation", dict(out=junk.ap, in_=h.ap, func=AF.Square, accum_out=ss.ap), reads=[h.b], writes=[junk.b, ss.b])
        rstd_ln(rs, ss)
        sch.op("dve", "scalar_tensor_tensor", dict(out=hn.ap, in0=h.ap, scalar=rs.ap, in1=gin.ap, op0=ALU.mult, op1=ALU.mult),
               reads=[h.b, rs.b, gin.b], writes=[hn.b])
        yield
        ptv = ptf.ap.bitcast(BF16)
        for kc2 in range(2):
            for q in range(4):
                kc = kc2 * 4 + q
                sch.op("pe", "transpose", dict(out=ptv[:, q * 128:(q + 1) * 128], in_=hn.ap[:, kc * 128:(kc + 1) * 128], identity=P.ident_bf.ap),
                       reads=[hn.b, P.ident_bf.b], writes=[ptf.b], inc=(q == 3))
            sch.op("act", "copy", dict(out=hnT.ap[:, kc2 * 512:(kc2 + 1) * 512], in_=ptv[:, 0:512]), reads=[ptf.b], writes=[hnT.b])
            yield
        cq = cos3[:, qt, :]
        sq = sin3[:, qt, :]
        for nb in range(5):
            pjb = pf[nb % 3]
            c0 = nb * 512
            c1 = min(NCOL, c0 + 512)
            for kc in range(8):
                sch.op("pe", "matmul", dict(out=pjb.ap[:, 0:c1 - c0], lhsT=hnT.ap[:, kc * 128:(kc + 1) * 128], rhs=wiv[:, kc, c0:c1],
                                            start=(kc == 0), stop=(kc == 7)),
                       reads=[hnT.b, w_in.b], writes=[pjb.b], inc=(kc == 7))
            yield
            if nb < 4:
                nh = 8 if nb < 3 else 6
                x = pjb.ap[:, 0:nh * 64].rearrange("p (h t i) -> p h t i", t=2, i=32)
                o = rp.ap[:, nb * 512:nb * 512 + nh * 64].rearrange("p (h t i) -> p h t i", t=2, i=32)
                cb = cq.unsqueeze(1).broadcast_to([128, nh, 32])
                sb_ = sq.unsqueeze(1).broadcast_to([128, nh, 32])
                tv = [t_.ap[:, 0:nh * 32].rearrange("p (h i) -> p h i", i=32) for t_ in tr]
                R_ = [pjb.b, cosT.b, sinT.b]
                sch.op("dve", "tensor_tensor", dict(out=tv[0], in0=x[:, :, 0, :], in1=cb, op=ALU.mult), reads=R_, writes=[tr[0].b])
                sch.op("dve", "tensor_tensor", dict(out=tv[1], in0=x[:, :, 1, :], in1=sb_, op=ALU.mult), reads=R_, writes=[tr[1].b])
                sch.op("dve", "tensor_tensor", dict(out=tv[2], in0=x[:, :, 1, :], in1=cb, op=ALU.mult), reads=R_, writes=[tr[2].b])
                sch.op("dve", "tensor_tensor", dict(out=tv[3], in0=x[:, :, 0, :], in1=sb_, op=ALU.mult), reads=R_, writes=[tr[3].b])
                sch.op("pool", "tensor_tensor", dict(out=o[:, :, 0, :], in0=tv[0], in1=tv[1], op=ALU.subtract),
                       reads=[tr[0].b, tr[1].b], writes=[rp.b])
                sch.op("pool", "tensor_tensor", dict(out=o[:, :, 1, :], in0=tv[2], in1=tv[3], op=ALU.add),
                       reads=[tr[2].b, tr[3].b], writes=[rp.b])
            if nb == 3:
                sch.op("act", "copy", dict(out=Vv[:, qt, 0:2, 0:64], in_=pjb.ap[:, 384:512].rearrange("p (n e) -> p n e", e=64)),
                       reads=[pjb.b], writes=[V_b[qt]])
            if nb == 4:
                sch.op("act", "copy", dict(out=Vv[:, qt, 2:4, 0:64], in_=pjb.ap[:, 0:128].rearrange("p (n e) -> p n e", e=64)),
                       reads=[pjb.b], writes=[V_b[qt]])
                sch.op("dve", "tensor_copy", dict(out=wsb.ap, in_=pjb.ap[:, 128:136]), reads=[pjb.b], writes=[wsb.b])
            yield
        sch.op("dve", "tensor_scalar", dict(out=sgnw.ap, in0=wsb.ap, scalar1=0.0, scalar2=None, op0=ALU.is_ge), reads=[wsb.b], writes=[sgnw.b])
        sch.op("dve", "tensor_scalar", dict(out=sgnw.ap, in0=sgnw.ap, scalar1=2.0, scalar2=-1.0, op0=ALU.mult, op1=ALU.add), reads=[sgnw.b], writes=[sgnw.b])
        sch.op("dve", "tensor_tensor", dict(out=absw.ap, in0=wsb.ap, in1=sgnw.ap, op=ALU.mult), reads=[wsb.b, sgnw.b], writes=[absw.b])
        qiv = rp.ap[:, QIO:QIO + 512].rearrange("p (h e) -> p h e", e=64)
        sch.op("dve", "tensor_tensor", dict(out=qiv, in0=qiv, in1=absw.ap.unsqueeze(2).broadcast_to([128, 8, 64]), op=ALU.mult),
               reads=[rp.b, absw.b], writes=[rp.b])
        yield

        def tr4(cols):
            for q, co in enumerate(cols):
                sch.op("pe", "transpose", dict(out=ptv[:, q * 128:(q + 1) * 128], in_=rp.ap[:, co:co + 128], identity=P.ident_bf.ap),
                       reads=[rp.b, P.ident_bf.b], writes=[ptf.b], inc=(q == len(cols) - 1))
        for i0 in (0, 4):
            tr4([QO + (i0 + b) * 128 for b in range(4)])
            sch.op("act", "copy", dict(out=qTq.ap[:, i0 * 128:(i0 + 4) * 128], in_=ptv[:, 0:512]), reads=[ptf.b], writes=[qTq.b])
            yield
        tr4([QIO + b * 128 for b in range(4)])
        sch.op("act", "copy", dict(out=qiT.ap[:, 0:512], in_=ptv[:, 0:512]), reads=[ptf.b], writes=[qiT.b])
        yield
        tr4([KO, KO + 128, KIO])
        sch.op("dve", "tensor_copy", dict(out=kTv[:, :, r0:r0 + 128], in_=ptv[:, 0:256].rearrange("p (b s) -> p b s", s=128)),
               reads=[ptf.b], writes=[kT_b[qt]])
        sch.op("dve", "tensor_copy", dict(out=kiT.ap[:, r0:r0 + 128], in_=ptv[:, 256:384]), reads=[ptf.b], writes=[kiT_b[qt]])
        yield
        nch = (L + 511) // 512
        ci = 0
        for c in range(nch):
            s0 = c * 512
            wdt = min(512, L - s0)
            kb_ = kiT_b[4 * c:min(NT, 4 * c + 4)]
            for hh in range(8):
                pb = pf[ci % 3]
                ci += 1
                half = (hh % 2) * 64
                sch.op("pe", "matmul", dict(out=pb.ap[:, 0:wdt], lhsT=qiT.ap[half:half + 64, (hh // 2) * 128:(hh // 2 + 1) * 128],
                                            rhs=kiT.ap[half:half + 64, s0:s0 + wdt], start=True, stop=True),
                       reads=[qiT.b] + kb_, writes=[pb.b])
                r = rr[hh % 2]
                sch.op("act", "activation", dict(out=r.ap[:, 0:wdt], in_=pb.ap[:, 0:wdt], func=AF.Relu), reads=[pb.b], writes=[r.b])
                if hh == 0:
                    sch.op("dve", "tensor_scalar", dict(out=acc.ap[:, s0:s0 + wdt], in0=r.ap[:, 0:wdt], scalar1=sgnw.ap[:, 0:1], scalar2=None, op0=ALU.mult),
                           reads=[r.b, sgnw.b], writes=[acc.b])
                else:
                    sch.op("dve", "scalar_tensor_tensor", dict(out=acc.ap[:, s0:s0 + wdt], in0=r.ap[:, 0:wdt], scalar=sgnw.ap[:, hh:hh + 1],
                                                               in1=acc.ap[:, s0:s0 + wdt], op0=ALU.mult, op1=ALU.add),
                           reads=[r.b, sgnw.b, acc.b], writes=[acc.b])
                if hh % 2 == 1:
                    yield
        if L > KSEL:
            sch.op("dve", "tensor_reduce", dict(out=lo.ap, in_=acc.ap[:, 0:L], axis=AX.X, op=ALU.min), reads=[acc.b], writes=[lo.b])
        sch.op("dve", "tensor_tensor", dict(out=acc.ap[:, r0:L], in0=acc.ap[:, r0:L], in1=negm.ap, op=ALU.add), reads=[acc.b, negm.b], writes=[acc.b])
        if L > KSEL:
            sch.op("dve", "tensor_reduce", dict(out=hi.ap, in_=acc.ap[:, 0:L], axis=AX.X, op=ALU.max), reads=[acc.b], writes=[hi.b])
            bb = [lo.b, hi.b, mid.b, cnt.b, ge.b, dd.b, wt.b]
            sch.op("dve", "tensor_tensor", dict(out=dd.ap, in0=hi.ap, in1=lo.ap, op=ALU.subtract), reads=bb, writes=[dd.b])
            sch.op("dve", "tensor_scalar", dict(out=wt.ap, in0=pow2.ap, scalar1=dd.ap, scalar2=None, op0=ALU.mult), reads=bb + [pow2.b], writes=[wt.b])
            sch.op("dve", "tensor_tensor", dict(out=mid.ap, in0=lo.ap, in1=wt.ap[:, 1:2], op=ALU.add), reads=bb, writes=[mid.b])
            yield
            LA = (int(L * 0.42) // 64) * 64
            nB = L - LA
            for it in range(NBIS):
                sch.op("act", "activation", dict(out=junkA.ap[:, 0:nB], in_=acc.ap[:, LA:L], func=AF.Sign, bias=mid.ap, scale=-1.0,
                                                 accum_out=cntB.ap), reads=[acc.b, mid.b], writes=[junkA.b, cntB.b])
                sch.op("dve", "tensor_scalar", dict(out=mask.ap[:, 0:LA], in0=acc.ap[:, 0:LA], scalar1=mid.ap, scalar2=None, op0=ALU.is_ge, op1=ALU.add,
                                                    accum_out=cnt.ap), reads=[acc.b, mid.b], writes=[mask.b, cnt.b])
                sch.op("dve", "scalar_tensor_tensor", dict(out=ge.ap, in0=cntB.ap, scalar=-0.5, in1=cnt.ap, op0=ALU.mult, op1=ALU.add),
                       reads=[cntB.b, cnt.b], writes=[ge.b])
                sch.op("dve", "scalar_tensor_tensor", dict(out=ge.ap, in0=ge.ap, scalar=float(KSEL) - 0.5 - nB / 2.0, in1=wt.ap[:, it + 1:it + 2],
                                                           op0=ALU.is_ge, op1=ALU.mult), reads=[ge.b, wt.b], writes=[ge.b])
                sch.op("dve", "scalar_tensor_tensor", dict(out=mid.ap, in0=ge.ap, scalar=wt.ap[:, it + 2:it + 3], in1=mid.ap,
                                                           op0=ALU.subtract, op1=ALU.add), reads=[ge.b, wt.b, mid.b], writes=[mid.b])
                yield
            sch.op("dve", "tensor_tensor", dict(out=lo.ap, in0=mid.ap, in1=wt.ap[:, NBIS + 1:NBIS + 2], op=ALU.subtract), reads=bb, writes=[lo.b])
            sch.op("dve", "tensor_scalar", dict(out=mask.ap[:, 0:L], in0=acc.ap[:, 0:L], scalar1=lo.ap, scalar2=None, op0=ALU.is_ge),
                   reads=[acc.b, lo.b], writes=[mask.b])
        else:
            sch.op("dve", "tensor_scalar", dict(out=mask.ap[:, 0:L], in0=acc.ap[:, 0:L], scalar1=-1.0e29, scalar2=None, op0=ALU.is_ge),
                   reads=[acc.b], writes=[mask.b])
        yield
        for i0 in range(0, qt + 1, 4):
            n = min(4, qt + 1 - i0)
            for q in range(n):
                st = i0 + q
                sch.op("pe", "transpose", dict(out=ptv[:, q * 128:(q + 1) * 128], in_=mask.ap[:, st * 128:(st + 1) * 128], identity=P.ident_bf.ap),
                       reads=[mask.b, P.ident_bf.b], writes=[ptf.b], inc=(q == n - 1))
            sch.op("act", "copy", dict(out=mT.ap[:, i0 * 128:(i0 + n) * 128], in_=ptv[:, 0:n * 128]), reads=[ptf.b], writes=[mT.b])
            yield

    def back(qt):
        r0 = qt * 128
        h = hq[qt % 2]
        qTq = qT[qt % 2]
        mTv = maskT[qt % 2].ap.rearrange("p (t q) -> p t q", q=128)
        mTb = maskT[qt % 2].b
        cnt_p = 0
        for n in range(4):
            half = (n % 2) * 64
            a = n // 2
            pob = ppv[n % 2]
            for st in range(qt + 1):
                pq = pqk[cnt_p % 2]
                p_ = pT[cnt_p % 4]
                sch.op("pe", "matmul", dict(out=pq.ap[:, 0:512], lhsT=kTv[half:half + 64, a, st * 128:(st + 1) * 128],
                                            rhs=qTq.ap[half:half + 64, a * 512:(a + 1) * 512], start=True, stop=True),
                       reads=[kT_b[st], qTq.b], writes=[pq.b])
                sch.op("act", "activation", dict(out=p_.ap, in_=pq.ap[:, 0:512], func=AF.Exp, scale=0.125), reads=[pq.b], writes=[p_.b])
                eng = "pool" if (cnt_p % 3 == 2 and not NO_POOL) else "dve"
                p3 = p_.ap.rearrange("p (g t) -> p g t", t=128)
                sch.op(eng, "tensor_tensor", dict(out=p3, in0=p3, in1=mTv[:, st, :].unsqueeze(1).broadcast_to([128, 4, 128]), op=ALU.mult),
                       reads=[p_.b, mTb], writes=[p_.b])
                for g in range(4):
                    sch.op("pe", "matmul", dict(out=pob.ap[:, g * 65:(g + 1) * 65], lhsT=p_.ap[:, g * 128:(g + 1) * 128], rhs=Vv[:, st, n, :],
                                                start=(st == 0 and g == 0), stop=(st == qt and g == 3)),
                           reads=[p_.b, V_b[st]], writes=[pob.b], inc=(g == 3))
                cnt_p += 1
                yield
            po3 = pob.ap[:, 0:260].rearrange("p (g e) -> p g e", e=65)
            sch.op("dve", "reciprocal", dict(out=rden.ap, in_=po3[:, :, 64]), reads=[pob.b], writes=[rden.b])
            ov = osb.ap[:, a * 512:(a + 1) * 512].rearrange("p (g two e) -> p g two e", two=2, e=64)[:, :, n % 2, :]
            sch.op("dve", "tensor_tensor", dict(out=ov, in0=po3[:, :, 0:64], in1=rden.ap.unsqueeze(2).broadcast_to([128, 4, 64]), op=ALU.mult),
                   reads=[pob.b, rden.b], writes=[osb.b])
        yield
        for kc2 in range(2):
            ptb = ppv[kc2]
            ptv = ptb.ap.bitcast(BF16)
            for q in range(4):
                kc = kc2 * 4 + q
                sch.op("pe", "transpose", dict(out=ptv[:, q * 128:(q + 1) * 128], in_=osb.ap[:, kc * 128:(kc + 1) * 128], identity=P.ident_bf.ap),
                       reads=[osb.b, P.ident_bf.b], writes=[ptb.b], inc=(q == 3))
            sch.op("act", "copy", dict(out=oT.ap[:, kc2 * 512:(kc2 + 1) * 512], in_=ptv[:, 0:512]), reads=[ptb.b], writes=[oT.b])
        yield
        for half in range(2):
            pob = pout[half]
            for kc in range(8):
                sch.op("pe", "matmul", dict(out=pob.ap[:, 0:512], lhsT=oT.ap[:, kc * 128:(kc + 1) * 128],
                                            rhs=w_out.ap[:, kc * D + half * 512:kc * D + (half + 1) * 512], start=(kc == 0), stop=(kc == 7)),
                       reads=[oT.b, w_out.b], writes=[pob.b], inc=(kc == 7))
            sch.op("act", "activation", dict(out=junk2.ap[:, 0:512], in_=pob.ap[:, 0:512], func=AF.Square, accum_out=rs2.ap[:, half:half + 1]),
                   reads=[pob.b], writes=[junk2.b, rs2.b])
            yield
        sch.op("dve", "tensor_tensor", dict(out=rs2.ap[:, 0:1], in0=rs2.ap[:, 0:1], in1=rs2.ap[:, 1:2], op=ALU.add), reads=[rs2.b], writes=[rs2.b])
        r1 = T(rs2.ap[:, 0:1], rs2.b)
        rstd_ln(r1, r1)
        for half in range(2):
            sch.op("dve", "scalar_tensor_tensor", dict(out=yt.ap[:, half * 512:(half + 1) * 512], in0=pout[half].ap[:, 0:512], scalar=rs2.ap[:, 0:1],
                                                       in1=gout.ap[:, half * 512:(half + 1) * 512], op0=ALU.mult, op1=ALU.mult),
                   reads=[pout[half].b, rs2.b, gout.b], writes=[yt.b])
        sch.op("pool", "tensor_tensor", dict(out=yt.ap, in0=yt.ap, in1=h.ap, op=ALU.add), reads=[yt.b, h.b], writes=[yt.b])
        sch.dma("sp", dst[r0:r0 + 128, :], yt.ap, reads=[yt.b])
        yield

    def count_units(gen_fn, qt):
        return None

    def n_front(qt):
        L = (qt + 1) * 128
        nch = (L + 511) // 512
        return 1 + 2 + 10 + 1 + 4 + 4 * nch + (2 + NBIS if L > KSEL else 1) + (qt + 4) // 4
    def n_back(qt):
        return 4 * (qt + 1) + 1 + 1 + 2 + 1

    def run_all(g):
        for _ in g:
            pass

    run_all(front(0))
    for qt in range(NT):
        gb = back(qt)
        if qt + 1 < NT:
            gf = front(qt + 1)
            nb_, nf_ = n_back(qt), n_front(qt + 1)
            ib = i_f = 0
            bdone = fdone = False
            while not (bdone and fdone):
                if not bdone and (fdone or NO_INTERLEAVE or ib * nf_ <= i_f * nb_):
                    try:
                        next(gb); ib += 1
                    except StopIteration:
                        bdone = True
                elif not fdone:
                    try:
                        next(gf); i_f += 1
                    except StopIteration:
                        fdone = True
        else:
            run_all(gb)
    P.phase_end()


SEQ = 4096
DEPTH = 4
S5_NI = 2


def build_program(S=SEQ, depth=DEPTH):
    P = Prog(S)
    x = P.din("x", [S, D])
    out = P.dout("out", [S, D])
    hbuf = P.dscratch("hbuf", [S, D])
    g = P.din("g", [depth * 4, D])
    s5cst = P.din("s5cst", [128, 130])
    negm = P.din("negm", [128, 128])
    for i in range(depth):
        j = i // 2
        src = x if i == 0 else hbuf
        grow = lambda k: g[i * 4 + k:i * 4 + k + 1, :]
        if i % 2 == 0:
            s5_phase(P, src, hbuf, P.din("s5small_%d" % j, [128, 128]), P.din("s5big_%d" % j, [128, 2048]), s5cst,
                     P.din("s5win_%d" % j, [D, E]), P.din("s5wglu_%d" % j, [E, E]), P.din("s5wout_%d" % j, [E, D]),
                     grow(0), grow(1), NI=S5_NI)
        else:
            dsa_phase(P, src, hbuf, P.din("awin_%d" % j, [D, NCOL]), P.din("awout_%d" % j, [D, D]), negm, grow(0), grow(1))
        dst = out if i == depth - 1 else hbuf
        mlp_phase(P, hbuf, dst, P.din("w1_%d" % i, [D, DFF]), P.din("w2_%d" % i, [DFF, D]), grow(2), grow(3))
    P.sch.barrier()
    P.sch.emit()
    return P


_CACHE = {}


def host_inputs(inputs, depth=DEPTH):
    f = lambda a: np.ascontiguousarray(np.asarray(a), dtype=np.float32)
    shared = {"ident": np.eye(128, dtype=np.float32), "s5cst": s5_consts(), "negm": dsa_consts(),
              "g": f(inputs["norm_g"]).reshape(-1, D)[:depth * 4]}
    for i in range(depth):
        j = i // 2
        shared["w1_%d" % i] = f(inputs["mlp_w1"][i])
        shared["w2_%d" % i] = f(inputs["mlp_w2"][i])
        if i % 2 == 0:
            small, big = s5_host_layout(*[np.asarray(inputs["ssm_" + n][j]) for n in
                                          ["lam_re", "lam_im", "log_dt", "b_re", "b_im", "c_re", "c_im", "d"]])
            shared["s5small_%d" % j] = small
            shared["s5big_%d" % j] = big
            shared["s5win_%d" % j] = f(inputs["ssm_w_in"][j])
            shared["s5wglu_%d" % j] = f(inputs["ssm_w_glu"][j])
            shared["s5wout_%d" % j] = f(inputs["ssm_w_out"][j])
        else:
            wip, wop = dsa_host_layout(np.asarray(inputs["att_w_in"][j]), np.asarray(inputs["att_w_out"][j]))
            shared["awin_%d" % j] = wip
            shared["awout_%d" % j] = wop
    return shared


def kernel(**inputs):
    x = np.asarray(inputs["x"], dtype=np.float32)
    B, S, _ = x.shape
    key = (S, DEPTH)
    if key not in _CACHE:
        _CACHE[key] = build_program(S, DEPTH)
    P = _CACHE[key]
    shared = host_inputs(inputs)
    in_maps = []
    for b in range(B):
        m = dict(shared)
        m["x"] = np.ascontiguousarray(x[b])
        in_maps.append(m)
    res = run_bass_kernel_spmd(P.nc, in_maps, core_ids=list(range(B)))
    return np.stack([np.asarray(r["out"]) for r in res.results], 0).astype(np.float32)
```

```python
import math
import numpy as np
from contextlib import ExitStack
import concourse.bass as bass
import concourse.mybir as mybir
from concourse.bass_utils import run_bass_kernel_spmd

F32 = mybir.dt.float32
BF16 = mybir.dt.bfloat16
I32 = mybir.dt.int32
ALU = mybir.AluOpType
AF = mybir.ActivationFunctionType
AX = mybir.AxisListType

D = 1024
DFF = 4096
EPS = 1e-6
NCORES = 8


class Buf:
    __slots__ = ("w", "r")

    def __init__(self):
        self.w = None
        self.r = {}


class T:
    __slots__ = ("ap", "b")

    def __init__(self, ap, b=None):
        self.ap = ap
        self.b = b if b is not None else Buf()


class Sched:
    ENGS = ("pe", "dve", "act", "pool", "sp")
    QUEUES = ("sp", "pool", "act")

    def __init__(self, nc, st, nslots=12):
        self.nc = nc
        self.sem = {e: st.enter_context(nc.semaphore("c_" + e)) for e in self.ENGS}
        self.cnt = {e: 0 for e in self.ENGS}
        self.seen = {e: {} for e in self.ENGS}
        self.ops = {e: [] for e in self.ENGS}
        self.slots = {}
        self.slot_i = {}
        for q in self.QUEUES:
            self.slots[q] = []
            self.slot_i[q] = 0
            for i in range(nslots):
                key = "d_%s%d" % (q, i)
                self.sem[key] = st.enter_context(nc.semaphore(key))
                self.slots[q].append([key, 0])

    def _deps(self, reads, writes):
        deps = {}

        def add(k, v):
            if deps.get(k, 0) < v:
                deps[k] = v

        for b in reads:
            if b.w is not None:
                add(*b.w)
        for b in writes:
            if b.w is not None:
                add(*b.w)
            for k, v in b.r.items():
                add(k, v)
        return deps

    def _waits(self, eng, deps):
        waits = []
        for k, v in deps.items():
            if k == eng and eng == "pe":
                continue
            if self.seen[eng].get(k, 0) < v:
                self.seen[eng][k] = v
                waits.append((k, v))
        return waits

    def op(self, eng, meth, kw, reads=(), writes=(), inc=True):
        fn = (meth, kw)
        waits = self._waits(eng, self._deps(reads, writes))
        if inc:
            self.cnt[eng] += 1
            tv = self.cnt[eng]
        else:
            tv = self.cnt[eng] + 1
        self.ops[eng].append((waits, fn, eng if inc else None, 1))
        for b in reads:
            if b.r.get(eng, 0) < tv:
                b.r[eng] = tv
        for b in writes:
            b.w = (eng, tv)
            b.r = {}

    def dma(self, q, out, in_, reads=(), writes=(), **kw):
        slot = self.slots[q][self.slot_i[q]]
        self.slot_i[q] = (self.slot_i[q] + 1) % len(self.slots[q])
        deps = self._deps(reads, writes)
        if slot[1] > 0 and deps.get(slot[0], 0) < slot[1]:
            deps[slot[0]] = slot[1]
        waits = self._waits(q, deps)
        slot[1] += 16
        tv = slot[1]
        key = slot[0]
        kw = dict(kw)
        kw["out"] = out
        kw["in_"] = in_
        self.ops[q].append((waits, ("dma_start", kw), key, 16))
        for b in reads:
            if b.r.get(key, 0) < tv:
                b.r[key] = tv
        for b in writes:
            b.w = (key, tv)
            b.r = {}

    def barrier(self):
        allv = {e: self.cnt[e] for e in self.ENGS}
        for q in self.QUEUES:
            for key, v in self.slots[q]:
                if v > 0:
                    allv[key] = v
        for e in self.ENGS:
            waits = []
            for k, v in allv.items():
                if v > 0 and self.seen[e].get(k, 0) < v:
                    self.seen[e][k] = v
                    waits.append((k, v))
            if waits:
                self.ops[e].append((waits, None, None, 0))

    def emit(self):
        names = {"pe": "tensor", "dve": "vector", "act": "scalar", "pool": "gpsimd", "sp": "sync"}
        with self.nc.Block() as block:
            for e in self.ENGS:
                ops = self.ops[e]

                def body(eng, ops=ops):
                    for waits, fn, inckey, incv in ops:
                        for k, v in waits:
                            eng.wait_ge(self.sem[k], v)
                        if fn is None:
                            continue
                        ins = getattr(eng, fn[0])(**fn[1])
                        if inckey is not None:
                            ins.then_inc(self.sem[inckey], incv)

                getattr(block, names[e])(body)


class Arena:
    def __init__(self, base_ap, nbytes):
        self.base = base_ap
        self.nbytes = nbytes
        self.off = 0
        self.marks = []

    def alloc(self, cols, dtype=F32, parts=128):
        esz = mybir.dt.size(dtype)
        nb = (cols * esz + 31) // 32 * 32
        assert self.off + nb <= self.nbytes, "arena overflow %d + %d > %d" % (self.off, nb, self.nbytes)
        v = self.base[0:parts, self.off // 4:(self.off + nb) // 4]
        if dtype != F32:
            v = v.bitcast(dtype)
        v = v[:, 0:cols]
        self.off += nb
        return T(v)

    def mark(self):
        self.marks.append(self.off)

    def release(self):
        self.off = self.marks.pop()


class Prog:
    def __init__(self, S, arena_bytes=200 * 1024):
        self.S = S
        self.st = ExitStack()
        st = self.st
        nc = bass.Bass("TRN2", target_bir_lowering=False)
        self.nc = nc
        self.dram = {}
        self.sch = Sched(nc, st)
        base = st.enter_context(nc.sbuf_tensor("arena", [128, arena_bytes // 4], F32))
        self.ar = Arena(base[:, :], arena_bytes)
        self.psum = []
        for i in range(8):
            t = st.enter_context(nc.psum_tensor("ps%d" % i, [128, 512], F32))
            self.psum.append(T(t[:, :]))
        self.ident_bf = self.ar.alloc(128, BF16)
        self.ident_f = self.ar.alloc(128, F32)
        self.eps = self.ar.alloc(1, F32)
        idd = self.din("ident", [128, 128])
        self.sch.dma("sp", self.ident_f.ap, idd, writes=[self.ident_f.b])
        self.sch.op("dve", "tensor_copy", dict(out=self.ident_bf.ap, in_=self.ident_f.ap),
                    reads=[self.ident_f.b], writes=[self.ident_bf.b])
        self.sch.op("dve", "memset", dict(ap=self.eps.ap, constant=EPS), writes=[self.eps.b])

    def din(self, name, shape, dtype=F32):
        t = self.nc.dram_tensor(name, list(shape), dtype, kind="ExternalInput").ap()
        self.dram[name] = t
        return t

    def dout(self, name, shape, dtype=F32):
        t = self.nc.dram_tensor(name, list(shape), dtype, kind="ExternalOutput").ap()
        self.dram[name] = t
        return t

    def dscratch(self, name, shape, dtype=F32):
        t = self.nc.dram_tensor(name, list(shape), dtype, kind="Internal").ap()
        self.dram[name] = t
        return t

    def phase_begin(self):
        self.ar.mark()

    def phase_end(self):
        self.sch.barrier()
        self.ar.release()

    def rstd(self, out, ss, n):
        sch = self.sch
        P = ss.ap.shape[0]
        sch.op("act", "activation", dict(out=out.ap, in_=ss.ap, func=AF.Sqrt, bias=self.eps.ap[0:P, :], scale=1.0 / n),
               reads=[ss.b, self.eps.b], writes=[out.b])
        sch.op("dve", "reciprocal", dict(out=out.ap, in_=out.ap), reads=[out.b], writes=[out.b])


def mlp_phase(P, src, dst, w1d, w2d, gin_row, gout_row):
    sch, ar, S = P.sch, P.ar, P.S
    TT = 256
    P.phase_begin()
    w1 = [ar.alloc(8 * 512, BF16) for _ in range(8)]
    w2 = [ar.alloc(4 * 1024, BF16) for _ in range(8)]
    w1src = w1d.rearrange("(c p) f -> p c f", p=128)
    w2src = w2d.rearrange("(c p) d -> p c d", p=128)
    for i in range(8):
        sch.dma("pool", w1[i].ap.rearrange("p (c f) -> p c f", f=512), w1src[:, :, i * 512:(i + 1) * 512],
                writes=[w1[i].b])
    for i in range(8):
        sch.dma("pool", w2[i].ap.rearrange("p (c d) -> p c d", d=1024), w2src[:, 4 * i:4 * i + 4, :],
                writes=[w2[i].b])
    gin = ar.alloc(D)
    gout = ar.alloc(D)
    sch.dma("sp", gin.ap, gin_row.partition_broadcast(128), writes=[gin.b])
    sch.dma("sp", gout.ap, gout_row.partition_broadcast(128), writes=[gout.b])
    NB = 2
    hs = [[ar.alloc(D) for _ in range(2)] for _ in range(NB)]
    hn = [ar.alloc(D, BF16) for _ in range(2)]
    hnT = [ar.alloc(8 * TT, BF16) for _ in range(NB)]
    ss = [ar.alloc(2) for _ in range(NB)]
    rs = [ar.alloc(2) for _ in range(NB)]
    rs2 = [ar.alloc(2) for _ in range(NB)]
    junk = ar.alloc(D, BF16)
    rr = [ar.alloc(TT) for _ in range(2)]
    aT = [ar.alloc(TT, BF16) for _ in range(2)]
    yt = ar.alloc(D)
    acc = P.psum[0:4]
    pm = P.psum[4:6]
    pt = P.psum[6:8]
    ntile = S // TT

    def load_tile(t):
        for sub in range(2):
            r0 = t * TT + sub * 128
            sch.dma("sp", hs[t % NB][sub].ap, src[r0:r0 + 128, :], writes=[hs[t % NB][sub].b])

    load_tile(0)
    for t in range(ntile):
        pb = t % NB
        h = hs[pb]
        for sub in range(2):
            sch.op("act", "activation", dict(out=junk.ap, in_=h[sub].ap, func=AF.Square,
                                             accum_out=ss[pb].ap[:, sub:sub + 1]),
                   reads=[h[sub].b], writes=[junk.b, ss[pb].b])
        P.rstd(rs[pb], ss[pb], D)
        for sub in range(2):
            sch.op("dve", "scalar_tensor_tensor", dict(out=hn[sub].ap, in0=h[sub].ap, scalar=rs[pb].ap[:, sub:sub + 1],
                                                       in1=gin.ap, op0=ALU.mult, op1=ALU.mult),
                   reads=[h[sub].b, rs[pb].b, gin.b], writes=[hn[sub].b])
        for kc in range(8):
            ptb = pt[kc % 2]
            ptv = ptb.ap.bitcast(BF16)
            for sub in range(2):
                sch.op("pe", "transpose", dict(out=ptv[:, sub * 128:(sub + 1) * 128],
                                               in_=hn[sub].ap[:, kc * 128:(kc + 1) * 128], identity=P.ident_bf.ap),
                       reads=[hn[sub].b, P.ident_bf.b], writes=[ptb.b], inc=(sub == 1))
            if kc % 2 == 0:
                sch.op("act", "copy", dict(out=hnT[pb].ap[:, kc * TT:(kc + 1) * TT], in_=ptv[:, 0:TT]),
                       reads=[ptb.b], writes=[hnT[pb].b])
            else:
                sch.op("dve", "tensor_copy", dict(out=hnT[pb].ap[:, kc * TT:(kc + 1) * TT], in_=ptv[:, 0:TT]),
                       reads=[ptb.b], writes=[hnT[pb].b])
        if t + 1 < ntile:
            load_tile(t + 1)

        def mm1(f):
            pmb = pm[f % 2]
            wt = w1[f // 4]
            c0 = (f % 4) * 128
            for kc in range(8):
                sch.op("pe", "matmul", dict(out=pmb.ap[:, 0:TT], lhsT=wt.ap[:, kc * 512 + c0:kc * 512 + c0 + 128],
                                            rhs=hnT[pb].ap[:, kc * TT:(kc + 1) * TT], start=(kc == 0), stop=(kc == 7)),
                       reads=[wt.b, hnT[pb].b], writes=[pmb.b], inc=(kc == 7))
            sch.op("act", "activation", dict(out=rr[f % 2].ap, in_=pmb.ap[:, 0:TT], func=AF.Relu),
                   reads=[pmb.b], writes=[rr[f % 2].b])
            sch.op("dve", "tensor_tensor", dict(out=aT[f % 2].ap, in0=rr[f % 2].ap, in1=rr[f % 2].ap, op=ALU.mult),
                   reads=[rr[f % 2].b], writes=[aT[f % 2].b])

        def mm2(f):
            wt = w2[f // 4]
            o = (f % 4) * 1024
            for sub in range(2):
                for half in range(2):
                    a = acc[sub * 2 + half]
                    sch.op("pe", "matmul", dict(out=a.ap[:, 0:512], lhsT=aT[f % 2].ap[:, sub * 128:(sub + 1) * 128],
                                                rhs=wt.ap[:, o + half * 512:o + (half + 1) * 512],
                                                start=(f == 0), stop=(f == 31)),
                           reads=[aT[f % 2].b, wt.b], writes=[a.b], inc=(f == 31 or (sub == 1 and half == 1)))

        mm1(0)
        for f in range(32):
            if f + 1 < 32:
                mm1(f + 1)
            mm2(f)
        for sub in range(2):
            for half in range(2):
                a = acc[sub * 2 + half]
                sch.op("act", "activation", dict(out=junk.ap[:, 0:512], in_=a.ap[:, 0:512], func=AF.Square,
                                                 accum_out=rs2[pb].ap[:, half:half + 1]),
                       reads=[a.b], writes=[junk.b, rs2[pb].b])
            sch.op("dve", "tensor_tensor", dict(out=rs2[pb].ap[:, 0:1], in0=rs2[pb].ap[:, 0:1],
                                                in1=rs2[pb].ap[:, 1:2], op=ALU.add),
                   reads=[rs2[pb].b], writes=[rs2[pb].b])
            r1 = T(rs2[pb].ap[:, 0:1], rs2[pb].b)
            P.rstd(r1, r1, D)
            for half in range(2):
                a = acc[sub * 2 + half]
                sch.op("dve", "scalar_tensor_tensor", dict(
                    out=yt.ap[:, half * 512:(half + 1) * 512], in0=a.ap[:, 0:512], scalar=rs2[pb].ap[:, 0:1],
                    in1=gout.ap[:, half * 512:(half + 1) * 512], op0=ALU.mult, op1=ALU.mult),
                    reads=[a.b, rs2[pb].b, gout.b], writes=[yt.b])
            sch.op("pool", "tensor_tensor", dict(out=h[sub].ap, in0=yt.ap, in1=h[sub].ap, op=ALU.add),
                   reads=[yt.b, h[sub].b], writes=[h[sub].b])
            r0 = t * TT + sub * 128
            sch.dma("sp", dst[r0:r0 + 128, :], h[sub].ap, reads=[h[sub].b])
    P.phase_end()


G, PST, CH, E = 32, 64, 16, 512
TWO_PI_S = 6.2831845


def s5_host_layout(lam_re, lam_im, log_dt, b_re, b_im, c_re, c_im, d_skip):
    lr = np.concatenate([lam_re.T, lam_re.T], 0)
    li = np.concatenate([lam_im.T, lam_im.T], 0)
    ld = np.broadcast_to(log_dt[None, :], (128, G))
    bre = b_re.transpose(1, 0, 2)
    bim = b_im.transpose(1, 0, 2)
    cre = c_re.transpose(2, 0, 1)
    cim = c_im.transpose(2, 0, 1)
    B1 = np.concatenate([bre, bim], 0).reshape(128, G * CH)
    B2 = np.concatenate([bim, bre], 0).reshape(128, G * CH)
    C1 = np.concatenate([cre, cim], 0).reshape(128, G * CH)
    C2 = np.concatenate([cim, cre], 0).reshape(128, G * CH)
    dcol = np.tile(d_skip.T, (8, 1))
    small = np.concatenate([lr, li, ld, dcol], 1)
    big = np.concatenate([B1, B2, C1, C2], 1)
    return np.ascontiguousarray(small, dtype=np.float32), np.ascontiguousarray(big, dtype=np.float32)


def s5_consts():
    sg = np.ones((128, 2), np.float32)
    sg[64:, 0] = -1.0
    sg[:64, 1] = -1.0
    j = np.arange(128) // 16
    mask = (j[None, :] >= j[:, None]).astype(np.float32)
    return np.concatenate([sg, mask], 1)


def s5_phase(P, src, dst, small_d, big_d, cst_d, w_in_d, w_glu_d, w_out_d, gin_row, gout_row, NI=2):
    sch, ar, S = P.sch, P.ar, P.S
    KP = S // 8
    NM = KP // NI
    NKB = KP // 128
    P.phase_begin()
    small = ar.alloc(128)
    big = ar.alloc(2048)
    cst = ar.alloc(130)
    sch.dma("sp", small.ap, small_d, writes=[small.b])
    sch.dma("sp", big.ap, big_d, writes=[big.b])
    sch.dma("sp", cst.ap, cst_d, writes=[cst.b])
    gin = ar.alloc(D)
    gout = ar.alloc(D)
    sch.dma("sp", gin.ap, gin_row.partition_broadcast(128), writes=[gin.b])
    sch.dma("sp", gout.ap, gout_row.partition_broadcast(128), writes=[gout.b])
    sg = cst.ap[:, 0:1]
    nsg = cst.ap[:, 1:2]
    cmask = cst.ap[:, 2:130]
    lr = small.ap[:, 0:32]
    li = small.ap[:, 32:64]
    ld = small.ap[:, 64:96]
    dcol = small.ap[:, 96:128]
    B1 = big.ap[:, 0:512].rearrange("p (g c) -> p g c", c=CH)
    B2 = big.ap[:, 512:1024].rearrange("p (g c) -> p g c", c=CH)
    C1 = big.ap[:, 1024:1536].rearrange("p (g c) -> p g c", c=CH)
    C2 = big.ap[:, 1536:2048].rearrange("p (g c) -> p g c", c=CH)

    EC = list(range(-7, 8 * NI + 1))
    EB = [8 * (NI - 1 - i) + 7 - j for i in range(NI) for j in range(8)]
    EX = EC + EB
    NE = len(EX)
    NC_ = len(EC)
    tb = T(None)
    def tl(cols, dt=F32):
        t = ar.alloc(cols, dt)
        return t.ap
    Pr = tl(32 * NE); Pi = tl(32 * NE); nsPi = tl(32 * NE); sPi = tl(32 * NE)
    Bb1 = tl(512); Bb2 = tl(512)
    zT = ar.alloc(4 * 8 * KP, BF16)
    zTv = zT.ap.rearrange("p (e j k) -> p e j k", j=8, k=KP)
    off_after_zT = ar.off
    U = ar.alloc(G * KP, BF16)
    Uv = U.ap.rearrange("p (g k) -> p g k", k=KP)
    ar.mark()
    dt_ = tl(32); lrdt = tl(32); lidt = tl(32)
    argm = tl(32 * NE); arga = tl(32 * NE); argi = tl(32 * NE, I32); argf = tl(32 * NE)
    m1 = tl(32 * NE)
    R = [small.b, big.b, cst.b, tb.b]
    W = [tb.b]
    def dve(meth, **kw):
        sch.op("dve", meth, kw, reads=R, writes=W)
    def act(**kw):
        sch.op("act", "activation", kw, reads=R, writes=W)
    act(out=dt_, in_=ld, func=AF.Exp)
    dve("tensor_tensor", out=lrdt, in0=lr, in1=dt_, op=ALU.mult)
    dve("tensor_tensor", out=lidt, in0=li, in1=dt_, op=ALU.mult)
    v3 = lambda a: a.rearrange("p (g n) -> p g n", n=NE)
    for i, n in enumerate(EX):
        dve("tensor_scalar", out=v3(argm)[:, :, i], in0=lrdt, scalar1=float(n), scalar2=None, op0=ALU.mult)
        dve("tensor_scalar", out=v3(arga)[:, :, i], in0=lidt, scalar1=float(n) / (2 * math.pi), scalar2=None, op0=ALU.mult)
    act(out=argm, in_=argm, func=AF.Exp)
    dve("tensor_copy", out=argi, in_=arga)
    dve("tensor_copy", out=argf, in_=argi)
    dve("tensor_tensor", out=arga, in0=arga, in1=argf, op=ALU.subtract)
    def wrap(y):
        dve("tensor_scalar", out=m1, in0=y, scalar1=0.5, scalar2=None, op0=ALU.is_gt)
        dve("tensor_tensor", out=y, in0=y, in1=m1, op=ALU.subtract)
        dve("tensor_scalar", out=m1, in0=y, scalar1=-0.5, scalar2=None, op0=ALU.is_lt)
        dve("tensor_tensor", out=y, in0=y, in1=m1, op=ALU.add)
    wrap(arga)
    act(out=Pi, in_=arga, func=AF.Sin, scale=TWO_PI_S)
    dve("tensor_scalar", out=arga, in0=arga, scalar1=0.25, scalar2=None, op0=ALU.add)
    wrap(arga)
    act(out=Pr, in_=arga, func=AF.Sin, scale=TWO_PI_S)
    dve("tensor_tensor", out=Pr, in0=Pr, in1=argm, op=ALU.mult)
    dve("tensor_tensor", out=Pi, in0=Pi, in1=argm, op=ALU.mult)
    dve("tensor_scalar", out=nsPi, in0=Pi, scalar1=nsg, scalar2=None, op0=ALU.mult)
    dve("tensor_scalar", out=sPi, in0=Pi, scalar1=sg, scalar2=None, op0=ALU.mult)
    i1 = EC.index(1)
    nr = tl(32); den = tl(32); fr = tl(32); fi = tl(32); t32 = tl(32); nsfi = tl(32); sfi = tl(32)
    dve("tensor_scalar", out=nr, in0=v3(Pr)[:, :, i1], scalar1=-1.0, scalar2=None, op0=ALU.add)
    ni_ = v3(Pi)[:, :, i1]
    dve("tensor_tensor", out=den, in0=lr, in1=lr, op=ALU.mult)
    dve("tensor_tensor", out=t32, in0=li, in1=li, op=ALU.mult)
    dve("tensor_tensor", out=den, in0=den, in1=t32, op=ALU.add)
    dve("reciprocal", out=den, in_=den)
    dve("tensor_tensor", out=fr, in0=nr, in1=lr, op=ALU.mult)
    dve("tensor_tensor", out=t32, in0=ni_, in1=li, op=ALU.mult)
    dve("tensor_tensor", out=fr, in0=fr, in1=t32, op=ALU.add)
    dve("tensor_tensor", out=fr, in0=fr, in1=den, op=ALU.mult)
    dve("tensor_tensor", out=fi, in0=ni_, in1=lr, op=ALU.mult)
    dve("tensor_tensor", out=t32, in0=nr, in1=li, op=ALU.mult)
    dve("tensor_tensor", out=fi, in0=fi, in1=t32, op=ALU.subtract)
    dve("tensor_tensor", out=fi, in0=fi, in1=den, op=ALU.mult)
    dve("tensor_scalar", out=nsfi, in0=fi, scalar1=nsg, scalar2=None, op0=ALU.mult)
    dve("tensor_scalar", out=sfi, in0=fi, scalar1=sg, scalar2=None, op0=ALU.mult)
    tB = tl(512)
    g3 = lambda a: a.rearrange("p (g c) -> p g c", c=CH)
    bc = lambda a: a.unsqueeze(2).broadcast_to([128, 32, CH])
    dve("tensor_tensor", out=g3(Bb1), in0=B1, in1=bc(fr), op=ALU.mult)
    dve("tensor_tensor", out=g3(tB), in0=B2, in1=bc(nsfi), op=ALU.mult)
    dve("tensor_tensor", out=Bb1, in0=Bb1, in1=tB, op=ALU.add)
    dve("tensor_tensor", out=g3(Bb2), in0=B2, in1=bc(fr), op=ALU.mult)
    dve("tensor_tensor", out=g3(tB), in0=B1, in1=bc(sfi), op=ALU.mult)
    dve("tensor_tensor", out=Bb2, in0=Bb2, in1=tB, op=ALU.add)
    iA = EC.index(8 * NI)
    A1 = v3(Pr)[:, :, iA]
    A2 = v3(nsPi)[:, :, iA]
    A2w = v3(sPi)[:, :, iA]

    NB8 = NI * 8
    def ba_tables(g, outst, outsw, tmp):
        o4 = lambda a: a.rearrange("p (n c) -> p n c", c=CH)
        prb = v3(Pr)[:, g, NC_:NE].unsqueeze(2).broadcast_to([128, NB8, CH])
        nspb = v3(nsPi)[:, g, NC_:NE].unsqueeze(2).broadcast_to([128, NB8, CH])
        spb = v3(sPi)[:, g, NC_:NE].unsqueeze(2).broadcast_to([128, NB8, CH])
        b1 = g3(Bb1)[:, g, :].unsqueeze(1).broadcast_to([128, NB8, CH])
        b2 = g3(Bb2)[:, g, :].unsqueeze(1).broadcast_to([128, NB8, CH])
        rw = dict(reads=[tb.b], writes=[outst.b, outsw.b, tmp.b])
        sch.op("dve", "tensor_tensor", dict(out=o4(outst.ap), in0=prb, in1=b1, op=ALU.mult), **rw)
        sch.op("dve", "tensor_tensor", dict(out=o4(tmp.ap), in0=nspb, in1=b2, op=ALU.mult), **rw)
        sch.op("dve", "tensor_tensor", dict(out=outst.ap, in0=outst.ap, in1=tmp.ap, op=ALU.add), **rw)
        if outsw is not outst:
            sch.op("dve", "tensor_tensor", dict(out=o4(outsw.ap), in0=prb, in1=b2, op=ALU.mult), **rw)
            sch.op("dve", "tensor_tensor", dict(out=o4(tmp.ap), in0=spb, in1=b1, op=ALU.mult), **rw)
            sch.op("dve", "tensor_tensor", dict(out=outsw.ap, in0=outsw.ap, in1=tmp.ap, op=ALU.add), **rw)

    sch.barrier()
    ar.release()
    ar.mark()
    w_in = ar.alloc(8 * E, BF16)
    sch.dma("pool", w_in.ap.rearrange("p (c e) -> p c e", e=E), w_in_d.rearrange("(c p) e -> p c e", p=128), writes=[w_in.b])
    hblk = ar.alloc(8 * D)
    hnj = [ar.alloc(D, BF16) for _ in range(2)]
    hnT = [ar.alloc(8 * 128, BF16) for _ in range(2)]
    utok = ar.alloc(8 * E, BF16)
    utv = utok.ap.rearrange("p (g j c) -> p g j c", j=8, c=CH)
    ss = ar.alloc(8); rs = ar.alloc(8)
    junk = ar.alloc(D, BF16)
    pt = P.psum[6:8]
    pu = P.psum[4:6]
    for kb in range(NKB):
        r0 = kb * 1024
        sch.dma("sp", hblk.ap, src[r0:r0 + 1024, :].rearrange("(k j) d -> k (j d)", j=8), writes=[hblk.b])
        for j in range(8):
            sch.op("act", "activation", dict(out=junk.ap, in_=hblk.ap[:, j * D:(j + 1) * D], func=AF.Square,
                                             accum_out=ss.ap[:, j:j + 1]), reads=[hblk.b], writes=[junk.b, ss.b])
        P.rstd(rs, ss, D)
        for j in range(8):
            hn = hnj[j % 2]
            sch.op("dve", "scalar_tensor_tensor", dict(out=hn.ap, in0=hblk.ap[:, j * D:(j + 1) * D],
                                                       scalar=rs.ap[:, j:j + 1], in1=gin.ap, op0=ALU.mult, op1=ALU.mult),
                   reads=[hblk.b, rs.b, gin.b], writes=[hn.b])
            hT = hnT[j % 2]
            for kc2 in range(2):
                ptb = pt[kc2]
                ptv = ptb.ap.bitcast(BF16)
                for q in range(4):
                    kc = kc2 * 4 + q
                    sch.op("pe", "transpose", dict(out=ptv[:, q * 128:(q + 1) * 128],
                                                   in_=hn.ap[:, kc * 128:(kc + 1) * 128], identity=P.ident_bf.ap),
                           reads=[hn.b, P.ident_bf.b], writes=[ptb.b], inc=(q == 3))
                if kc2 == 0:
                    sch.op("act", "copy", dict(out=hT.ap[:, 0:512], in_=ptv[:, 0:512]), reads=[ptb.b], writes=[hT.b])
                else:
                    sch.op("dve", "tensor_copy", dict(out=hT.ap[:, 512:1024], in_=ptv[:, 0:512]), reads=[ptb.b], writes=[hT.b])
            pub = pu[j % 2]
            for kc in range(8):
                sch.op("pe", "matmul", dict(out=pub.ap[:, 0:E], lhsT=hT.ap[:, kc * 128:(kc + 1) * 128],
                                            rhs=w_in.ap[:, kc * E:(kc + 1) * E], start=(kc == 0), stop=(kc == 7)),
                       reads=[hT.b, w_in.b], writes=[pub.b], inc=(kc == 7))
            sch.op("act", "copy", dict(out=utv[:, :, j, :], in_=pub.ap[:, 0:E].rearrange("p (g c) -> p g c", c=CH)),
                   reads=[pub.b], writes=[utok.b])
        for g4 in range(8):
            ptb = pt[g4 % 2]
            ptv = ptb.ap.bitcast(BF16)
            for q in range(4):
                g = g4 * 4 + q
                sch.op("pe", "transpose", dict(out=ptv[:, q * 128:(q + 1) * 128], in_=utok.ap[:, g * 128:(g + 1) * 128],
                                               identity=P.ident_bf.ap),
                       reads=[utok.b, P.ident_bf.b], writes=[ptb.b], inc=(q == 3))
            eng = ("act", "copy") if g4 % 2 == 0 else ("dve", "tensor_copy")
            sch.op(eng[0], eng[1], dict(out=Uv[:, g4 * 4:g4 * 4 + 4, kb * 128:(kb + 1) * 128],
                                        in_=ptv[:, 0:512].rearrange("p (g k) -> p g k", k=128)),
                   reads=[ptb.b], writes=[U.b])

    sch.barrier()
    ar.release()
    Sst = ar.alloc(G * NM, BF16)
    Ssw = ar.alloc(G * NM, BF16)
    Sstv = Sst.ap.rearrange("p (g m) -> p g m", m=NM)
    Sswv = Ssw.ap.rearrange("p (g m) -> p g m", m=NM)
    bast = [ar.alloc(NB8 * CH) for _ in range(2)]
    basw = [ar.alloc(NB8 * CH) for _ in range(2)]
    btmp = ar.alloc(NB8 * CH)
    wsst = [ar.alloc(NI * 128, BF16) for _ in range(2)]
    wssw = [ar.alloc(NI * 128, BF16) for _ in range(2)]
    ps_s = P.psum[0:4]
    for g in range(G):
        pb = g % 2
        ba_tables(g, bast[pb], basw[pb], btmp)
        for which, (ba, ws) in enumerate(((bast[pb], wsst[pb]), (basw[pb], wssw[pb]))):
            ptb = pt[which]
            for i in range(NI):
                sch.op("pe", "transpose", dict(out=ptb.ap[:, i * 128:(i + 1) * 128], in_=ba.ap[:, i * 128:(i + 1) * 128],
                                               identity=P.ident_f.ap),
                       reads=[ba.b, P.ident_f.b], writes=[ptb.b], inc=(i == NI - 1))
            sch.op("act", "copy", dict(out=ws.ap, in_=ptb.ap[:, 0:NI * 128]), reads=[ptb.b], writes=[ws.b])
        for which, (ws, Sv, Sb) in enumerate(((wsst[pb], Sstv, Sst), (wssw[pb], Sswv, Ssw))):
            psb = ps_s[(g % 2) * 2 + which]
            for i in range(NI):
                rhs = Uv[:, g, :].rearrange("p (m i) -> p m i", i=NI)[:, :, i]
                sch.op("pe", "matmul", dict(out=psb.ap[:, 0:NM], lhsT=ws.ap[:, i * 128:(i + 1) * 128], rhs=rhs,
                                            start=(i == 0), stop=(i == NI - 1)),
                       reads=[ws.b, U.b], writes=[psb.b], inc=(i == NI - 1))
            sch.op("dve", "tensor_copy", dict(out=Sv[:, g, :], in_=psb.ap[:, 0:NM]), reads=[psb.b], writes=[Sb.b])

    Xs = ar.alloc(32); Xw = ar.alloc(32); t1 = ar.alloc(32); t2 = ar.alloc(32)
    sb = T(None)
    rw = dict(reads=[tb.b, sb.b, Sst.b, Ssw.b], writes=[sb.b])
    sch.op("dve", "memset", dict(ap=Xs.ap, constant=0.0), **rw)
    sch.op("dve", "memset", dict(ap=Xw.ap, constant=0.0), **rw)
    for m in range(NM):
        tt = lambda **kw: sch.op("dve", "tensor_tensor", kw, **rw)
        tt(out=t1.ap, in0=Xs.ap, in1=A1, op=ALU.mult)
        tt(out=t2.ap, in0=Xw.ap, in1=A2, op=ALU.mult)
        tt(out=t1.ap, in0=t1.ap, in1=t2.ap, op=ALU.add)
        tt(out=t2.ap, in0=Xw.ap, in1=A1, op=ALU.mult)
        tt(out=Xw.ap, in0=Xs.ap, in1=A2w, op=ALU.mult)
        tt(out=Xw.ap, in0=Xw.ap, in1=t2.ap, op=ALU.add)
        tt(out=Xw.ap, in0=Xw.ap, in1=Sswv[:, :, m], op=ALU.add)
        tt(out=Xs.ap, in0=t1.ap, in1=Sstv[:, :, m], op=ALU.add)
        sch.op("dve", "tensor_copy", dict(out=Sstv[:, :, m], in_=Xs.ap), reads=[sb.b], writes=[sb.b, Sst.b])

    cast = [ar.alloc((NC_) * CH) for _ in range(2)]
    ctmp = ar.alloc((NC_) * CH)
    L1 = [ar.alloc(128) for _ in range(2)]
    Tg = [ar.alloc(NI * 128, BF16) for _ in range(2)]
    WY = [ar.alloc(NI * 128, BF16) for _ in range(2)]
    tmask = ar.alloc(128)
    zg = [ar.alloc(KP, BF16) for _ in range(2)]
    gx2 = ar.alloc(KP); gt = ar.alloc(KP)
    psy = P.psum[0:2]
    pst = P.psum[2:4]
    for g in range(G):
        pb = g % 2
        ba_tables(g, bast[pb], bast[pb], btmp)
        o4 = lambda a: a.rearrange("p (n c) -> p n c", c=CH)
        prc = v3(Pr)[:, g, 0:NC_].unsqueeze(2).broadcast_to([128, NC_, CH])
        nspc = v3(nsPi)[:, g, 0:NC_].unsqueeze(2).broadcast_to([128, NC_, CH])
        c1 = C1[:, g, :].unsqueeze(1).broadcast_to([128, NC_, CH])
        c2 = C2[:, g, :].unsqueeze(1).broadcast_to([128, NC_, CH])
        rwc = dict(reads=[tb.b, big.b], writes=[cast[pb].b, ctmp.b])
        sch.op("dve", "tensor_tensor", dict(out=o4(cast[pb].ap), in0=prc, in1=c1, op=ALU.mult), **rwc)
        sch.op("dve", "tensor_tensor", dict(out=o4(ctmp.ap), in0=nspc, in1=c2, op=ALU.mult), **rwc)
        sch.op("dve", "tensor_tensor", dict(out=cast[pb].ap, in0=cast[pb].ap, in1=ctmp.ap, op=ALU.add), **rwc)
        sch.op("dve", "tensor_scalar", dict(out=L1[pb].ap, in0=bast[pb].ap[:, (NI - 1) * 128:NI * 128], scalar1=sg, scalar2=None, op0=ALU.mult),
               reads=[bast[pb].b, cst.b], writes=[L1[pb].b])
        ptb = pst[pb]
        for dl in range(NI):
            sch.op("pe", "matmul", dict(out=ptb.ap[:, dl * 128:(dl + 1) * 128], lhsT=L1[pb].ap,
                                        rhs=cast[pb].ap[:, dl * 128:(dl + 1) * 128], start=True, stop=True),
                   reads=[L1[pb].b, cast[pb].b], writes=[ptb.b], inc=(dl == NI - 1))
        sch.op("dve", "tensor_tensor", dict(out=tmask.ap, in0=ptb.ap[:, 0:128], in1=cmask, op=ALU.mult),
               reads=[ptb.b, cst.b], writes=[tmask.b])
        sch.op("dve", "scalar_tensor_tensor", dict(out=Tg[pb].ap[:, 0:128], in0=P.ident_f.ap, scalar=dcol[:, g:g + 1], in1=tmask.ap,
                                                   op0=ALU.mult, op1=ALU.add),
               reads=[tmask.b, P.ident_f.b, small.b], writes=[Tg[pb].b])
        if NI > 1:
            sch.op("act", "copy", dict(out=Tg[pb].ap[:, 128:NI * 128], in_=ptb.ap[:, 128:NI * 128]), reads=[ptb.b], writes=[Tg[pb].b])
        sch.op("dve", "tensor_scalar", dict(out=WY[pb].ap, in0=cast[pb].ap[:, 128:128 + NI * 128], scalar1=sg, scalar2=None, op0=ALU.mult),
               reads=[cast[pb].b, cst.b], writes=[WY[pb].b])
        yb = psy[pb]
        yv = yb.ap[:, 0:KP].rearrange("p (m i) -> p m i", i=NI)
        uv = Uv[:, g, :].rearrange("p (m i) -> p m i", i=NI)
        for dl in range(NI):
            sch.op("pe", "matmul", dict(out=yv[:, :, dl:NI], lhsT=Tg[pb].ap[:, dl * 128:(dl + 1) * 128], rhs=uv[:, :, 0:NI - dl],
                                        start=(dl == 0), stop=False),
                   reads=[Tg[pb].b, U.b], writes=[yb.b], inc=False)
        for i in range(NI):
            sch.op("pe", "matmul", dict(out=yv[:, 1:NM, i], lhsT=WY[pb].ap[:, i * 128:(i + 1) * 128], rhs=Sstv[:, g, 0:NM - 1],
                                        start=False, stop=(i == NI - 1)),
                   reads=[WY[pb].b, Sst.b], writes=[yb.b], inc=(i == NI - 1))
        y = yb.ap[:, 0:KP]
        sch.op("act", "activation", dict(out=gx2.ap, in_=y, func=AF.Square), reads=[yb.b], writes=[gx2.b])
        sch.op("dve", "tensor_scalar", dict(out=gx2.ap, in0=gx2.ap, scalar1=0.044715, scalar2=1.0, op0=ALU.mult, op1=ALU.add),
               reads=[gx2.b], writes=[gx2.b])
        sch.op("dve", "tensor_tensor", dict(out=gt.ap, in0=y, in1=gx2.ap, op=ALU.mult), reads=[yb.b, gx2.b], writes=[gt.b])
        sch.op("act", "activation", dict(out=gt.ap, in_=gt.ap, func=AF.Sigmoid, scale=1.5957691216057308), reads=[gt.b], writes=[gt.b])
        sch.op("dve", "tensor_tensor", dict(out=zg[pb].ap, in0=y, in1=gt.ap, op=ALU.mult), reads=[yb.b, gt.b], writes=[zg[pb].b])
        ech, gi = g // 8, g % 8
        for j in range(8):
            sch.dma("sp" if j % 2 == 0 else "pool", zTv[gi * 16:(gi + 1) * 16, ech, j, :], zg[pb].ap[j * 16:(j + 1) * 16, :],
                    reads=[zg[pb].b], writes=[zT.b])

    sch.barrier()
    ar.off = off_after_zT
    w_glu = ar.alloc(4 * E, BF16)
    w_out = ar.alloc(4 * D, BF16)
    sch.dma("pool", w_glu.ap.rearrange("p (c e) -> p c e", e=E), w_glu_d.rearrange("(c p) e -> p c e", p=128), writes=[w_glu.b])
    sch.dma("pool", w_out.ap.rearrange("p (c e) -> p c e", e=D), w_out_d.rearrange("(c p) e -> p c e", p=128), writes=[w_out.b])
    junk = ar.alloc(D, BF16)
    z2 = [ar.alloc(4 * 512, BF16) for _ in range(2)]
    sgm = ar.alloc(512)
    hres = [ar.alloc(D) for _ in range(2)]
    yt = ar.alloc(D)
    rs2 = ar.alloc(2)
    pg = P.psum[4:6]
    po = P.psum[0:4]
    NKC = KP // 512 if KP >= 512 else 1
    KW = min(KP, 512)
    cnt = 0
    for j in range(8):
        for kc5 in range(NKC):
            k0 = kc5 * KW
            zz = z2[cnt % 2]
            for eo in range(4):
                pgb = pg[eo % 2]
                for ei in range(4):
                    sch.op("pe", "matmul", dict(out=pgb.ap[:, 0:KW], lhsT=w_glu.ap[:, ei * E + eo * 128:ei * E + (eo + 1) * 128],
                                                rhs=zTv[:, ei, j, k0:k0 + KW], start=(ei == 0), stop=(ei == 3)),
                           reads=[w_glu.b, zT.b], writes=[pgb.b], inc=(ei == 3))
                sch.op("act", "activation", dict(out=sgm.ap[:, 0:KW], in_=pgb.ap[:, 0:KW], func=AF.Sigmoid), reads=[pgb.b], writes=[sgm.b])
                sch.op("dve", "tensor_tensor", dict(out=zz.ap[:, eo * 512:eo * 512 + KW], in0=sgm.ap[:, 0:KW], in1=zTv[:, eo, j, k0:k0 + KW], op=ALU.mult),
                       reads=[sgm.b, zT.b], writes=[zz.b])
            for kq in range(KW // 128):
                kk = k0 + kq * 128
                hb = hres[kq % 2]
                rows = src.rearrange("(k j) d -> j k d", j=8)[j, kk:kk + 128, :]
                orows = dst.rearrange("(k j) d -> j k d", j=8)[j, kk:kk + 128, :]
                sch.dma("sp", hb.ap, rows, writes=[hb.b])
                for half in range(2):
                    pob = po[(kq % 2) * 2 + half]
                    for ei in range(4):
                        sch.op("pe", "matmul", dict(out=pob.ap[:, 0:512], lhsT=zz.ap[:, ei * 512 + kq * 128:ei * 512 + (kq + 1) * 128],
                                                    rhs=w_out.ap[:, ei * D + half * 512:ei * D + (half + 1) * 512],
                                                    start=(ei == 0), stop=(ei == 3)),
                               reads=[zz.b, w_out.b], writes=[pob.b], inc=(ei == 3))
                    sch.op("act", "activation", dict(out=junk.ap[:, 0:512], in_=pob.ap[:, 0:512], func=AF.Square,
                                                     accum_out=rs2.ap[:, half:half + 1]), reads=[pob.b], writes=[junk.b, rs2.b])
                sch.op("dve", "tensor_tensor", dict(out=rs2.ap[:, 0:1], in0=rs2.ap[:, 0:1], in1=rs2.ap[:, 1:2], op=ALU.add),
                       reads=[rs2.b], writes=[rs2.b])
                r1 = T(rs2.ap[:, 0:1], rs2.b)
                P.rstd(r1, r1, D)
                for half in range(2):
                    pob = po[(kq % 2) * 2 + half]
                    sch.op("dve", "scalar_tensor_tensor", dict(out=yt.ap[:, half * 512:(half + 1) * 512], in0=pob.ap[:, 0:512],
                                                               scalar=rs2.ap[:, 0:1], in1=gout.ap[:, half * 512:(half + 1) * 512],
                                                               op0=ALU.mult, op1=ALU.mult),
                           reads=[pob.b, rs2.b, gout.b], writes=[yt.b])
                sch.op("pool", "tensor_tensor", dict(out=hb.ap, in0=yt.ap, in1=hb.ap, op=ALU.add), reads=[yt.b, hb.b], writes=[hb.b])
                sch.dma("sp", orows, hb.ap, reads=[hb.b])
            cnt += 1
    P.phase_end()


HP = [0, 4, 1, 5, 2, 6, 3, 7, 8, 12, 9, 13, 10, 14, 11, 15]
QO, KO, QIO, KIO, VO, WIO, NCOL = 0, 1024, 1280, 1792, 1920, 2176, 2184
NEG = -1.0e30
NBIS = 22
import os
NO_INTERLEAVE = bool(int(os.environ.get('NO_INTERLEAVE', '0')))
FRONT_PRIO = float(os.environ.get('FRONT_PRIO', '1.0'))
NO_POOL = bool(int(os.environ.get('NO_POOL', '1')))


def dsa_host_layout(w_in, w_out):
    q = w_in[:, 0:1024].reshape(1024, 16, 64)[:, HP, :].reshape(1024, 1024)
    k = w_in[:, 1024:1280]
    v = w_in[:, 1280:1536]
    qi = w_in[:, 1536:2048]
    ki = w_in[:, 2048:2112]
    wi = w_in[:, 2112:2120]
    wip = np.concatenate([q, k, qi, ki, ki, v, wi], 1)
    wop = w_out.reshape(16, 64, 1024)[HP].reshape(1024, 1024)
    return np.ascontiguousarray(wip, dtype=np.float32), np.ascontiguousarray(wop, dtype=np.float32)


def dsa_consts():
    r = np.arange(128)
    negm = np.where(r[None, :] > r[:, None], NEG, 0.0).astype(np.float32)
    return negm


def dsa_phase(P, src, dst, w_in_d, w_out_d, negm_d, gin_row, gout_row):
    sch, ar, S = P.sch, P.ar, P.S
    NT = S // 128
    KSEL = min(256, S // 4)
    P.phase_begin()
    w_in = ar.alloc(8 * NCOL, BF16)
    w_out = ar.alloc(8 * D, BF16)
    wiv = w_in.ap.rearrange("p (c e) -> p c e", e=NCOL)
    wsrc = w_in_d.rearrange("(c p) e -> p c e", p=128)
    for c0 in range(0, NCOL, 512):
        c1 = min(NCOL, c0 + 512)
        sch.dma("pool", wiv[:, :, c0:c1], wsrc[:, :, c0:c1], writes=[w_in.b])
    sch.dma("pool", w_out.ap.rearrange("p (c e) -> p c e", e=D), w_out_d.rearrange("(c p) e -> p c e", p=128), writes=[w_out.b])
    gin = ar.alloc(D); gout = ar.alloc(D); negm = ar.alloc(128)
    sch.dma("sp", gin.ap, gin_row.partition_broadcast(128), writes=[gin.b])
    sch.dma("sp", gout.ap, gout_row.partition_broadcast(128), writes=[gout.b])
    sch.dma("sp", negm.ap, negm_d, writes=[negm.b])
    cosT = ar.alloc(NT * 32); sinT = ar.alloc(NT * 32)
    ar.mark()
    tb = T(None)
    posi = ar.alloc(NT, I32); posf = ar.alloc(NT); fi_ = ar.alloc(32, I32); invf = ar.alloc(32)
    ang = ar.alloc(NT * 32); angi = ar.alloc(NT * 32, I32); angf = ar.alloc(NT * 32); m1 = ar.alloc(NT * 32)
    rw = dict(reads=[tb.b], writes=[tb.b])
    sch.op("pool", "iota", dict(out=posi.ap, pattern=[[128, NT]], base=0, channel_multiplier=1), **rw)
    sch.op("pool", "iota", dict(out=fi_.ap, pattern=[[1, 32]], base=0, channel_multiplier=0), **rw)
    dve = lambda meth, **kw: sch.op("dve", meth, kw, **rw)
    dve("tensor_copy", out=posf.ap, in_=posi.ap)
    dve("tensor_copy", out=invf.ap, in_=fi_.ap)
    sch.op("act", "activation", dict(out=invf.ap, in_=invf.ap, func=AF.Exp, scale=-math.log(10000.0) / 32.0), **rw)
    dve("tensor_scalar", out=invf.ap, in0=invf.ap, scalar1=1.0 / (2 * math.pi), scalar2=None, op0=ALU.mult)
    a3 = lambda t: t.ap.rearrange("p (q i) -> p q i", i=32)
    dve("tensor_tensor", out=a3(ang), in0=posf.ap.unsqueeze(2).broadcast_to([128, NT, 32]),
        in1=invf.ap.unsqueeze(1).broadcast_to([128, NT, 32]), op=ALU.mult)
    dve("tensor_copy", out=angi.ap, in_=ang.ap)
    dve("tensor_copy", out=angf.ap, in_=angi.ap)
    dve("tensor_tensor", out=ang.ap, in0=ang.ap, in1=angf.ap, op=ALU.subtract)
    def wrap():
        dve("tensor_scalar", out=m1.ap, in0=ang.ap, scalar1=0.5, scalar2=None, op0=ALU.is_gt)
        dve("tensor_tensor", out=ang.ap, in0=ang.ap, in1=m1.ap, op=ALU.subtract)
        dve("tensor_scalar", out=m1.ap, in0=ang.ap, scalar1=-0.5, scalar2=None, op0=ALU.is_lt)
        dve("tensor_tensor", out=ang.ap, in0=ang.ap, in1=m1.ap, op=ALU.add)
    wrap()
    sch.op("act", "activation", dict(out=sinT.ap, in_=ang.ap, func=AF.Sin, scale=TWO_PI_S), reads=[tb.b], writes=[tb.b, sinT.b])
    dve("tensor_scalar", out=ang.ap, in0=ang.ap, scalar1=0.25, scalar2=None, op0=ALU.add)
    wrap()
    sch.op("act", "activation", dict(out=cosT.ap, in_=ang.ap, func=AF.Sin, scale=TWO_PI_S), reads=[tb.b], writes=[tb.b, cosT.b])
    sch.barrier()
    ar.release()
    cos3 = cosT.ap.rearrange("p (q i) -> p q i", i=32)
    sin3 = sinT.ap.rearrange("p (q i) -> p q i", i=32)

    kT = ar.alloc(2 * S, BF16)
    kTv = kT.ap.rearrange("p (b s) -> p b s", s=S)
    kiT = ar.alloc(S, BF16)
    V = ar.alloc(NT * 4 * 65, BF16)
    Vv = V.ap.rearrange("p (t n e) -> p t n e", n=4, e=65)
    kT_b = [Buf() for _ in range(NT)]
    kiT_b = [Buf() for _ in range(NT)]
    V_b = [Buf() for _ in range(NT)]
    sch.op("pool", "memset", dict(ap=V.ap, constant=1.0), writes=V_b)
    acc = ar.alloc(S)
    mask = ar.alloc(S, BF16)
    maskT = [ar.alloc(S, BF16) for _ in range(2)]
    hq = [ar.alloc(D) for _ in range(2)]
    qT = [ar.alloc(8 * 128, BF16) for _ in range(2)]
    hn = ar.alloc(D, BF16)
    hnT = ar.alloc(8 * 128, BF16)
    rp = ar.alloc(1920, BF16)
    tr = [ar.alloc(256) for _ in range(4)]
    wsb = ar.alloc(8); absw = ar.alloc(8); sgnw = ar.alloc(8)
    qiT = ar.alloc(4 * 128, BF16)
    rr = [ar.alloc(512) for _ in range(2)]
    pT = [ar.alloc(512, BF16) for _ in range(4)]
    osb = ar.alloc(D, BF16)
    oT = ar.alloc(8 * 128, BF16)
    yt = ar.alloc(D)
    junk = ar.alloc(D, BF16)
    junk2 = ar.alloc(512, BF16)
    ss = ar.alloc(1); rs = ar.alloc(1); rs2 = ar.alloc(2)
    lo = ar.alloc(1); hi = ar.alloc(1); mid = ar.alloc(1); cnt = ar.alloc(1); ge = ar.alloc(1); dd = ar.alloc(1)
    rden = ar.alloc(4)
    wt = ar.alloc(NBIS + 2); pow2 = ar.alloc(NBIS + 2); cntB = ar.alloc(1)
    junkA = ar.alloc(S - (int(S * 0.42) // 64) * 64 + 64, BF16)
    for k in range(NBIS + 2):
        sch.op("pool", "memset", dict(ap=pow2.ap[:, k:k + 1], constant=2.0 ** (-k)), writes=[pow2.b])
    pqk = P.psum[0:2]
    ppv = P.psum[2:4]
    pout = P.psum[0:2]
    pf = P.psum[4:7]
    ptf = P.psum[7]

    def rstd_ln(out, ssq):
        sch.op("act", "activation", dict(out=out.ap, in_=ssq.ap, func=AF.Ln, bias=P.eps.ap, scale=1.0 / D), reads=[ssq.b, P.eps.b], writes=[out.b])
        sch.op("act", "activation", dict(out=out.ap, in_=out.ap, func=AF.Exp, scale=-0.5), reads=[out.b], writes=[out.b])

    def front(qt):
        r0 = qt * 128
        L = (qt + 1) * 128
        h = hq[qt % 2]
        qTq = qT[qt % 2]
        mT = maskT[qt % 2]
        sch.dma("sp", h.ap, src[r0:r0 + 128, :], writes=[h.b])
        sch.op("act", "activation", dict(out=junk.ap, in_=h.ap, func=AF.Square, accum_out=ss.ap), reads=[h.b], writes=[junk.b, ss.b])
        rstd_ln(rs, ss)
        sch.op("dve", "scalar_tensor_tensor", dict(out=hn.ap, in0=h.ap, scalar=rs.ap, in1=gin.ap, op0=ALU.mult, op1=ALU.mult),
               reads=[h.b, rs.b, gin.b], writes=[hn.b])
        yield
        ptv = ptf.ap.bitcast(BF16)
        for kc2 in range(2):
            for q in range(4):
                kc = kc2 * 4 + q
                sch.op("pe", "transpose", dict(out=ptv[:, q * 128:(q + 1) * 128], in_=hn.ap[:, kc * 128:(kc + 1) * 128], identity=P.ident_bf.ap),
                       reads=[hn.b, P.ident_bf.b], writes=[ptf.b], inc=(q == 3))
            sch.op("act", "copy", dict(out=hnT.ap[:, kc2 * 512:(kc2 + 1) * 512], in_=ptv[:, 0:512]), reads=[ptf.b], writes=[hnT.b])
            yield
        cq = cos3[:, qt, :]
        sq = sin3[:, qt, :]
        for nb in range(5):
            pjb = pf[nb % 3]
            c0 = nb * 512
            c1 = min(NCOL, c0 + 512)
            for kc in range(8):
                sch.op("pe", "matmul", dict(out=pjb.ap[:, 0:c1 - c0], lhsT=hnT.ap[:, kc * 128:(kc + 1) * 128], rhs=wiv[:, kc, c0:c1],
                                            start=(kc == 0), stop=(kc == 7)),
                       reads=[hnT.b, w_in.b], writes=[pjb.b], inc=(kc == 7))
            yield
            if nb < 4:
                nh = 8 if nb < 3 else 6
                x = pjb.ap[:, 0:nh * 64].rearrange("p (h t i) -> p h t i", t=2, i=32)
                o = rp.ap[:, nb * 512:nb * 512 + nh * 64].rearrange("p (h t i) -> p h t i", t=2, i=32)
                cb = cq.unsqueeze(1).broadcast_to([128, nh, 32])
                sb_ = sq.unsqueeze(1).broadcast_to([128, nh, 32])
                tv = [t_.ap[:, 0:nh * 32].rearrange("p (h i) -> p h i", i=32) for t_ in tr]
                R_ = [pjb.b, cosT.b, sinT.b]
                sch.op("dve", "tensor_tensor", dict(out=tv[0], in0=x[:, :, 0, :], in1=cb, op=ALU.mult), reads=R_, writes=[tr[0].b])
                sch.op("dve", "tensor_tensor", dict(out=tv[1], in0=x[:, :, 1, :], in1=sb_, op=ALU.mult), reads=R_, writes=[tr[1].b])
                sch.op("dve", "tensor_tensor", dict(out=tv[2], in0=x[:, :, 1, :], in1=cb, op=ALU.mult), reads=R_, writes=[tr[2].b])
                sch.op("dve", "tensor_tensor", dict(out=tv[3], in0=x[:, :, 0, :], in1=sb_, op=ALU.mult), reads=R_, writes=[tr[3].b])
                sch.op("pool", "tensor_tensor", dict(out=o[:, :, 0, :], in0=tv[0], in1=tv[1], op=ALU.subtract),
                       reads=[tr[0].b, tr[1].b], writes=[rp.b])
                sch.op("pool", "tensor_tensor", dict(out=o[:, :, 1, :], in0=tv[2], in1=tv[3], op=ALU.add),
                       reads=[tr[2].b, tr[3].b], writes=[rp.b])
            if nb == 3:
                sch.op("act", "copy", dict(out=Vv[:, qt, 0:2, 0:64], in_=pjb.ap[:, 384:512].rearrange("p (n e) -> p n e", e=64)),
                       reads=[pjb.b], writes=[V_b[qt]])
            if nb == 4:
                sch.op("act", "copy", dict(out=Vv[:, qt, 2:4, 0:64], in_=pjb.ap[:, 0:128].rearrange("p (n e) -> p n e", e=64)),
                       reads=[pjb.b], writes=[V_b[qt]])
                sch.op("dve", "tensor_copy", dict(out=wsb.ap, in_=pjb.ap[:, 128:136]), reads=[pjb.b], writes=[wsb.b])
            yield
        sch.op("dve", "tensor_scalar", dict(out=sgnw.ap, in0=wsb.ap, scalar1=0.0, scalar2=None, op0=ALU.is_ge), reads=[wsb.b], writes=[sgnw.b])
        sch.op("dve", "tensor_scalar", dict(out=sgnw.ap, in0=sgnw.ap, scalar1=2.0, scalar2=-1.0, op0=ALU.mult, op1=ALU.add), reads=[sgnw.b], writes=[sgnw.b])
        sch.op("dve", "tensor_tensor", dict(out=absw.ap, in0=wsb.ap, in1=sgnw.ap, op=ALU.mult), reads=[wsb.b, sgnw.b], writes=[absw.b])
        qiv = rp.ap[:, QIO:QIO + 512].rearrange("p (h e) -> p h e", e=64)
        sch.op("dve", "tensor_tensor", dict(out=qiv, in0=qiv, in1=absw.ap.unsqueeze(2).broadcast_to([128, 8, 64]), op=ALU.mult),
               reads=[rp.b, absw.b], writes=[rp.b])
        yield

        def tr4(cols):
            for q, co in enumerate(cols):
                sch.op("pe", "transpose", dict(out=ptv[:, q * 128:(q + 1) * 128], in_=rp.ap[:, co:co + 128], identity=P.ident_bf.ap),
                       reads=[rp.b, P.ident_bf.b], writes=[ptf.b], inc=(q == len(cols) - 1))
        for i0 in (0, 4):
            tr4([QO + (i0 + b) * 128 for b in range(4)])
            sch.op("act", "copy", dict(out=qTq.ap[:, i0 * 128:(i0 + 4) * 128], in_=ptv[:, 0:512]), reads=[ptf.b], writes=[qTq.b])
            yield
        tr4([QIO + b * 128 for b in range(4)])
        sch.op("act", "copy", dict(out=qiT.ap[:, 0:512], in_=ptv[:, 0:512]), reads=[ptf.b], writes=[qiT.b])
        yield
        tr4([KO, KO + 128, KIO])
        sch.op("dve", "tensor_copy", dict(out=kTv[:, :, r0:r0 + 128], in_=ptv[:, 0:256].rearrange("p (b s) -> p b s", s=128)),
               reads=[ptf.b], writes=[kT_b[qt]])
        sch.op("dve", "tensor_copy", dict(out=kiT.ap[:, r0:r0 + 128], in_=ptv[:, 256:384]), reads=[ptf.b], writes=[kiT_b[qt]])
        yield
        nch = (L + 511) // 512
        ci = 0
        for c in range(nch):
            s0 = c * 512
            wdt = min(512, L - s0)
            kb_ = kiT_b[4 * c:min(NT, 4 * c + 4)]
            for hh in range(8):
                pb = pf[ci % 3]
                ci += 1
                half = (hh % 2) * 64
                sch.op("pe", "matmul", dict(out=pb.ap[:, 0:wdt], lhsT=qiT.ap[half:half + 64, (hh // 2) * 128:(hh // 2 + 1) * 128],
                                            rhs=kiT.ap[half:half + 64, s0:s0 + wdt], start=True, stop=True),
                       reads=[qiT.b] + kb_, writes=[pb.b])
                r = rr[hh % 2]
                sch.op("act", "activation", dict(out=r.ap[:, 0:wdt], in_=pb.ap[:, 0:wdt], func=AF.Relu), reads=[pb.b], writes=[r.b])
                if hh == 0:
                    sch.op("dve", "tensor_scalar", dict(out=acc.ap[:, s0:s0 + wdt], in0=r.ap[:, 0:wdt], scalar1=sgnw.ap[:, 0:1], scalar2=None, op0=ALU.mult),
                           reads=[r.b, sgnw.b], writes=[acc.b])
                else:
                    sch.op("dve", "scalar_tensor_tensor", dict(out=acc.ap[:, s0:s0 + wdt], in0=r.ap[:, 0:wdt], scalar=sgnw.ap[:, hh:hh + 1],
                                                               in1=acc.ap[:, s0:s0 + wdt], op0=ALU.mult, op1=ALU.add),
                           reads=[r.b, sgnw.b, acc.b], writes=[acc.b])
                if hh % 2 == 1:
                    yield
        if L > KSEL:
            sch.op("dve", "tensor_reduce", dict(out=lo.ap, in_=acc.ap[:, 0:L], axis=AX.X, op=ALU.min), reads=[acc.b], writes=[lo.b])
        sch.op("dve", "tensor_tensor", dict(out=acc.ap[:, r0:L], in0=acc.ap[:, r0:L], in1=negm.ap, op=ALU.add), reads=[acc.b, negm.b], writes=[acc.b])
        if L > KSEL:
            sch.op("dve", "tensor_reduce", dict(out=hi.ap, in_=acc.ap[:, 0:L], axis=AX.X, op=ALU.max), reads=[acc.b], writes=[hi.b])
            bb = [lo.b, hi.b, mid.b, cnt.b, ge.b, dd.b, wt.b]
            sch.op("dve", "tensor_tensor", dict(out=dd.ap, in0=hi.ap, in1=lo.ap, op=ALU.subtract), reads=bb, writes=[dd.b])
            sch.op("dve", "tensor_scalar", dict(out=wt.ap, in0=pow2.ap, scalar1=dd.ap, scalar2=None, op0=ALU.mult), reads=bb + [pow2.b], writes=[wt.b])
            sch.op("dve", "tensor_tensor", dict(out=mid.ap, in0=lo.ap, in1=wt.ap[:, 1:2], op=ALU.add), reads=bb, writes=[mid.b])
            yield
            LA = (int(L * 0.42) // 64) * 64
            nB = L - LA
            for it in range(NBIS):
                sch.op("act", "activation", dict(out=junkA.ap[:, 0:nB], in_=acc.ap[:, LA:L], func=AF.Sign, bias=mid.ap, scale=-1.0,
                                                 accum_out=cntB.ap), reads=[acc.b, mid.b], writes=[junkA.b, cntB.b])
                sch.op("dve", "tensor_scalar", dict(out=mask.ap[:, 0:LA], in0=acc.ap[:, 0:LA], scalar1=mid.ap, scalar2=None, op0=ALU.is_ge, op1=ALU.add,
                                                    accum_out=cnt.ap), reads=[acc.b, mid.b], writes=[mask.b, cnt.b])
                sch.op("dve", "scalar_tensor_tensor", dict(out=ge.ap, in0=cntB.ap, scalar=-0.5, in1=cnt.ap, op0=ALU.mult, op1=ALU.add),
                       reads=[cntB.b, cnt.b], writes=[ge.b])
                sch.op("dve", "scalar_tensor_tensor", dict(out=ge.ap, in0=ge.ap, scalar=float(KSEL) - 0.5 - nB / 2.0, in1=wt.ap[:, it + 1:it + 2],
                                                           op0=ALU.is_ge, op1=ALU.mult), reads=[ge.b, wt.b], writes=[ge.b])
                sch.op("dve", "scalar_tensor_tensor", dict(out=mid.ap, in0=ge.ap, scalar=wt.ap[:, it + 2:it + 3], in1=mid.ap,
                                                           op0=ALU.subtract, op1=ALU.add), reads=[ge.b, wt.b, mid.b], writes=[mid.b])
                yield
            sch.op("dve", "tensor_tensor", dict(out=lo.ap, in0=mid.ap, in1=wt.ap[:, NBIS + 1:NBIS + 2], op=ALU.subtract), reads=bb, writes=[lo.b])
            sch.op("dve", "tensor_scalar", dict(out=mask.ap[:, 0:L], in0=acc.ap[:, 0:L], scalar1=lo.ap, scalar2=None, op0=ALU.is_ge),
                   reads=[acc.b, lo.b], writes=[mask.b])
        else:
            sch.op("dve", "tensor_scalar", dict(out=mask.ap[:, 0:L], in0=acc.ap[:, 0:L], scalar1=-1.0e29, scalar2=None, op0=ALU.is_ge),
                   reads=[acc.b], writes=[mask.b])
        yield
        for i0 in range(0, qt + 1, 4):
            n = min(4, qt + 1 - i0)
            for q in range(n):
                st = i0 + q
                sch.op("pe", "transpose", dict(out=ptv[:, q * 128:(q + 1) * 128], in_=mask.ap[:, st * 128:(st + 1) * 128], identity=P.ident_bf.ap),
                       reads=[mask.b, P.ident_bf.b], writes=[ptf.b], inc=(q == n - 1))
            sch.op("act", "copy", dict(out=mT.ap[:, i0 * 128:(i0 + n) * 128], in_=ptv[:, 0:n * 128]), reads=[ptf.b], writes=[mT.b])
            yield

    def back(qt):
        r0 = qt * 128
        h = hq[qt % 2]
        qTq = qT[qt % 2]
        mTv = maskT[qt % 2].ap.rearrange("p (t q) -> p t q", q=128)
        mTb = maskT[qt % 2].b
        cnt_p = 0
        for n in range(4):
            half = (n % 2) * 64
            a = n // 2
            pob = ppv[n % 2]
            for st in range(qt + 1):
                pq = pqk[cnt_p % 2]
                p_ = pT[cnt_p % 4]
                sch.op("pe", "matmul", dict(out=pq.ap[:, 0:512], lhsT=kTv[half:half + 64, a, st * 128:(st + 1) * 128],
                                            rhs=qTq.ap[half:half + 64, a * 512:(a + 1) * 512], start=True, stop=True),
                       reads=[kT_b[st], qTq.b], writes=[pq.b])
                sch.op("act", "activation", dict(out=p_.ap, in_=pq.ap[:, 0:512], func=AF.Exp, scale=0.125), reads=[pq.b], writes=[p_.b])
                eng = "pool" if (cnt_p % 3 == 2 and not NO_POOL) else "dve"
                p3 = p_.ap.rearrange("p (g t) -> p g t", t=128)
                sch.op(eng, "tensor_tensor", dict(out=p3, in0=p3, in1=mTv[:, st, :].unsqueeze(1).broadcast_to([128, 4, 128]), op=ALU.mult),
                       reads=[p_.b, mTb], writes=[p_.b])
                for g in range(4):
                    sch.op("pe", "matmul", dict(out=pob.ap[:, g * 65:(g + 1) * 65], lhsT=p_.ap[:, g * 128:(g + 1) * 128], rhs=Vv[:, st, n, :],
                                                start=(st == 0 and g == 0), stop=(st == qt and g == 3)),
                           reads=[p_.b, V_b[st]], writes=[pob.b], inc=(g == 3))
                cnt_p += 1
                yield
            po3 = pob.ap[:, 0:260].rearrange("p (g e) -> p g e", e=65)
            sch.op("dve", "reciprocal", dict(out=rden.ap, in_=po3[:, :, 64]), reads=[pob.b], writes=[rden.b])
            ov = osb.ap[:, a * 512:(a + 1) * 512].rearrange("p (g two e) -> p g two e", two=2, e=64)[:, :, n % 2, :]
            sch.op("dve", "tensor_tensor", dict(out=ov, in0=po3[:, :, 0:64], in1=rden.ap.unsqueeze(2).broadcast_to([128, 4, 64]), op=ALU.mult),
                   reads=[pob.b, rden.b], writes=[osb.b])
        yield
        for kc2 in range(2):
            ptb = ppv[kc2]
            ptv = ptb.ap.bitcast(BF16)
            for q in range(4):
                kc = kc2 * 4 + q
                sch.op("pe", "transpose", dict(out=ptv[:, q * 128:(q + 1) * 128], in_=osb.ap[:, kc * 128:(kc + 1) * 128], identity=P.ident_bf.ap),
                       reads=[osb.b, P.ident_bf.b], writes=[ptb.b], inc=(q == 3))
            sch.op("act", "copy", dict(out=oT.ap[:, kc2 * 512:(kc2 + 1) * 512], in_=ptv[:, 0:512]), reads=[ptb.b], writes=[oT.b])
        yield
        for half in range(2):
            pob = pout[half]
            for kc in range(8):
                sch.op("pe", "matmul", dict(out=pob.ap[:, 0:512], lhsT=oT.ap[:, kc * 128:(kc + 1) * 128],
                                            rhs=w_out.ap[:, kc * D + half * 512:kc * D + (half + 1) * 512], start=(kc == 0), stop=(kc == 7)),
                       reads=[oT.b, w_out.b], writes=[pob.b], inc=(kc == 7))
            sch.op("act", "activation", dict(out=junk2.ap[:, 0:512], in_=pob.ap[:, 0:512], func=AF.Square, accum_out=rs2.ap[:, half:half + 1]),
                   reads=[pob.b], writes=[junk2.b, rs2.b])
            yield
        sch.op("dve", "tensor_tensor", dict(out=rs2.ap[:, 0:1], in0=rs2.ap[:, 0:1], in1=rs2.ap[:, 1:2], op=ALU.add), reads=[rs2.b], writes=[rs2.b])
        r1 = T(rs2.ap[:, 0:1], rs2.b)
        rstd_ln(r1, r1)
        for half in range(2):
            sch.op("dve", "scalar_tensor_tensor", dict(out=yt.ap[:, half * 512:(half + 1) * 512], in0=pout[half].ap[:, 0:512], scalar=rs2.ap[:, 0:1],
                                                       in1=gout.ap[:, half * 512:(half + 1) * 512], op0=ALU.mult, op1=ALU.mult),
                   reads=[pout[half].b, rs2.b, gout.b], writes=[yt.b])
        sch.op("pool", "tensor_tensor", dict(out=yt.ap, in0=yt.ap, in1=h.ap, op=ALU.add), reads=[yt.b, h.b], writes=[yt.b])
        sch.dma("sp", dst[r0:r0 + 128, :], yt.ap, reads=[yt.b])
        yield

    def count_units(gen_fn, qt):
        return None

    def n_front(qt):
        L = (qt + 1) * 128
        nch = (L + 511) // 512
        return 1 + 2 + 10 + 1 + 4 + 4 * nch + (2 + NBIS if L > KSEL else 1) + (qt + 4) // 4
    def n_back(qt):
        return 4 * (qt + 1) + 1 + 1 + 2 + 1

    def run_all(g):
        for _ in g:
            pass

    run_all(front(0))
    for qt in range(NT):
        gb = back(qt)
        if qt + 1 < NT:
            gf = front(qt + 1)
            nb_, nf_ = n_back(qt), n_front(qt + 1)
            ib = i_f = 0
            bdone = fdone = False
            while not (bdone and fdone):
                if not bdone and (fdone or NO_INTERLEAVE or ib * nf_ * FRONT_PRIO <= i_f * nb_):
                    try:
                        next(gb); ib += 1
                    except StopIteration:
                        bdone = True
                elif not fdone:
                    try:
                        next(gf); i_f += 1
                    except StopIteration:
                        fdone = True
        else:
            run_all(gb)
    P.phase_end()


SEQ = 4096
DEPTH = 4
S5_NI = 4


def build_program(S=SEQ, depth=DEPTH):
    P = Prog(S)
    x = P.din("x", [S, D])
    out = P.dout("out", [S, D])
    hbuf = P.dscratch("hbuf", [S, D])
    g = P.din("g", [depth * 4, D])
    s5cst = P.din("s5cst", [128, 130])
    negm = P.din("negm", [128, 128])
    for i in range(depth):
        j = i // 2
        src = x if i == 0 else hbuf
        grow = lambda k: g[i * 4 + k:i * 4 + k + 1, :]
        if i % 2 == 0:
            s5_phase(P, src, hbuf, P.din("s5small_%d" % j, [128, 128]), P.din("s5big_%d" % j, [128, 2048]), s5cst,
                     P.din("s5win_%d" % j, [D, E]), P.din("s5wglu_%d" % j, [E, E]), P.din("s5wout_%d" % j, [E, D]),
                     grow(0), grow(1), NI=S5_NI)
        else:
            dsa_phase(P, src, hbuf, P.din("awin_%d" % j, [D, NCOL]), P.din("awout_%d" % j, [D, D]), negm, grow(0), grow(1))
        dst = out if i == depth - 1 else hbuf
        mlp_phase(P, hbuf, dst, P.din("w1_%d" % i, [D, DFF]), P.din("w2_%d" % i, [DFF, D]), grow(2), grow(3))
    P.sch.barrier()
    P.sch.emit()
    return P


_CACHE = {}


def host_inputs(inputs, depth=DEPTH):
    f = lambda a: np.ascontiguousarray(np.asarray(a), dtype=np.float32)
    shared = {"ident": np.eye(128, dtype=np.float32), "s5cst": s5_consts(), "negm": dsa_consts(),
              "g": f(inputs["norm_g"]).reshape(-1, D)[:depth * 4]}
    for i in range(depth):
        j = i // 2
        shared["w1_%d" % i] = f(inputs["mlp_w1"][i])
        shared["w2_%d" % i] = f(inputs["mlp_w2"][i])
        if i % 2 == 0:
            small, big = s5_host_layout(*[np.asarray(inputs["ssm_" + n][j]) for n in
                                          ["lam_re", "lam_im", "log_dt", "b_re", "b_im", "c_re", "c_im", "d"]])
            shared["s5small_%d" % j] = small
            shared["s5big_%d" % j] = big
            shared["s5win_%d" % j] = f(inputs["ssm_w_in"][j])
            shared["s5wglu_%d" % j] = f(inputs["ssm_w_glu"][j])
            shared["s5wout_%d" % j] = f(inputs["ssm_w_out"][j])
        else:
            wip, wop = dsa_host_layout(np.asarray(inputs["att_w_in"][j]), np.asarray(inputs["att_w_out"][j]))
            shared["awin_%d" % j] = wip
            shared["awout_%d" % j] = wop
    return shared


def kernel(**inputs):
    x = np.asarray(inputs["x"], dtype=np.float32)
    B, S, _ = x.shape
    key = (S, DEPTH)
    if key not in _CACHE:
        _CACHE[key] = build_program(S, DEPTH)
    P = _CACHE[key]
    shared = host_inputs(inputs)
    in_maps = []
    for b in range(B):
        m = dict(shared)
        m["x"] = np.ascontiguousarray(x[b])
        in_maps.append(m)
    res = run_bass_kernel_spmd(P.nc, in_maps, core_ids=list(range(B)))
    return np.stack([np.asarray(r["out"]) for r in res.results], 0).astype(np.float32)
```

```python
import math
import numpy as np
from contextlib import ExitStack
import concourse.bass as bass
import concourse.mybir as mybir
from concourse.bass_utils import run_bass_kernel_spmd

F32 = mybir.dt.float32
BF16 = mybir.dt.bfloat16
I32 = mybir.dt.int32
ALU = mybir.AluOpType
AF = mybir.ActivationFunctionType
AX = mybir.AxisListType

D = 1024
DFF = 4096
EPS = 1e-6
NCORES = 8


class Buf:
    __slots__ = ("w", "r", "excl")

    def __init__(self, excl=False):
        self.w = None
        self.r = {}
        self.excl = excl


class T:
    __slots__ = ("ap", "b")

    def __init__(self, ap, b=None):
        self.ap = ap
        self.b = b if b is not None else Buf()


class Sched:
    ENGS = ("pe", "dve", "act", "pool", "sp")
    QUEUES = ("sp", "pool", "act")

    def __init__(self, nc, st, nslots=12):
        self.nc = nc
        self.sem = {e: st.enter_context(nc.semaphore("c_" + e)) for e in self.ENGS}
        self.cnt = {e: 0 for e in self.ENGS}
        self.seen = {e: {} for e in self.ENGS}
        self.ops = {e: [] for e in self.ENGS}
        self.slots = {}
        self.slot_i = {}
        for q in self.QUEUES:
            self.slots[q] = []
            self.slot_i[q] = 0
            for i in range(nslots):
                key = "d_%s%d" % (q, i)
                self.sem[key] = st.enter_context(nc.semaphore(key))
                self.slots[q].append([key, 0])

    def _deps(self, reads, writes):
        deps = {}

        def add(k, v):
            if deps.get(k, 0) < v:
                deps[k] = v

        for b in reads:
            if b.w is not None:
                add(*b.w)
            if b.excl:
                for k, v in b.r.items():
                    add(k, v)
        for b in writes:
            if b.w is not None:
                add(*b.w)
            for k, v in b.r.items():
                add(k, v)
        return deps

    def _waits(self, eng, deps):
        waits = []
        for k, v in deps.items():
            if k == eng and eng == "pe":
                continue
            if self.seen[eng].get(k, 0) < v:
                self.seen[eng][k] = v
                waits.append((k, v))
        return waits

    def op(self, eng, meth, kw, reads=(), writes=(), inc=True):
        fn = (meth, kw)
        waits = self._waits(eng, self._deps(reads, writes))
        if inc:
            self.cnt[eng] += 1
            tv = self.cnt[eng]
        else:
            tv = self.cnt[eng] + 1
        self.ops[eng].append((waits, fn, eng if inc else None, 1))
        for b in reads:
            if b.r.get(eng, 0) < tv:
                b.r[eng] = tv
        for b in writes:
            b.w = (eng, tv)
            b.r = {}

    def dma(self, q, out, in_, reads=(), writes=(), **kw):
        slot = self.slots[q][self.slot_i[q]]
        self.slot_i[q] = (self.slot_i[q] + 1) % len(self.slots[q])
        deps = self._deps(reads, writes)
        if slot[1] > 0 and deps.get(slot[0], 0) < slot[1]:
            deps[slot[0]] = slot[1]
        waits = self._waits(q, deps)
        slot[1] += 16
        tv = slot[1]
        key = slot[0]
        kw = dict(kw)
        kw["out"] = out
        kw["in_"] = in_
        self.ops[q].append((waits, ("dma_start", kw), key, 16))
        for b in reads:
            if b.r.get(key, 0) < tv:
                b.r[key] = tv
        for b in writes:
            b.w = (key, tv)
            b.r = {}

    def barrier(self):
        allv = {e: self.cnt[e] for e in self.ENGS}
        for q in self.QUEUES:
            for key, v in self.slots[q]:
                if v > 0:
                    allv[key] = v
        for e in self.ENGS:
            waits = []
            for k, v in allv.items():
                if v > 0 and self.seen[e].get(k, 0) < v:
                    self.seen[e][k] = v
                    waits.append((k, v))
            if waits:
                self.ops[e].append((waits, None, None, 0))

    def emit(self):
        names = {"pe": "tensor", "dve": "vector", "act": "scalar", "pool": "gpsimd", "sp": "sync"}
        with self.nc.Block() as block:
            for e in self.ENGS:
                ops = self.ops[e]

                def body(eng, ops=ops):
                    for waits, fn, inckey, incv in ops:
                        for k, v in waits:
                            eng.wait_ge(self.sem[k], v)
                        if fn is None:
                            continue
                        ins = getattr(eng, fn[0])(**fn[1])
                        if inckey is not None:
                            ins.then_inc(self.sem[inckey], incv)

                getattr(block, names[e])(body)


class Arena:
    def __init__(self, base_ap, nbytes):
        self.base = base_ap
        self.nbytes = nbytes
        self.off = 0
        self.marks = []

    def alloc(self, cols, dtype=F32, parts=128):
        esz = mybir.dt.size(dtype)
        nb = (cols * esz + 31) // 32 * 32
        assert self.off + nb <= self.nbytes, "arena overflow %d + %d > %d" % (self.off, nb, self.nbytes)
        v = self.base[0:parts, self.off // 4:(self.off + nb) // 4]
        if dtype != F32:
            v = v.bitcast(dtype)
        v = v[:, 0:cols]
        self.off += nb
        return T(v)

    def mark(self):
        self.marks.append(self.off)

    def release(self):
        self.off = self.marks.pop()


class Prog:
    def __init__(self, S, arena_bytes=200 * 1024):
        self.S = S
        self.st = ExitStack()
        st = self.st
        nc = bass.Bass("TRN2", target_bir_lowering=False)
        self.nc = nc
        self.dram = {}
        self.sch = Sched(nc, st)
        base = st.enter_context(nc.sbuf_tensor("arena", [128, arena_bytes // 4], F32))
        self.ar = Arena(base[:, :], arena_bytes)
        self.psum = []
        for i in range(8):
            t = st.enter_context(nc.psum_tensor("ps%d" % i, [128, 512], F32))
            self.psum.append(T(t[:, :], Buf(excl=True)))
        self.ident_bf = self.ar.alloc(128, BF16)
        self.ident_f = self.ar.alloc(128, F32)
        self.eps = self.ar.alloc(1, F32)
        idd = self.din("ident", [128, 128])
        self.sch.dma("sp", self.ident_f.ap, idd, writes=[self.ident_f.b])
        self.sch.op("dve", "tensor_copy", dict(out=self.ident_bf.ap, in_=self.ident_f.ap),
                    reads=[self.ident_f.b], writes=[self.ident_bf.b])
        self.sch.op("dve", "memset", dict(ap=self.eps.ap, constant=EPS), writes=[self.eps.b])

    def din(self, name, shape, dtype=F32):
        t = self.nc.dram_tensor(name, list(shape), dtype, kind="ExternalInput").ap()
        self.dram[name] = t
        return t

    def dout(self, name, shape, dtype=F32):
        t = self.nc.dram_tensor(name, list(shape), dtype, kind="ExternalOutput").ap()
        self.dram[name] = t
        return t

    def dscratch(self, name, shape, dtype=F32):
        t = self.nc.dram_tensor(name, list(shape), dtype, kind="Internal").ap()
        self.dram[name] = t
        return t

    def phase_begin(self):
        self.ar.mark()

    def phase_end(self):
        self.sch.barrier()
        self.ar.release()

    def rstd(self, out, ss, n):
        sch = self.sch
        P = ss.ap.shape[0]
        sch.op("act", "activation", dict(out=out.ap, in_=ss.ap, func=AF.Sqrt, bias=self.eps.ap[0:P, :], scale=1.0 / n),
               reads=[ss.b, self.eps.b], writes=[out.b])
        sch.op("dve", "reciprocal", dict(out=out.ap, in_=out.ap), reads=[out.b], writes=[out.b])


def mlp_phase(P, src, dst, w1d, w2d, gin_row, gout_row):
    sch, ar, S = P.sch, P.ar, P.S
    TT = 256
    P.phase_begin()
    w1 = [ar.alloc(8 * 512, BF16) for _ in range(8)]
    w2 = [ar.alloc(4 * 1024, BF16) for _ in range(8)]
    w1src = w1d.rearrange("(c p) f -> p c f", p=128)
    w2src = w2d.rearrange("(c p) d -> p c d", p=128)
    for i in range(8):
        sch.dma("pool", w1[i].ap.rearrange("p (c f) -> p c f", f=512), w1src[:, :, i * 512:(i + 1) * 512],
                writes=[w1[i].b])
    for i in range(8):
        sch.dma("pool", w2[i].ap.rearrange("p (c d) -> p c d", d=1024), w2src[:, 4 * i:4 * i + 4, :],
                writes=[w2[i].b])
    gin = ar.alloc(D)
    gout = ar.alloc(D)
    sch.dma("sp", gin.ap, gin_row.partition_broadcast(128), writes=[gin.b])
    sch.dma("sp", gout.ap, gout_row.partition_broadcast(128), writes=[gout.b])
    NB = 2
    hs = [[ar.alloc(D) for _ in range(2)] for _ in range(NB)]
    hn = [ar.alloc(D, BF16) for _ in range(2)]
    hnT = [ar.alloc(8 * TT, BF16) for _ in range(NB)]
    ss = [ar.alloc(2) for _ in range(NB)]
    rs = [ar.alloc(2) for _ in range(NB)]
    rs2 = [ar.alloc(2) for _ in range(NB)]
    junk = ar.alloc(D, BF16)
    rr = [ar.alloc(TT) for _ in range(2)]
    aT = [ar.alloc(TT, BF16) for _ in range(2)]
    yt = ar.alloc(D)
    acc = P.psum[0:4]
    pm = P.psum[4:6]
    pt = P.psum[6:8]
    ntile = S // TT

    def load_tile(t):
        for sub in range(2):
            r0 = t * TT + sub * 128
            sch.dma("sp", hs[t % NB][sub].ap, src[r0:r0 + 128, :], writes=[hs[t % NB][sub].b])

    load_tile(0)
    for t in range(ntile):
        pb = t % NB
        h = hs[pb]
        for sub in range(2):
            sch.op("act", "activation", dict(out=junk.ap, in_=h[sub].ap, func=AF.Square,
                                             accum_out=ss[pb].ap[:, sub:sub + 1]),
                   reads=[h[sub].b], writes=[junk.b, ss[pb].b])
        P.rstd(rs[pb], ss[pb], D)
        for sub in range(2):
            sch.op("dve", "scalar_tensor_tensor", dict(out=hn[sub].ap, in0=h[sub].ap, scalar=rs[pb].ap[:, sub:sub + 1],
                                                       in1=gin.ap, op0=ALU.mult, op1=ALU.mult),
                   reads=[h[sub].b, rs[pb].b, gin.b], writes=[hn[sub].b])
        for kc in range(8):
            ptb = pt[kc % 2]
            ptv = ptb.ap.bitcast(BF16)
            for sub in range(2):
                sch.op("pe", "transpose", dict(out=ptv[:, sub * 128:(sub + 1) * 128],
                                               in_=hn[sub].ap[:, kc * 128:(kc + 1) * 128], identity=P.ident_bf.ap),
                       reads=[hn[sub].b, P.ident_bf.b], writes=[ptb.b], inc=(sub == 1))
            if kc % 2 == 0:
                sch.op("act", "copy", dict(out=hnT[pb].ap[:, kc * TT:(kc + 1) * TT], in_=ptv[:, 0:TT]),
                       reads=[ptb.b], writes=[hnT[pb].b])
            else:
                sch.op("dve", "tensor_copy", dict(out=hnT[pb].ap[:, kc * TT:(kc + 1) * TT], in_=ptv[:, 0:TT]),
                       reads=[ptb.b], writes=[hnT[pb].b])
        if t + 1 < ntile:
            load_tile(t + 1)

        def mm1(f):
            pmb = pm[f % 2]
            wt = w1[f // 4]
            c0 = (f % 4) * 128
            for kc in range(8):
                sch.op("pe", "matmul", dict(out=pmb.ap[:, 0:TT], lhsT=wt.ap[:, kc * 512 + c0:kc * 512 + c0 + 128],
                                            rhs=hnT[pb].ap[:, kc * TT:(kc + 1) * TT], start=(kc == 0), stop=(kc == 7)),
                       reads=[wt.b, hnT[pb].b], writes=[pmb.b], inc=(kc == 7))
            sch.op("act", "activation", dict(out=rr[f % 2].ap, in_=pmb.ap[:, 0:TT], func=AF.Relu),
                   reads=[pmb.b], writes=[rr[f % 2].b])
            sch.op("dve", "tensor_tensor", dict(out=aT[f % 2].ap, in0=rr[f % 2].ap, in1=rr[f % 2].ap, op=ALU.mult),
                   reads=[rr[f % 2].b], writes=[aT[f % 2].b])

        def mm2(f):
            wt = w2[f // 4]
            o = (f % 4) * 1024
            for sub in range(2):
                for half in range(2):
                    a = acc[sub * 2 + half]
                    sch.op("pe", "matmul", dict(out=a.ap[:, 0:512], lhsT=aT[f % 2].ap[:, sub * 128:(sub + 1) * 128],
                                                rhs=wt.ap[:, o + half * 512:o + (half + 1) * 512],
                                                start=(f == 0), stop=(f == 31)),
                           reads=[aT[f % 2].b, wt.b], writes=[a.b], inc=(f == 31 or (sub == 1 and half == 1)))

        mm1(0)
        for f in range(32):
            if f + 1 < 32:
                mm1(f + 1)
            mm2(f)
        for sub in range(2):
            for half in range(2):
                a = acc[sub * 2 + half]
                sch.op("act", "activation", dict(out=junk.ap[:, 0:512], in_=a.ap[:, 0:512], func=AF.Square,
                                                 accum_out=rs2[pb].ap[:, half:half + 1]),
                       reads=[a.b], writes=[junk.b, rs2[pb].b])
            sch.op("dve", "tensor_tensor", dict(out=rs2[pb].ap[:, 0:1], in0=rs2[pb].ap[:, 0:1],
                                                in1=rs2[pb].ap[:, 1:2], op=ALU.add),
                   reads=[rs2[pb].b], writes=[rs2[pb].b])
            r1 = T(rs2[pb].ap[:, 0:1], rs2[pb].b)
            P.rstd(r1, r1, D)
            for half in range(2):
                a = acc[sub * 2 + half]
                sch.op("dve", "scalar_tensor_tensor", dict(
                    out=yt.ap[:, half * 512:(half + 1) * 512], in0=a.ap[:, 0:512], scalar=rs2[pb].ap[:, 0:1],
                    in1=gout.ap[:, half * 512:(half + 1) * 512], op0=ALU.mult, op1=ALU.mult),
                    reads=[a.b, rs2[pb].b, gout.b], writes=[yt.b])
            sch.op("pool", "tensor_tensor", dict(out=h[sub].ap, in0=yt.ap, in1=h[sub].ap, op=ALU.add),
                   reads=[yt.b, h[sub].b], writes=[h[sub].b])
            r0 = t * TT + sub * 128
            sch.dma("sp", dst[r0:r0 + 128, :], h[sub].ap, reads=[h[sub].b])
    P.phase_end()


G, PST, CH, E = 32, 64, 16, 512
TWO_PI_S = 6.2831845


def s5_host_layout(lam_re, lam_im, log_dt, b_re, b_im, c_re, c_im, d_skip):
    lr = np.concatenate([lam_re.T, lam_re.T], 0)
    li = np.concatenate([lam_im.T, lam_im.T], 0)
    ld = np.broadcast_to(log_dt[None, :], (128, G))
    bre = b_re.transpose(1, 0, 2)
    bim = b_im.transpose(1, 0, 2)
    cre = c_re.transpose(2, 0, 1)
    cim = c_im.transpose(2, 0, 1)
    B1 = np.concatenate([bre, bim], 0).reshape(128, G * CH)
    B2 = np.concatenate([bim, bre], 0).reshape(128, G * CH)
    C1 = np.concatenate([cre, cim], 0).reshape(128, G * CH)
    C2 = np.concatenate([cim, cre], 0).reshape(128, G * CH)
    dcol = np.tile(d_skip.T, (8, 1))
    small = np.concatenate([lr, li, ld, dcol], 1)
    big = np.concatenate([B1, B2, C1, C2], 1)
    return np.ascontiguousarray(small, dtype=np.float32), np.ascontiguousarray(big, dtype=np.float32)


def s5_consts():
    sg = np.ones((128, 2), np.float32)
    sg[64:, 0] = -1.0
    sg[:64, 1] = -1.0
    j = np.arange(128) // 16
    mask = (j[None, :] >= j[:, None]).astype(np.float32)
    return np.concatenate([sg, mask], 1)


def s5_phase(P, src, dst, small_d, big_d, cst_d, w_in_d, w_glu_d, w_out_d, gin_row, gout_row, NI=2):
    sch, ar, S = P.sch, P.ar, P.S
    KP = S // 8
    NM = KP // NI
    NKB = KP // 128
    P.phase_begin()
    small = ar.alloc(128)
    big = ar.alloc(2048)
    cst = ar.alloc(130)
    sch.dma("sp", small.ap, small_d, writes=[small.b])
    sch.dma("sp", big.ap, big_d, writes=[big.b])
    sch.dma("sp", cst.ap, cst_d, writes=[cst.b])
    gin = ar.alloc(D)
    gout = ar.alloc(D)
    sch.dma("sp", gin.ap, gin_row.partition_broadcast(128), writes=[gin.b])
    sch.dma("sp", gout.ap, gout_row.partition_broadcast(128), writes=[gout.b])
    sg = cst.ap[:, 0:1]
    nsg = cst.ap[:, 1:2]
    cmask = cst.ap[:, 2:130]
    lr = small.ap[:, 0:32]
    li = small.ap[:, 32:64]
    ld = small.ap[:, 64:96]
    dcol = small.ap[:, 96:128]
    B1 = big.ap[:, 0:512].rearrange("p (g c) -> p g c", c=CH)
    B2 = big.ap[:, 512:1024].rearrange("p (g c) -> p g c", c=CH)
    C1 = big.ap[:, 1024:1536].rearrange("p (g c) -> p g c", c=CH)
    C2 = big.ap[:, 1536:2048].rearrange("p (g c) -> p g c", c=CH)

    EC = list(range(-7, 8 * NI + 1))
    EB = [8 * (NI - 1 - i) + 7 - j for i in range(NI) for j in range(8)]
    EX = EC + EB
    NE = len(EX)
    NC_ = len(EC)
    tb = T(None)
    def tl(cols, dt=F32):
        t = ar.alloc(cols, dt)
        return t.ap
    Pr = tl(32 * NE); Pi = tl(32 * NE); nsPi = tl(32 * NE); sPi = tl(32 * NE)
    Bb1 = tl(512); Bb2 = tl(512)
    zT = ar.alloc(4 * 8 * KP, BF16)
    zTv = zT.ap.rearrange("p (e j k) -> p e j k", j=8, k=KP)
    off_after_zT = ar.off
    U = ar.alloc(G * KP, BF16)
    Uv = U.ap.rearrange("p (g k) -> p g k", k=KP)
    ar.mark()
    dt_ = tl(32); lrdt = tl(32); lidt = tl(32)
    argm = tl(32 * NE); arga = tl(32 * NE); argi = tl(32 * NE, I32); argf = tl(32 * NE)
    m1 = tl(32 * NE)
    R = [small.b, big.b, cst.b, tb.b]
    W = [tb.b]
    def dve(meth, **kw):
        sch.op("dve", meth, kw, reads=R, writes=W)
    def act(**kw):
        sch.op("act", "activation", kw, reads=R, writes=W)
    act(out=dt_, in_=ld, func=AF.Exp)
    dve("tensor_tensor", out=lrdt, in0=lr, in1=dt_, op=ALU.mult)
    dve("tensor_tensor", out=lidt, in0=li, in1=dt_, op=ALU.mult)
    v3 = lambda a: a.rearrange("p (g n) -> p g n", n=NE)
    for i, n in enumerate(EX):
        dve("tensor_scalar", out=v3(argm)[:, :, i], in0=lrdt, scalar1=float(n), scalar2=None, op0=ALU.mult)
        dve("tensor_scalar", out=v3(arga)[:, :, i], in0=lidt, scalar1=float(n) / (2 * math.pi), scalar2=None, op0=ALU.mult)
    act(out=argm, in_=argm, func=AF.Exp)
    dve("tensor_copy", out=argi, in_=arga)
    dve("tensor_copy", out=argf, in_=argi)
    dve("tensor_tensor", out=arga, in0=arga, in1=argf, op=ALU.subtract)
    def wrap(y):
        dve("tensor_scalar", out=m1, in0=y, scalar1=0.5, scalar2=None, op0=ALU.is_gt)
        dve("tensor_tensor", out=y, in0=y, in1=m1, op=ALU.subtract)
        dve("tensor_scalar", out=m1, in0=y, scalar1=-0.5, scalar2=None, op0=ALU.is_lt)
        dve("tensor_tensor", out=y, in0=y, in1=m1, op=ALU.add)
    wrap(arga)
    act(out=Pi, in_=arga, func=AF.Sin, scale=TWO_PI_S)
    dve("tensor_scalar", out=arga, in0=arga, scalar1=0.25, scalar2=None, op0=ALU.add)
    wrap(arga)
    act(out=Pr, in_=arga, func=AF.Sin, scale=TWO_PI_S)
    dve("tensor_tensor", out=Pr, in0=Pr, in1=argm, op=ALU.mult)
    dve("tensor_tensor", out=Pi, in0=Pi, in1=argm, op=ALU.mult)
    dve("tensor_scalar", out=nsPi, in0=Pi, scalar1=nsg, scalar2=None, op0=ALU.mult)
    dve("tensor_scalar", out=sPi, in0=Pi, scalar1=sg, scalar2=None, op0=ALU.mult)
    i1 = EC.index(1)
    nr = tl(32); den = tl(32); fr = tl(32); fi = tl(32); t32 = tl(32); nsfi = tl(32); sfi = tl(32)
    dve("tensor_scalar", out=nr, in0=v3(Pr)[:, :, i1], scalar1=-1.0, scalar2=None, op0=ALU.add)
    ni_ = v3(Pi)[:, :, i1]
    dve("tensor_tensor", out=den, in0=lr, in1=lr, op=ALU.mult)
    dve("tensor_tensor", out=t32, in0=li, in1=li, op=ALU.mult)
    dve("tensor_tensor", out=den, in0=den, in1=t32, op=ALU.add)
    dve("reciprocal", out=den, in_=den)
    dve("tensor_tensor", out=fr, in0=nr, in1=lr, op=ALU.mult)
    dve("tensor_tensor", out=t32, in0=ni_, in1=li, op=ALU.mult)
    dve("tensor_tensor", out=fr, in0=fr, in1=t32, op=ALU.add)
    dve("tensor_tensor", out=fr, in0=fr, in1=den, op=ALU.mult)
    dve("tensor_tensor", out=fi, in0=ni_, in1=lr, op=ALU.mult)
    dve("tensor_tensor", out=t32, in0=nr, in1=li, op=ALU.mult)
    dve("tensor_tensor", out=fi, in0=fi, in1=t32, op=ALU.subtract)
    dve("tensor_tensor", out=fi, in0=fi, in1=den, op=ALU.mult)
    dve("tensor_scalar", out=nsfi, in0=fi, scalar1=nsg, scalar2=None, op0=ALU.mult)
    dve("tensor_scalar", out=sfi, in0=fi, scalar1=sg, scalar2=None, op0=ALU.mult)
    tB = tl(512)
    g3 = lambda a: a.rearrange("p (g c) -> p g c", c=CH)
    bc = lambda a: a.unsqueeze(2).broadcast_to([128, 32, CH])
    dve("tensor_tensor", out=g3(Bb1), in0=B1, in1=bc(fr), op=ALU.mult)
    dve("tensor_tensor", out=g3(tB), in0=B2, in1=bc(nsfi), op=ALU.mult)
    dve("tensor_tensor", out=Bb1, in0=Bb1, in1=tB, op=ALU.add)
    dve("tensor_tensor", out=g3(Bb2), in0=B2, in1=bc(fr), op=ALU.mult)
    dve("tensor_tensor", out=g3(tB), in0=B1, in1=bc(sfi), op=ALU.mult)
    dve("tensor_tensor", out=Bb2, in0=Bb2, in1=tB, op=ALU.add)
    iA = EC.index(8 * NI)
    A1 = v3(Pr)[:, :, iA]
    A2 = v3(nsPi)[:, :, iA]
    A2w = v3(sPi)[:, :, iA]

    NB8 = NI * 8
    def ba_tables(g, outst, outsw, tmp):
        o4 = lambda a: a.rearrange("p (n c) -> p n c", c=CH)
        prb = v3(Pr)[:, g, NC_:NE].unsqueeze(2).broadcast_to([128, NB8, CH])
        nspb = v3(nsPi)[:, g, NC_:NE].unsqueeze(2).broadcast_to([128, NB8, CH])
        spb = v3(sPi)[:, g, NC_:NE].unsqueeze(2).broadcast_to([128, NB8, CH])
        b1 = g3(Bb1)[:, g, :].unsqueeze(1).broadcast_to([128, NB8, CH])
        b2 = g3(Bb2)[:, g, :].unsqueeze(1).broadcast_to([128, NB8, CH])
        rw = dict(reads=[tb.b], writes=[outst.b, outsw.b, tmp.b])
        sch.op("dve", "tensor_tensor", dict(out=o4(outst.ap), in0=prb, in1=b1, op=ALU.mult), **rw)
        sch.op("dve", "tensor_tensor", dict(out=o4(tmp.ap), in0=nspb, in1=b2, op=ALU.mult), **rw)
        sch.op("dve", "tensor_tensor", dict(out=outst.ap, in0=outst.ap, in1=tmp.ap, op=ALU.add), **rw)
        if outsw is not outst:
            sch.op("dve", "tensor_tensor", dict(out=o4(outsw.ap), in0=prb, in1=b2, op=ALU.mult), **rw)
            sch.op("dve", "tensor_tensor", dict(out=o4(tmp.ap), in0=spb, in1=b1, op=ALU.mult), **rw)
            sch.op("dve", "tensor_tensor", dict(out=outsw.ap, in0=outsw.ap, in1=tmp.ap, op=ALU.add), **rw)

    sch.barrier()
    ar.release()
    ar.mark()
    w_in = ar.alloc(8 * E, BF16)
    sch.dma("pool", w_in.ap.rearrange("p (c e) -> p c e", e=E), w_in_d.rearrange("(c p) e -> p c e", p=128), writes=[w_in.b])
    hblk = ar.alloc(8 * D)
    hnj = [ar.alloc(D, BF16) for _ in range(2)]
    hnT = [ar.alloc(8 * 128, BF16) for _ in range(2)]
    utok = ar.alloc(8 * E, BF16)
    utv = utok.ap.rearrange("p (g j c) -> p g j c", j=8, c=CH)
    ss = ar.alloc(8); rs = ar.alloc(8)
    junk = ar.alloc(D, BF16)
    pt = P.psum[6:8]
    pu = P.psum[4:6]
    for kb in range(NKB):
        r0 = kb * 1024
        sch.dma("sp", hblk.ap, src[r0:r0 + 1024, :].rearrange("(k j) d -> k (j d)", j=8), writes=[hblk.b])
        for j in range(8):
            sch.op("act", "activation", dict(out=junk.ap, in_=hblk.ap[:, j * D:(j + 1) * D], func=AF.Square,
                                             accum_out=ss.ap[:, j:j + 1]), reads=[hblk.b], writes=[junk.b, ss.b])
        P.rstd(rs, ss, D)
        for j in range(8):
            hn = hnj[j % 2]
            sch.op("dve", "scalar_tensor_tensor", dict(out=hn.ap, in0=hblk.ap[:, j * D:(j + 1) * D],
                                                       scalar=rs.ap[:, j:j + 1], in1=gin.ap, op0=ALU.mult, op1=ALU.mult),
                   reads=[hblk.b, rs.b, gin.b], writes=[hn.b])
            hT = hnT[j % 2]
            for kc2 in range(2):
                ptb = pt[kc2]
                ptv = ptb.ap.bitcast(BF16)
                for q in range(4):
                    kc = kc2 * 4 + q
                    sch.op("pe", "transpose", dict(out=ptv[:, q * 128:(q + 1) * 128],
                                                   in_=hn.ap[:, kc * 128:(kc + 1) * 128], identity=P.ident_bf.ap),
                           reads=[hn.b, P.ident_bf.b], writes=[ptb.b], inc=(q == 3))
                if kc2 == 0:
                    sch.op("act", "copy", dict(out=hT.ap[:, 0:512], in_=ptv[:, 0:512]), reads=[ptb.b], writes=[hT.b])
                else:
                    sch.op("dve", "tensor_copy", dict(out=hT.ap[:, 512:1024], in_=ptv[:, 0:512]), reads=[ptb.b], writes=[hT.b])
            pub = pu[j % 2]
            for kc in range(8):
                sch.op("pe", "matmul", dict(out=pub.ap[:, 0:E], lhsT=hT.ap[:, kc * 128:(kc + 1) * 128],
                                            rhs=w_in.ap[:, kc * E:(kc + 1) * E], start=(kc == 0), stop=(kc == 7)),
                       reads=[hT.b, w_in.b], writes=[pub.b], inc=(kc == 7))
            sch.op("act", "copy", dict(out=utv[:, :, j, :], in_=pub.ap[:, 0:E].rearrange("p (g c) -> p g c", c=CH)),
                   reads=[pub.b], writes=[utok.b])
        for g4 in range(8):
            ptb = pt[g4 % 2]
            ptv = ptb.ap.bitcast(BF16)
            for q in range(4):
                g = g4 * 4 + q
                sch.op("pe", "transpose", dict(out=ptv[:, q * 128:(q + 1) * 128], in_=utok.ap[:, g * 128:(g + 1) * 128],
                                               identity=P.ident_bf.ap),
                       reads=[utok.b, P.ident_bf.b], writes=[ptb.b], inc=(q == 3))
            eng = ("act", "copy") if g4 % 2 == 0 else ("dve", "tensor_copy")
            sch.op(eng[0], eng[1], dict(out=Uv[:, g4 * 4:g4 * 4 + 4, kb * 128:(kb + 1) * 128],
                                        in_=ptv[:, 0:512].rearrange("p (g k) -> p g k", k=128)),
                   reads=[ptb.b], writes=[U.b])

    sch.barrier()
    ar.release()
    Sst = ar.alloc(G * NM, BF16)
    Ssw = ar.alloc(G * NM, BF16)
    Sstv = Sst.ap.rearrange("p (g m) -> p g m", m=NM)
    Sswv = Ssw.ap.rearrange("p (g m) -> p g m", m=NM)
    bast = [ar.alloc(NB8 * CH) for _ in range(2)]
    basw = [ar.alloc(NB8 * CH) for _ in range(2)]
    btmp = ar.alloc(NB8 * CH)
    wsst = [ar.alloc(NI * 128, BF16) for _ in range(2)]
    wssw = [ar.alloc(NI * 128, BF16) for _ in range(2)]
    ps_s = P.psum[0:4]
    for g in range(G):
        pb = g % 2
        ba_tables(g, bast[pb], basw[pb], btmp)
        for which, (ba, ws) in enumerate(((bast[pb], wsst[pb]), (basw[pb], wssw[pb]))):
            ptb = pt[which]
            for i in range(NI):
                sch.op("pe", "transpose", dict(out=ptb.ap[:, i * 128:(i + 1) * 128], in_=ba.ap[:, i * 128:(i + 1) * 128],
                                               identity=P.ident_f.ap),
                       reads=[ba.b, P.ident_f.b], writes=[ptb.b], inc=(i == NI - 1))
            sch.op("act", "copy", dict(out=ws.ap, in_=ptb.ap[:, 0:NI * 128]), reads=[ptb.b], writes=[ws.b])
        for which, (ws, Sv, Sb) in enumerate(((wsst[pb], Sstv, Sst), (wssw[pb], Sswv, Ssw))):
            psb = ps_s[(g % 2) * 2 + which]
            for i in range(NI):
                rhs = Uv[:, g, :].rearrange("p (m i) -> p m i", i=NI)[:, :, i]
                sch.op("pe", "matmul", dict(out=psb.ap[:, 0:NM], lhsT=ws.ap[:, i * 128:(i + 1) * 128], rhs=rhs,
                                            start=(i == 0), stop=(i == NI - 1)),
                       reads=[ws.b, U.b], writes=[psb.b], inc=(i == NI - 1))
            sch.op("dve", "tensor_copy", dict(out=Sv[:, g, :], in_=psb.ap[:, 0:NM]), reads=[psb.b], writes=[Sb.b])

    Xs = ar.alloc(32); Xw = ar.alloc(32); t1 = ar.alloc(32); t2 = ar.alloc(32)
    sb = T(None)
    rw = dict(reads=[tb.b, sb.b, Sst.b, Ssw.b], writes=[sb.b])
    sch.op("dve", "memset", dict(ap=Xs.ap, constant=0.0), **rw)
    sch.op("dve", "memset", dict(ap=Xw.ap, constant=0.0), **rw)
    for m in range(NM):
        tt = lambda **kw: sch.op("dve", "tensor_tensor", kw, **rw)
        tt(out=t1.ap, in0=Xs.ap, in1=A1, op=ALU.mult)
        tt(out=t2.ap, in0=Xw.ap, in1=A2, op=ALU.mult)
        tt(out=t1.ap, in0=t1.ap, in1=t2.ap, op=ALU.add)
        tt(out=t2.ap, in0=Xw.ap, in1=A1, op=ALU.mult)
        tt(out=Xw.ap, in0=Xs.ap, in1=A2w, op=ALU.mult)
        tt(out=Xw.ap, in0=Xw.ap, in1=t2.ap, op=ALU.add)
        tt(out=Xw.ap, in0=Xw.ap, in1=Sswv[:, :, m], op=ALU.add)
        tt(out=Xs.ap, in0=t1.ap, in1=Sstv[:, :, m], op=ALU.add)
        sch.op("dve", "tensor_copy", dict(out=Sstv[:, :, m], in_=Xs.ap), reads=[sb.b], writes=[sb.b, Sst.b])

    cast = [ar.alloc((NC_) * CH) for _ in range(2)]
    ctmp = ar.alloc((NC_) * CH)
    L1 = [ar.alloc(128) for _ in range(2)]
    Tg = [ar.alloc(NI * 128, BF16) for _ in range(2)]
    WY = [ar.alloc(NI * 128, BF16) for _ in range(2)]
    tmask = ar.alloc(128)
    zg = [ar.alloc(KP, BF16) for _ in range(2)]
    gx2 = ar.alloc(KP); gt = ar.alloc(KP)
    psy = P.psum[0:2]
    pst = P.psum[2:4]
    for g in range(G):
        pb = g % 2
        ba_tables(g, bast[pb], bast[pb], btmp)
        o4 = lambda a: a.rearrange("p (n c) -> p n c", c=CH)
        prc = v3(Pr)[:, g, 0:NC_].unsqueeze(2).broadcast_to([128, NC_, CH])
        nspc = v3(nsPi)[:, g, 0:NC_].unsqueeze(2).broadcast_to([128, NC_, CH])
        c1 = C1[:, g, :].unsqueeze(1).broadcast_to([128, NC_, CH])
        c2 = C2[:, g, :].unsqueeze(1).broadcast_to([128, NC_, CH])
        rwc = dict(reads=[tb.b, big.b], writes=[cast[pb].b, ctmp.b])
        sch.op("dve", "tensor_tensor", dict(out=o4(cast[pb].ap), in0=prc, in1=c1, op=ALU.mult), **rwc)
        sch.op("dve", "tensor_tensor", dict(out=o4(ctmp.ap), in0=nspc, in1=c2, op=ALU.mult), **rwc)
        sch.op("dve", "tensor_tensor", dict(out=cast[pb].ap, in0=cast[pb].ap, in1=ctmp.ap, op=ALU.add), **rwc)
        sch.op("dve", "tensor_scalar", dict(out=L1[pb].ap, in0=bast[pb].ap[:, (NI - 1) * 128:NI * 128], scalar1=sg, scalar2=None, op0=ALU.mult),
               reads=[bast[pb].b, cst.b], writes=[L1[pb].b])
        ptb = pst[pb]
        for dl in range(NI):
            sch.op("pe", "matmul", dict(out=ptb.ap[:, dl * 128:(dl + 1) * 128], lhsT=L1[pb].ap,
                                        rhs=cast[pb].ap[:, dl * 128:(dl + 1) * 128], start=True, stop=True),
                   reads=[L1[pb].b, cast[pb].b], writes=[ptb.b], inc=(dl == NI - 1))
        sch.op("dve", "tensor_tensor", dict(out=tmask.ap, in0=ptb.ap[:, 0:128], in1=cmask, op=ALU.mult),
               reads=[ptb.b, cst.b], writes=[tmask.b])
        sch.op("dve", "scalar_tensor_tensor", dict(out=Tg[pb].ap[:, 0:128], in0=P.ident_f.ap, scalar=dcol[:, g:g + 1], in1=tmask.ap,
                                                   op0=ALU.mult, op1=ALU.add),
               reads=[tmask.b, P.ident_f.b, small.b], writes=[Tg[pb].b])
        if NI > 1:
            sch.op("act", "copy", dict(out=Tg[pb].ap[:, 128:NI * 128], in_=ptb.ap[:, 128:NI * 128]), reads=[ptb.b], writes=[Tg[pb].b])
        sch.op("dve", "tensor_scalar", dict(out=WY[pb].ap, in0=cast[pb].ap[:, 128:128 + NI * 128], scalar1=sg, scalar2=None, op0=ALU.mult),
               reads=[cast[pb].b, cst.b], writes=[WY[pb].b])
        yb = psy[pb]
        yv = yb.ap[:, 0:KP].rearrange("p (m i) -> p m i", i=NI)
        uv = Uv[:, g, :].rearrange("p (m i) -> p m i", i=NI)
        for dl in range(NI):
            sch.op("pe", "matmul", dict(out=yv[:, :, dl:NI], lhsT=Tg[pb].ap[:, dl * 128:(dl + 1) * 128], rhs=uv[:, :, 0:NI - dl],
                                        start=(dl == 0), stop=False),
                   reads=[Tg[pb].b, U.b], writes=[yb.b], inc=False)
        for i in range(NI):
            sch.op("pe", "matmul", dict(out=yv[:, 1:NM, i], lhsT=WY[pb].ap[:, i * 128:(i + 1) * 128], rhs=Sstv[:, g, 0:NM - 1],
                                        start=False, stop=(i == NI - 1)),
                   reads=[WY[pb].b, Sst.b], writes=[yb.b], inc=(i == NI - 1))
        y = yb.ap[:, 0:KP]
        sch.op("act", "activation", dict(out=gx2.ap, in_=y, func=AF.Square), reads=[yb.b], writes=[gx2.b])
        sch.op("dve", "tensor_scalar", dict(out=gx2.ap, in0=gx2.ap, scalar1=0.044715, scalar2=1.0, op0=ALU.mult, op1=ALU.add),
               reads=[gx2.b], writes=[gx2.b])
        sch.op("dve", "tensor_tensor", dict(out=gt.ap, in0=y, in1=gx2.ap, op=ALU.mult), reads=[yb.b, gx2.b], writes=[gt.b])
        sch.op("act", "activation", dict(out=gt.ap, in_=gt.ap, func=AF.Sigmoid, scale=1.5957691216057308), reads=[gt.b], writes=[gt.b])
        sch.op("dve", "tensor_tensor", dict(out=zg[pb].ap, in0=y, in1=gt.ap, op=ALU.mult), reads=[yb.b, gt.b], writes=[zg[pb].b])
        ech, gi = g // 8, g % 8
        for j in range(8):
            sch.dma("sp" if j % 2 == 0 else "pool", zTv[gi * 16:(gi + 1) * 16, ech, j, :], zg[pb].ap[j * 16:(j + 1) * 16, :],
                    reads=[zg[pb].b], writes=[zT.b])

    sch.barrier()
    ar.off = off_after_zT
    w_glu = ar.alloc(4 * E, BF16)
    w_out = ar.alloc(4 * D, BF16)
    sch.dma("pool", w_glu.ap.rearrange("p (c e) -> p c e", e=E), w_glu_d.rearrange("(c p) e -> p c e", p=128), writes=[w_glu.b])
    sch.dma("pool", w_out.ap.rearrange("p (c e) -> p c e", e=D), w_out_d.rearrange("(c p) e -> p c e", p=128), writes=[w_out.b])
    junk = ar.alloc(D, BF16)
    z2 = [ar.alloc(4 * 512, BF16) for _ in range(2)]
    sgm = ar.alloc(512)
    hres = [ar.alloc(D) for _ in range(2)]
    yt = ar.alloc(D)
    rs2 = ar.alloc(2)
    pg = P.psum[4:6]
    po = P.psum[0:4]
    NKC = KP // 512 if KP >= 512 else 1
    KW = min(KP, 512)
    cnt = 0
    for j in range(8):
        for kc5 in range(NKC):
            k0 = kc5 * KW
            zz = z2[cnt % 2]
            for eo in range(4):
                pgb = pg[eo % 2]
                for ei in range(4):
                    sch.op("pe", "matmul", dict(out=pgb.ap[:, 0:KW], lhsT=w_glu.ap[:, ei * E + eo * 128:ei * E + (eo + 1) * 128],
                                                rhs=zTv[:, ei, j, k0:k0 + KW], start=(ei == 0), stop=(ei == 3)),
                           reads=[w_glu.b, zT.b], writes=[pgb.b], inc=(ei == 3))
                sch.op("act", "activation", dict(out=sgm.ap[:, 0:KW], in_=pgb.ap[:, 0:KW], func=AF.Sigmoid), reads=[pgb.b], writes=[sgm.b])
                sch.op("dve", "tensor_tensor", dict(out=zz.ap[:, eo * 512:eo * 512 + KW], in0=sgm.ap[:, 0:KW], in1=zTv[:, eo, j, k0:k0 + KW], op=ALU.mult),
                       reads=[sgm.b, zT.b], writes=[zz.b])
            for kq in range(KW // 128):
                kk = k0 + kq * 128
                hb = hres[kq % 2]
                rows = src.rearrange("(k j) d -> j k d", j=8)[j, kk:kk + 128, :]
                orows = dst.rearrange("(k j) d -> j k d", j=8)[j, kk:kk + 128, :]
                sch.dma("sp", hb.ap, rows, writes=[hb.b])
                for half in range(2):
                    pob = po[(kq % 2) * 2 + half]
                    for ei in range(4):
                        sch.op("pe", "matmul", dict(out=pob.ap[:, 0:512], lhsT=zz.ap[:, ei * 512 + kq * 128:ei * 512 + (kq + 1) * 128],
                                                    rhs=w_out.ap[:, ei * D + half * 512:ei * D + (half + 1) * 512],
                                                    start=(ei == 0), stop=(ei == 3)),
                               reads=[zz.b, w_out.b], writes=[pob.b], inc=(ei == 3))
                    sch.op("act", "activation", dict(out=junk.ap[:, 0:512], in_=pob.ap[:, 0:512], func=AF.Square,
                                                     accum_out=rs2.ap[:, half:half + 1]), reads=[pob.b], writes=[junk.b, rs2.b])
                sch.op("dve", "tensor_tensor", dict(out=rs2.ap[:, 0:1], in0=rs2.ap[:, 0:1], in1=rs2.ap[:, 1:2], op=ALU.add),
                       reads=[rs2.b], writes=[rs2.b])
                r1 = T(rs2.ap[:, 0:1], rs2.b)
                P.rstd(r1, r1, D)
                for half in range(2):
                    pob = po[(kq % 2) * 2 + half]
                    sch.op("dve", "scalar_tensor_tensor", dict(out=yt.ap[:, half * 512:(half + 1) * 512], in0=pob.ap[:, 0:512],
                                                               scalar=rs2.ap[:, 0:1], in1=gout.ap[:, half * 512:(half + 1) * 512],
                                                               op0=ALU.mult, op1=ALU.mult),
                           reads=[pob.b, rs2.b, gout.b], writes=[yt.b])
                sch.op("pool", "tensor_tensor", dict(out=hb.ap, in0=yt.ap, in1=hb.ap, op=ALU.add), reads=[yt.b, hb.b], writes=[hb.b])
                sch.dma("sp", orows, hb.ap, reads=[hb.b])
            cnt += 1
    P.phase_end()


HP = [0, 4, 1, 5, 2, 6, 3, 7, 8, 12, 9, 13, 10, 14, 11, 15]
QO, KO, QIO, KIO, VO, WIO, NCOL = 0, 1024, 1280, 1792, 1920, 2176, 2184
NEG = -1.0e30
NBIS = 22
MASK_BIG = 29952.0
import os
NO_INTERLEAVE = bool(int(os.environ.get('NO_INTERLEAVE', '0')))
DVE_SHARE = float(os.environ.get('DVE_SHARE', '0.5'))
FRONT_PRIO = float(os.environ.get('FRONT_PRIO', '1.0'))
NO_POOL = bool(int(os.environ.get('NO_POOL', '1')))


def dsa_host_layout(w_in, w_out):
    q = w_in[:, 0:1024].reshape(1024, 16, 64)[:, HP, :].reshape(1024, 1024)
    k = w_in[:, 1024:1280]
    v = w_in[:, 1280:1536]
    qi = w_in[:, 1536:2048]
    ki = w_in[:, 2048:2112]
    wi = w_in[:, 2112:2120]
    wip = np.concatenate([q, k, qi, ki, ki, v, wi], 1)
    wop = w_out.reshape(16, 64, 1024)[HP].reshape(1024, 1024)
    return np.ascontiguousarray(wip, dtype=np.float32), np.ascontiguousarray(wop, dtype=np.float32)


def dsa_consts():
    r = np.arange(128)
    negm = np.where(r[None, :] > r[:, None], NEG, 0.0).astype(np.float32)
    return negm


def dsa_phase(P, src, dst, w_in_d, w_out_d, negm_d, gin_row, gout_row):
    sch, ar, S = P.sch, P.ar, P.S
    NT = S // 128
    KSEL = min(256, S // 4)
    P.phase_begin()
    w_in = ar.alloc(8 * NCOL, BF16)
    w_out = ar.alloc(8 * D, BF16)
    wiv = w_in.ap.rearrange("p (c e) -> p c e", e=NCOL)
    wsrc = w_in_d.rearrange("(c p) e -> p c e", p=128)
    for c0 in range(0, NCOL, 512):
        c1 = min(NCOL, c0 + 512)
        sch.dma("pool", wiv[:, :, c0:c1], wsrc[:, :, c0:c1], writes=[w_in.b])
    sch.dma("pool", w_out.ap.rearrange("p (c e) -> p c e", e=D), w_out_d.rearrange("(c p) e -> p c e", p=128), writes=[w_out.b])
    gin = ar.alloc(D); gout = ar.alloc(D); negm = ar.alloc(128)
    sch.dma("sp", gin.ap, gin_row.partition_broadcast(128), writes=[gin.b])
    sch.dma("sp", gout.ap, gout_row.partition_broadcast(128), writes=[gout.b])
    sch.dma("sp", negm.ap, negm_d, writes=[negm.b])
    cosT = ar.alloc(NT * 32); sinT = ar.alloc(NT * 32)
    ar.mark()
    tb = T(None)
    posi = ar.alloc(NT, I32); posf = ar.alloc(NT); fi_ = ar.alloc(32, I32); invf = ar.alloc(32)
    ang = ar.alloc(NT * 32); angi = ar.alloc(NT * 32, I32); angf = ar.alloc(NT * 32); m1 = ar.alloc(NT * 32)
    rw = dict(reads=[tb.b], writes=[tb.b])
    sch.op("pool", "iota", dict(out=posi.ap, pattern=[[128, NT]], base=0, channel_multiplier=1), **rw)
    sch.op("pool", "iota", dict(out=fi_.ap, pattern=[[1, 32]], base=0, channel_multiplier=0), **rw)
    dve = lambda meth, **kw: sch.op("dve", meth, kw, **rw)
    dve("tensor_copy", out=posf.ap, in_=posi.ap)
    dve("tensor_copy", out=invf.ap, in_=fi_.ap)
    sch.op("act", "activation", dict(out=invf.ap, in_=invf.ap, func=AF.Exp, scale=-math.log(10000.0) / 32.0), **rw)
    dve("tensor_scalar", out=invf.ap, in0=invf.ap, scalar1=1.0 / (2 * math.pi), scalar2=None, op0=ALU.mult)
    a3 = lambda t: t.ap.rearrange("p (q i) -> p q i", i=32)
    dve("tensor_tensor", out=a3(ang), in0=posf.ap.unsqueeze(2).broadcast_to([128, NT, 32]),
        in1=invf.ap.unsqueeze(1).broadcast_to([128, NT, 32]), op=ALU.mult)
    dve("tensor_copy", out=angi.ap, in_=ang.ap)
    dve("tensor_copy", out=angf.ap, in_=angi.ap)
    dve("tensor_tensor", out=ang.ap, in0=ang.ap, in1=angf.ap, op=ALU.subtract)
    def wrap():
        dve("tensor_scalar", out=m1.ap, in0=ang.ap, scalar1=0.5, scalar2=None, op0=ALU.is_gt)
        dve("tensor_tensor", out=ang.ap, in0=ang.ap, in1=m1.ap, op=ALU.subtract)
        dve("tensor_scalar", out=m1.ap, in0=ang.ap, scalar1=-0.5, scalar2=None, op0=ALU.is_lt)
        dve("tensor_tensor", out=ang.ap, in0=ang.ap, in1=m1.ap, op=ALU.add)
    wrap()
    sch.op("act", "activation", dict(out=sinT.ap, in_=ang.ap, func=AF.Sin, scale=TWO_PI_S), reads=[tb.b], writes=[tb.b, sinT.b])
    dve("tensor_scalar", out=ang.ap, in0=ang.ap, scalar1=0.25, scalar2=None, op0=ALU.add)
    wrap()
    sch.op("act", "activation", dict(out=cosT.ap, in_=ang.ap, func=AF.Sin, scale=TWO_PI_S), reads=[tb.b], writes=[tb.b, cosT.b])
    sch.barrier()
    ar.release()
    cos3 = cosT.ap.rearrange("p (q i) -> p q i", i=32)
    sin3 = sinT.ap.rearrange("p (q i) -> p q i", i=32)

    kT = ar.alloc(2 * S, BF16)
    kTv = kT.ap.rearrange("p (b s) -> p b s", s=S)
    kiT = ar.alloc(S, BF16)
    V = ar.alloc(NT * 4 * 65, BF16)
    Vv = V.ap.rearrange("p (t n e) -> p t n e", n=4, e=65)
    kT_b = [Buf() for _ in range(NT)]
    kiT_b = [Buf() for _ in range(NT)]
    V_b = [Buf() for _ in range(NT)]
    sch.op("pool", "memset", dict(ap=V.ap, constant=1.0), writes=V_b)
    acc = ar.alloc(S)
    mask = ar.alloc(S, BF16)
    maskT = [ar.alloc(S, BF16) for _ in range(2)]
    negbig = ar.alloc(1)
    sch.op("pool", "memset", dict(ap=negbig.ap, constant=-MASK_BIG), writes=[negbig.b])
    hq = [ar.alloc(D) for _ in range(2)]
    qT = [ar.alloc(8 * 128, BF16) for _ in range(2)]
    hn = ar.alloc(D, BF16)
    hnT = ar.alloc(8 * 128, BF16)
    rp = ar.alloc(1920, BF16)
    tr = [ar.alloc(256) for _ in range(4)]
    wsb = ar.alloc(8); absw = ar.alloc(8); sgnw = ar.alloc(8)
    qiT = ar.alloc(4 * 128, BF16)
    rr = [ar.alloc(512) for _ in range(2)]
    pT = [ar.alloc(512, BF16) for _ in range(4)]
    osb = ar.alloc(D, BF16)
    oT = ar.alloc(8 * 128, BF16)
    yt = ar.alloc(D)
    junk = ar.alloc(D, BF16)
    junk2 = ar.alloc(512, BF16)
    ss = ar.alloc(1); rs = ar.alloc(1); rs2 = ar.alloc(2)
    lo = ar.alloc(1); hi = ar.alloc(1); mid = ar.alloc(1); cnt = ar.alloc(1); ge = ar.alloc(1); dd = ar.alloc(1)
    rden = ar.alloc(4)
    wt = ar.alloc(NBIS + 2); pow2 = ar.alloc(NBIS + 2); cntB = ar.alloc(1)
    junkA = ar.alloc(S - (int(S * DVE_SHARE) // 64) * 64 + 64, BF16)
    for k in range(NBIS + 2):
        sch.op("pool", "memset", dict(ap=pow2.ap[:, k:k + 1], constant=2.0 ** (-k)), writes=[pow2.b])
    pqk = P.psum[0:2]
    ppv = P.psum[2:4]
    pout = P.psum[0:2]
    pf = P.psum[4:7]
    ptf = P.psum[7]

    def rstd_ln(out, ssq):
        sch.op("act", "activation", dict(out=out.ap, in_=ssq.ap, func=AF.Ln, bias=P.eps.ap, scale=1.0 / D), reads=[ssq.b, P.eps.b], writes=[out.b])
        sch.op("act", "activation", dict(out=out.ap, in_=out.ap, func=AF.Exp, scale=-0.5), reads=[out.b], writes=[out.b])

    def front(qt):
        r0 = qt * 128
        L = (qt + 1) * 128
        h = hq[qt % 2]
        qTq = qT[qt % 2]
        mT = maskT[qt % 2]
        sch.dma("sp", h.ap, src[r0:r0 + 128, :], writes=[h.b])
        sch.op("act", "activation", dict(out=junk.ap, in_=h.ap, func=AF.Square, accum_out=ss.ap), reads=[h.b], writes=[junk.b, ss.b])
        rstd_ln(rs, ss)
        sch.op("dve", "scalar_tensor_tensor", dict(out=hn.ap, in0=h.ap, scalar=rs.ap, in1=gin.ap, op0=ALU.mult, op1=ALU.mult),
               reads=[h.b, rs.b, gin.b], writes=[hn.b])
        yield
        ptv = ptf.ap.bitcast(BF16)
        for kc2 in range(2):
            for q in range(4):
                kc = kc2 * 4 + q
                sch.op("pe", "transpose", dict(out=ptv[:, q * 128:(q + 1) * 128], in_=hn.ap[:, kc * 128:(kc + 1) * 128], identity=P.ident_bf.ap),
                       reads=[hn.b, P.ident_bf.b], writes=[ptf.b], inc=(q == 3))
            sch.op("act", "copy", dict(out=hnT.ap[:, kc2 * 512:(kc2 + 1) * 512], in_=ptv[:, 0:512]), reads=[ptf.b], writes=[hnT.b])
            yield
        cq = cos3[:, qt, :]
        sq = sin3[:, qt, :]
        for nb in range(5):
            pjb = pf[nb % 3]
            c0 = nb * 512
            c1 = min(NCOL, c0 + 512)
            for kc in range(8):
                sch.op("pe", "matmul", dict(out=pjb.ap[:, 0:c1 - c0], lhsT=hnT.ap[:, kc * 128:(kc + 1) * 128], rhs=wiv[:, kc, c0:c1],
                                            start=(kc == 0), stop=(kc == 7)),
                       reads=[hnT.b, w_in.b], writes=[pjb.b], inc=(kc == 7))
            yield
            if nb < 4:
                nh = 8 if nb < 3 else 6
                x = pjb.ap[:, 0:nh * 64].rearrange("p (h t i) -> p h t i", t=2, i=32)
                o = rp.ap[:, nb * 512:nb * 512 + nh * 64].rearrange("p (h t i) -> p h t i", t=2, i=32)
                cb = cq.unsqueeze(1).broadcast_to([128, nh, 32])
                sb_ = sq.unsqueeze(1).broadcast_to([128, nh, 32])
                tv = [t_.ap[:, 0:nh * 32].rearrange("p (h i) -> p h i", i=32) for t_ in tr]
                R_ = [pjb.b, cosT.b, sinT.b]
                sch.op("dve", "tensor_tensor", dict(out=tv[0], in0=x[:, :, 0, :], in1=cb, op=ALU.mult), reads=R_, writes=[tr[0].b])
                sch.op("dve", "tensor_tensor", dict(out=tv[1], in0=x[:, :, 1, :], in1=sb_, op=ALU.mult), reads=R_, writes=[tr[1].b])
                sch.op("dve", "tensor_tensor", dict(out=tv[2], in0=x[:, :, 1, :], in1=cb, op=ALU.mult), reads=R_, writes=[tr[2].b])
                sch.op("dve", "tensor_tensor", dict(out=tv[3], in0=x[:, :, 0, :], in1=sb_, op=ALU.mult), reads=R_, writes=[tr[3].b])
                sch.op("pool", "tensor_tensor", dict(out=o[:, :, 0, :], in0=tv[0], in1=tv[1], op=ALU.subtract),
                       reads=[tr[0].b, tr[1].b], writes=[rp.b])
                sch.op("pool", "tensor_tensor", dict(out=o[:, :, 1, :], in0=tv[2], in1=tv[3], op=ALU.add),
                       reads=[tr[2].b, tr[3].b], writes=[rp.b])
            if nb == 3:
                sch.op("act", "copy", dict(out=Vv[:, qt, 0:2, 0:64], in_=pjb.ap[:, 384:512].rearrange("p (n e) -> p n e", e=64)),
                       reads=[pjb.b], writes=[V_b[qt]])
            if nb == 4:
                sch.op("act", "copy", dict(out=Vv[:, qt, 2:4, 0:64], in_=pjb.ap[:, 0:128].rearrange("p (n e) -> p n e", e=64)),
                       reads=[pjb.b], writes=[V_b[qt]])
                sch.op("dve", "tensor_copy", dict(out=wsb.ap, in_=pjb.ap[:, 128:136]), reads=[pjb.b], writes=[wsb.b])
            yield
        sch.op("dve", "tensor_scalar", dict(out=sgnw.ap, in0=wsb.ap, scalar1=0.0, scalar2=None, op0=ALU.is_ge), reads=[wsb.b], writes=[sgnw.b])
        sch.op("dve", "tensor_scalar", dict(out=sgnw.ap, in0=sgnw.ap, scalar1=2.0, scalar2=-1.0, op0=ALU.mult, op1=ALU.add), reads=[sgnw.b], writes=[sgnw.b])
        sch.op("dve", "tensor_tensor", dict(out=absw.ap, in0=wsb.ap, in1=sgnw.ap, op=ALU.mult), reads=[wsb.b, sgnw.b], writes=[absw.b])
        qiv = rp.ap[:, QIO:QIO + 512].rearrange("p (h e) -> p h e", e=64)
        sch.op("dve", "tensor_tensor", dict(out=qiv, in0=qiv, in1=absw.ap.unsqueeze(2).broadcast_to([128, 8, 64]), op=ALU.mult),
               reads=[rp.b, absw.b], writes=[rp.b])
        yield

        def tr4(cols):
            for q, co in enumerate(cols):
                sch.op("pe", "transpose", dict(out=ptv[:, q * 128:(q + 1) * 128], in_=rp.ap[:, co:co + 128], identity=P.ident_bf.ap),
                       reads=[rp.b, P.ident_bf.b], writes=[ptf.b], inc=(q == len(cols) - 1))
        for i0 in (0, 4):
            tr4([QO + (i0 + b) * 128 for b in range(4)])
            sch.op("act", "copy", dict(out=qTq.ap[:, i0 * 128:(i0 + 4) * 128], in_=ptv[:, 0:512]), reads=[ptf.b], writes=[qTq.b])
            yield
        tr4([QIO + b * 128 for b in range(4)])
        sch.op("act", "copy", dict(out=qiT.ap[:, 0:512], in_=ptv[:, 0:512]), reads=[ptf.b], writes=[qiT.b])
        yield
        tr4([KO, KO + 128, KIO])
        sch.op("dve", "tensor_copy", dict(out=kTv[:, :, r0:r0 + 128], in_=ptv[:, 0:256].rearrange("p (b s) -> p b s", s=128)),
               reads=[ptf.b], writes=[kT_b[qt]])
        sch.op("dve", "tensor_copy", dict(out=kiT.ap[:, r0:r0 + 128], in_=ptv[:, 256:384]), reads=[ptf.b], writes=[kiT_b[qt]])
        yield
        nch = (L + 511) // 512
        ci = 0
        for c in range(nch):
            s0 = c * 512
            wdt = min(512, L - s0)
            kb_ = kiT_b[4 * c:min(NT, 4 * c + 4)]
            for hh in range(8):
                pb = pf[ci % 3]
                ci += 1
                half = (hh % 2) * 64
                sch.op("pe", "matmul", dict(out=pb.ap[:, 0:wdt], lhsT=qiT.ap[half:half + 64, (hh // 2) * 128:(hh // 2 + 1) * 128],
                                            rhs=kiT.ap[half:half + 64, s0:s0 + wdt], start=True, stop=True),
                       reads=[qiT.b] + kb_, writes=[pb.b])
                r = rr[hh % 2]
                sch.op("act", "activation", dict(out=r.ap[:, 0:wdt], in_=pb.ap[:, 0:wdt], func=AF.Relu), reads=[pb.b], writes=[r.b])
                if hh == 0:
                    sch.op("dve", "tensor_scalar", dict(out=acc.ap[:, s0:s0 + wdt], in0=r.ap[:, 0:wdt], scalar1=sgnw.ap[:, 0:1], scalar2=None, op0=ALU.mult),
                           reads=[r.b, sgnw.b], writes=[acc.b])
                else:
                    sch.op("dve", "scalar_tensor_tensor", dict(out=acc.ap[:, s0:s0 + wdt], in0=r.ap[:, 0:wdt], scalar=sgnw.ap[:, hh:hh + 1],
                                                               in1=acc.ap[:, s0:s0 + wdt], op0=ALU.mult, op1=ALU.add),
                           reads=[r.b, sgnw.b, acc.b], writes=[acc.b])
                if hh % 2 == 1:
                    yield
        if L > KSEL:
            sch.op("dve", "tensor_reduce", dict(out=lo.ap, in_=acc.ap[:, 0:L], axis=AX.X, op=ALU.min), reads=[acc.b], writes=[lo.b])
        sch.op("dve", "tensor_tensor", dict(out=acc.ap[:, r0:L], in0=acc.ap[:, r0:L], in1=negm.ap, op=ALU.add), reads=[acc.b, negm.b], writes=[acc.b])
        if L > KSEL:
            sch.op("dve", "tensor_reduce", dict(out=hi.ap, in_=acc.ap[:, 0:L], axis=AX.X, op=ALU.max), reads=[acc.b], writes=[hi.b])
            bb = [lo.b, hi.b, mid.b, cnt.b, ge.b, dd.b, wt.b]
            sch.op("dve", "tensor_tensor", dict(out=dd.ap, in0=hi.ap, in1=lo.ap, op=ALU.subtract), reads=bb, writes=[dd.b])
            sch.op("dve", "tensor_scalar", dict(out=wt.ap, in0=pow2.ap, scalar1=dd.ap, scalar2=None, op0=ALU.mult), reads=bb + [pow2.b], writes=[wt.b])
            sch.op("dve", "tensor_tensor", dict(out=mid.ap, in0=lo.ap, in1=wt.ap[:, 1:2], op=ALU.add), reads=bb, writes=[mid.b])
            yield
            LA = (int(L * DVE_SHARE) // 64) * 64
            nB = L - LA
            for it in range(NBIS):
                sch.op("act", "activation", dict(out=junkA.ap[:, 0:nB], in_=acc.ap[:, LA:L], func=AF.Sign, bias=mid.ap, scale=-1.0,
                                                 accum_out=cntB.ap), reads=[acc.b, mid.b], writes=[junkA.b, cntB.b])
                sch.op("dve", "tensor_scalar", dict(out=mask.ap[:, 0:LA], in0=acc.ap[:, 0:LA], scalar1=mid.ap, scalar2=None, op0=ALU.is_ge, op1=ALU.add,
                                                    accum_out=cnt.ap), reads=[acc.b, mid.b], writes=[mask.b, cnt.b])
                sch.op("dve", "scalar_tensor_tensor", dict(out=ge.ap, in0=cntB.ap, scalar=-0.5, in1=cnt.ap, op0=ALU.mult, op1=ALU.add),
                       reads=[cntB.b, cnt.b], writes=[ge.b])
                sch.op("dve", "scalar_tensor_tensor", dict(out=ge.ap, in0=ge.ap, scalar=float(KSEL) - 0.5 - nB / 2.0, in1=wt.ap[:, it + 1:it + 2],
                                                           op0=ALU.is_ge, op1=ALU.mult), reads=[ge.b, wt.b], writes=[ge.b])
                sch.op("dve", "scalar_tensor_tensor", dict(out=mid.ap, in0=ge.ap, scalar=wt.ap[:, it + 2:it + 3], in1=mid.ap,
                                                           op0=ALU.subtract, op1=ALU.add), reads=[ge.b, wt.b, mid.b], writes=[mid.b])
                yield
            sch.op("dve", "tensor_tensor", dict(out=lo.ap, in0=mid.ap, in1=wt.ap[:, NBIS + 1:NBIS + 2], op=ALU.subtract), reads=bb, writes=[lo.b])
            sch.op("dve", "tensor_scalar", dict(out=mask.ap[:, 0:L], in0=acc.ap[:, 0:L], scalar1=lo.ap, scalar2=None, op0=ALU.is_ge),
                   reads=[acc.b, lo.b], writes=[mask.b])
        else:
            sch.op("dve", "tensor_scalar", dict(out=mask.ap[:, 0:L], in0=acc.ap[:, 0:L], scalar1=-1.0e29, scalar2=None, op0=ALU.is_ge),
                   reads=[acc.b], writes=[mask.b])
        yield
        for i0 in range(0, qt + 1, 4):
            n = min(4, qt + 1 - i0)
            for q in range(n):
                st = i0 + q
                sch.op("pe", "transpose", dict(out=ptv[:, q * 128:(q + 1) * 128], in_=mask.ap[:, st * 128:(st + 1) * 128], identity=P.ident_bf.ap),
                       reads=[mask.b, P.ident_bf.b], writes=[ptf.b], inc=(q == n - 1))
            sch.op("act", "activation", dict(out=mT.ap[:, i0 * 128:(i0 + n) * 128], in_=ptv[:, 0:n * 128], func=AF.Identity,
                                             scale=MASK_BIG, bias=negbig.ap), reads=[ptf.b, negbig.b], writes=[mT.b])
            yield

    def back(qt):
        r0 = qt * 128
        h = hq[qt % 2]
        qTq = qT[qt % 2]
        mTv = maskT[qt % 2].ap.rearrange("p (t q) -> p t q", q=128)
        mTb = maskT[qt % 2].b
        cnt_p = 0
        for n in range(4):
            half = (n % 2) * 64
            a = n // 2
            pob = ppv[n % 2]
            for st in range(qt + 1):
                pq = pqk[cnt_p % 2]
                p_ = pT[cnt_p % 4]
                sch.op("pe", "matmul", dict(out=pq.ap[:, 0:512], lhsT=kTv[half:half + 64, a, st * 128:(st + 1) * 128],
                                            rhs=qTq.ap[half:half + 64, a * 512:(a + 1) * 512], start=True, stop=False),
                       reads=[kT_b[st], qTq.b], writes=[pq.b], inc=False)
                sch.op("pe", "matmul", dict(out=pq.ap[:, 0:512].rearrange("p (g t) -> p g t", t=128), lhsT=P.ident_bf.ap,
                                            rhs=mTv[:, st, :].unsqueeze(1).broadcast_to([128, 4, 128]), start=False, stop=True),
                       reads=[mTb, P.ident_bf.b], writes=[pq.b])
                sch.op("act", "activation", dict(out=p_.ap, in_=pq.ap[:, 0:512], func=AF.Exp, scale=0.125), reads=[pq.b], writes=[p_.b])
                for g in range(4):
                    sch.op("pe", "matmul", dict(out=pob.ap[:, g * 65:(g + 1) * 65], lhsT=p_.ap[:, g * 128:(g + 1) * 128], rhs=Vv[:, st, n, :],
                                                start=(st == 0 and g == 0), stop=(st == qt and g == 3)),
                           reads=[p_.b, V_b[st]], writes=[pob.b], inc=(g == 3))
                cnt_p += 1
                yield
            po3 = pob.ap[:, 0:260].rearrange("p (g e) -> p g e", e=65)
            sch.op("dve", "reciprocal", dict(out=rden.ap, in_=po3[:, :, 64]), reads=[pob.b], writes=[rden.b])
            ov = osb.ap[:, a * 512:(a + 1) * 512].rearrange("p (g two e) -> p g two e", two=2, e=64)[:, :, n % 2, :]
            sch.op("dve", "tensor_tensor", dict(out=ov, in0=po3[:, :, 0:64], in1=rden.ap.unsqueeze(2).broadcast_to([128, 4, 64]), op=ALU.mult),
                   reads=[pob.b, rden.b], writes=[osb.b])
        yield
        for kc2 in range(2):
            ptb = ppv[kc2]
            ptv = ptb.ap.bitcast(BF16)
            for q in range(4):
                kc = kc2 * 4 + q
                sch.op("pe", "transpose", dict(out=ptv[:, q * 128:(q + 1) * 128], in_=osb.ap[:, kc * 128:(kc + 1) * 128], identity=P.ident_bf.ap),
                       reads=[osb.b, P.ident_bf.b], writes=[ptb.b], inc=(q == 3))
            sch.op("act", "copy", dict(out=oT.ap[:, kc2 * 512:(kc2 + 1) * 512], in_=ptv[:, 0:512]), reads=[ptb.b], writes=[oT.b])
        yield
        for half in range(2):
            pob = pout[half]
            for kc in range(8):
                sch.op("pe", "matmul", dict(out=pob.ap[:, 0:512], lhsT=oT.ap[:, kc * 128:(kc + 1) * 128],
                                            rhs=w_out.ap[:, kc * D + half * 512:kc * D + (half + 1) * 512], start=(kc == 0), stop=(kc == 7)),
                       reads=[oT.b, w_out.b], writes=[pob.b], inc=(kc == 7))
            sch.op("act", "activation", dict(out=junk2.ap[:, 0:512], in_=pob.ap[:, 0:512], func=AF.Square, accum_out=rs2.ap[:, half:half + 1]),
                   reads=[pob.b], writes=[junk2.b, rs2.b])
            yield
        sch.op("dve", "tensor_tensor", dict(out=rs2.ap[:, 0:1], in0=rs2.ap[:, 0:1], in1=rs2.ap[:, 1:2], op=ALU.add), reads=[rs2.b], writes=[rs2.b])
        r1 = T(rs2.ap[:, 0:1], rs2.b)
        rstd_ln(r1, r1)
        for half in range(2):
            sch.op("dve", "scalar_tensor_tensor", dict(out=yt.ap[:, half * 512:(half + 1) * 512], in0=pout[half].ap[:, 0:512], scalar=rs2.ap[:, 0:1],
                                                       in1=gout.ap[:, half * 512:(half + 1) * 512], op0=ALU.mult, op1=ALU.mult),
                   reads=[pout[half].b, rs2.b, gout.b], writes=[yt.b])
        sch.op("pool", "tensor_tensor", dict(out=yt.ap, in0=yt.ap, in1=h.ap, op=ALU.add), reads=[yt.b, h.b], writes=[yt.b])
        sch.dma("sp", dst[r0:r0 + 128, :], yt.ap, reads=[yt.b])
        yield

    def count_units(gen_fn, qt):
        return None

    def n_front(qt):
        L = (qt + 1) * 128
        nch = (L + 511) // 512
        return 1 + 2 + 10 + 1 + 4 + 4 * nch + (2 + NBIS if L > KSEL else 1) + (qt + 4) // 4
    def n_back(qt):
        return 4 * (qt + 1) + 1 + 1 + 2 + 1

    def run_all(g):
        for _ in g:
            pass

    run_all(front(0))
    for qt in range(NT):
        gb = back(qt)
        if qt + 1 < NT:
            gf = front(qt + 1)
            nb_, nf_ = n_back(qt), n_front(qt + 1)
            ib = i_f = 0
            bdone = fdone = False
            while not (bdone and fdone):
                if not bdone and (fdone or NO_INTERLEAVE or ib * nf_ * FRONT_PRIO <= i_f * nb_):
                    try:
                        next(gb); ib += 1
                    except StopIteration:
                        bdone = True
                elif not fdone:
                    try:
                        next(gf); i_f += 1
                    except StopIteration:
                        fdone = True
        else:
            run_all(gb)
    P.phase_end()


SEQ = 4096
DEPTH = 4
S5_NI = 4


def build_program(S=SEQ, depth=DEPTH):
    P = Prog(S)
    x = P.din("x", [S, D])
    out = P.dout("out", [S, D])
    hbuf = P.dscratch("hbuf", [S, D])
    g = P.din("g", [depth * 4, D])
    s5cst = P.din("s5cst", [128, 130])
    negm = P.din("negm", [128, 128])
    for i in range(depth):
        j = i // 2
        src = x if i == 0 else hbuf
        grow = lambda k: g[i * 4 + k:i * 4 + k + 1, :]
        if i % 2 == 0:
            s5_phase(P, src, hbuf, P.din("s5small_%d" % j, [128, 128]), P.din("s5big_%d" % j, [128, 2048]), s5cst,
                     P.din("s5win_%d" % j, [D, E]), P.din("s5wglu_%d" % j, [E, E]), P.din("s5wout_%d" % j, [E, D]),
                     grow(0), grow(1), NI=S5_NI)
        else:
            dsa_phase(P, src, hbuf, P.din("awin_%d" % j, [D, NCOL]), P.din("awout_%d" % j, [D, D]), negm, grow(0), grow(1))
        dst = out if i == depth - 1 else hbuf
        mlp_phase(P, hbuf, dst, P.din("w1_%d" % i, [D, DFF]), P.din("w2_%d" % i, [DFF, D]), grow(2), grow(3))
    P.sch.barrier()
    P.sch.emit()
    return P


_CACHE = {}


def host_inputs(inputs, depth=DEPTH):
    f = lambda a: np.ascontiguousarray(np.asarray(a), dtype=np.float32)
    shared = {"ident": np.eye(128, dtype=np.float32), "s5cst": s5_consts(), "negm": dsa_consts(),
              "g": f(inputs["norm_g"]).reshape(-1, D)[:depth * 4]}
    for i in range(depth):
        j = i // 2
        shared["w1_%d" % i] = f(inputs["mlp_w1"][i])
        shared["w2_%d" % i] = f(inputs["mlp_w2"][i])
        if i % 2 == 0:
            small, big = s5_host_layout(*[np.asarray(inputs["ssm_" + n][j]) for n in
                                          ["lam_re", "lam_im", "log_dt", "b_re", "b_im", "c_re", "c_im", "d"]])
            shared["s5small_%d" % j] = small
            shared["s5big_%d" % j] = big
            shared["s5win_%d" % j] = f(inputs["ssm_w_in"][j])
            shared["s5wglu_%d" % j] = f(inputs["ssm_w_glu"][j])
            shared["s5wout_%d" % j] = f(inputs["ssm_w_out"][j])
        else:
            wip, wop = dsa_host_layout(np.asarray(inputs["att_w_in"][j]), np.asarray(inputs["att_w_out"][j]))
            shared["awin_%d" % j] = wip
            shared["awout_%d" % j] = wop
    return shared


def kernel(**inputs):
    x = np.asarray(inputs["x"], dtype=np.float32)
    B, S, _ = x.shape
    key = (S, DEPTH)
    if key not in _CACHE:
        _CACHE[key] = build_program(S, DEPTH)
    P = _CACHE[key]
    shared = host_inputs(inputs)
    in_maps = []
    for b in range(B):
        m = dict(shared)
        m["x"] = np.ascontiguousarray(x[b])
        in_maps.append(m)
    res = run_bass_kernel_spmd(P.nc, in_maps, core_ids=list(range(B)))
    return np.stack([np.asarray(r["out"]) for r in res.results], 0).astype(np.float32)
```

```python
import math
import numpy as np
from contextlib import ExitStack
import concourse.bass as bass
import concourse.mybir as mybir
from concourse.bass_utils import run_bass_kernel_spmd

F32 = mybir.dt.float32
BF16 = mybir.dt.bfloat16
I32 = mybir.dt.int32
ALU = mybir.AluOpType
AF = mybir.ActivationFunctionType
AX = mybir.AxisListType

D = 1024
DFF = 4096
EPS = 1e-6
NCORES = 8


class Buf:
    __slots__ = ("w", "r", "excl")

    def __init__(self, excl=False):
        self.w = None
        self.r = {}
        self.excl = excl


class T:
    __slots__ = ("ap", "b")

    def __init__(self, ap, b=None):
        self.ap = ap
        self.b = b if b is not None else Buf()


class Sched:
    ENGS = ("pe", "dve", "act", "pool", "sp")
    QUEUES = ("sp", "pool", "act")

    def __init__(self, nc, st, nslots=12):
        self.nc = nc
        self.sem = {e: st.enter_context(nc.semaphore("c_" + e)) for e in self.ENGS}
        self.cnt = {e: 0 for e in self.ENGS}
        self.seen = {e: {} for e in self.ENGS}
        self.ops = {e: [] for e in self.ENGS}
        self.slots = {}
        self.slot_i = {}
        for q in self.QUEUES:
            self.slots[q] = []
            self.slot_i[q] = 0
            for i in range(nslots):
                key = "d_%s%d" % (q, i)
                self.sem[key] = st.enter_context(nc.semaphore(key))
                self.slots[q].append([key, 0])

    def _deps(self, reads, writes):
        deps = {}

        def add(k, v):
            if deps.get(k, 0) < v:
                deps[k] = v

        for b in reads:
            if b.w is not None:
                add(*b.w)
            if b.excl:
                for k, v in b.r.items():
                    add(k, v)
        for b in writes:
            if b.w is not None:
                add(*b.w)
            for k, v in b.r.items():
                add(k, v)
        return deps

    def _waits(self, eng, deps):
        waits = []
        for k, v in deps.items():
            if k == eng and eng == "pe":
                continue
            if self.seen[eng].get(k, 0) < v:
                self.seen[eng][k] = v
                waits.append((k, v))
        return waits

    def op(self, eng, meth, kw, reads=(), writes=(), inc=True):
        fn = (meth, kw)
        waits = self._waits(eng, self._deps(reads, writes))
        if inc:
            self.cnt[eng] += 1
            tv = self.cnt[eng]
        else:
            tv = self.cnt[eng] + 1
        self.ops[eng].append((waits, fn, eng if inc else None, 1))
        for b in reads:
            if b.r.get(eng, 0) < tv:
                b.r[eng] = tv
        for b in writes:
            b.w = (eng, tv)
            b.r = {}

    def dma(self, q, out, in_, reads=(), writes=(), **kw):
        slot = self.slots[q][self.slot_i[q]]
        self.slot_i[q] = (self.slot_i[q] + 1) % len(self.slots[q])
        deps = self._deps(reads, writes)
        if slot[1] > 0 and deps.get(slot[0], 0) < slot[1]:
            deps[slot[0]] = slot[1]
        waits = self._waits(q, deps)
        slot[1] += 16
        tv = slot[1]
        key = slot[0]
        kw = dict(kw)
        kw["out"] = out
        kw["in_"] = in_
        self.ops[q].append((waits, ("dma_start", kw), key, 16))
        for b in reads:
            if b.r.get(key, 0) < tv:
                b.r[key] = tv
        for b in writes:
            b.w = (key, tv)
            b.r = {}

    def barrier(self):
        allv = {e: self.cnt[e] for e in self.ENGS}
        for q in self.QUEUES:
            for key, v in self.slots[q]:
                if v > 0:
                    allv[key] = v
        for e in self.ENGS:
            waits = []
            for k, v in allv.items():
                if v > 0 and self.seen[e].get(k, 0) < v:
                    self.seen[e][k] = v
                    waits.append((k, v))
            if waits:
                self.ops[e].append((waits, None, None, 0))

    def emit(self):
        names = {"pe": "tensor", "dve": "vector", "act": "scalar", "pool": "gpsimd", "sp": "sync"}
        with self.nc.Block() as block:
            for e in self.ENGS:
                ops = self.ops[e]

                def body(eng, ops=ops):
                    for waits, fn, inckey, incv in ops:
                        for k, v in waits:
                            eng.wait_ge(self.sem[k], v)
                        if fn is None:
                            continue
                        ins = getattr(eng, fn[0])(**fn[1])
                        if inckey is not None:
                            ins.then_inc(self.sem[inckey], incv)

                getattr(block, names[e])(body)


class Arena:
    def __init__(self, base_ap, nbytes):
        self.base = base_ap
        self.nbytes = nbytes
        self.off = 0
        self.marks = []

    def alloc(self, cols, dtype=F32, parts=128):
        esz = mybir.dt.size(dtype)
        nb = (cols * esz + 31) // 32 * 32
        assert self.off + nb <= self.nbytes, "arena overflow %d + %d > %d" % (self.off, nb, self.nbytes)
        v = self.base[0:parts, self.off // 4:(self.off + nb) // 4]
        if dtype != F32:
            v = v.bitcast(dtype)
        v = v[:, 0:cols]
        self.off += nb
        return T(v)

    def mark(self):
        self.marks.append(self.off)

    def release(self):
        self.off = self.marks.pop()


class Prog:
    def __init__(self, S, arena_bytes=200 * 1024):
        self.S = S
        self.st = ExitStack()
        st = self.st
        nc = bass.Bass("TRN2", target_bir_lowering=False)
        self.nc = nc
        self.dram = {}
        self.sch = Sched(nc, st)
        base = st.enter_context(nc.sbuf_tensor("arena", [128, arena_bytes // 4], F32))
        self.ar = Arena(base[:, :], arena_bytes)
        self.psum = []
        for i in range(8):
            t = st.enter_context(nc.psum_tensor("ps%d" % i, [128, 512], F32))
            self.psum.append(T(t[:, :], Buf(excl=True)))
        self.ident_bf = self.ar.alloc(128, BF16)
        self.ident_f = self.ar.alloc(128, F32)
        self.eps = self.ar.alloc(1, F32)
        idd = self.din("ident", [128, 128])
        self.sch.dma("sp", self.ident_f.ap, idd, writes=[self.ident_f.b])
        self.sch.op("dve", "tensor_copy", dict(out=self.ident_bf.ap, in_=self.ident_f.ap),
                    reads=[self.ident_f.b], writes=[self.ident_bf.b])
        self.sch.op("dve", "memset", dict(ap=self.eps.ap, constant=EPS), writes=[self.eps.b])

    def din(self, name, shape, dtype=F32):
        t = self.nc.dram_tensor(name, list(shape), dtype, kind="ExternalInput").ap()
        self.dram[name] = t
        return t

    def dout(self, name, shape, dtype=F32):
        t = self.nc.dram_tensor(name, list(shape), dtype, kind="ExternalOutput").ap()
        self.dram[name] = t
        return t

    def dscratch(self, name, shape, dtype=F32):
        t = self.nc.dram_tensor(name, list(shape), dtype, kind="Internal").ap()
        self.dram[name] = t
        return t

    def phase_begin(self):
        self.ar.mark()

    def phase_end(self):
        self.sch.barrier()
        self.ar.release()

    def rstd(self, out, ss, n):
        sch = self.sch
        P = ss.ap.shape[0]
        sch.op("act", "activation", dict(out=out.ap, in_=ss.ap, func=AF.Sqrt, bias=self.eps.ap[0:P, :], scale=1.0 / n),
               reads=[ss.b, self.eps.b], writes=[out.b])
        sch.op("dve", "reciprocal", dict(out=out.ap, in_=out.ap), reads=[out.b], writes=[out.b])


def mlp_phase(P, src, dst, w1d, w2d, gin_row, gout_row):
    sch, ar, S = P.sch, P.ar, P.S
    TT = 256
    P.phase_begin()
    w1 = [ar.alloc(8 * 512, BF16) for _ in range(8)]
    w2 = [ar.alloc(4 * 1024, BF16) for _ in range(8)]
    w1src = w1d.rearrange("(c p) f -> p c f", p=128)
    w2src = w2d.rearrange("(c p) d -> p c d", p=128)
    for i in range(8):
        sch.dma("pool", w1[i].ap.rearrange("p (c f) -> p c f", f=512), w1src[:, :, i * 512:(i + 1) * 512],
                writes=[w1[i].b])
    for i in range(8):
        sch.dma("pool", w2[i].ap.rearrange("p (c d) -> p c d", d=1024), w2src[:, 4 * i:4 * i + 4, :],
                writes=[w2[i].b])
    gin = ar.alloc(D)
    gout = ar.alloc(D)
    sch.dma("sp", gin.ap, gin_row.partition_broadcast(128), writes=[gin.b])
    sch.dma("sp", gout.ap, gout_row.partition_broadcast(128), writes=[gout.b])
    NB = 2
    hs = [[ar.alloc(D) for _ in range(2)] for _ in range(NB)]
    hn = [ar.alloc(D, BF16) for _ in range(2)]
    hnT = [ar.alloc(8 * TT, BF16) for _ in range(NB)]
    ss = [ar.alloc(2) for _ in range(NB)]
    rs = [ar.alloc(2) for _ in range(NB)]
    rs2 = [ar.alloc(2) for _ in range(NB)]
    junk = ar.alloc(D, BF16)
    rr = [ar.alloc(TT) for _ in range(2)]
    aT = [ar.alloc(TT, BF16) for _ in range(2)]
    yt = ar.alloc(D)
    acc = P.psum[0:4]
    pm = P.psum[4:6]
    pt = P.psum[6:8]
    ntile = S // TT

    def load_tile(t):
        for sub in range(2):
            r0 = t * TT + sub * 128
            sch.dma("sp", hs[t % NB][sub].ap, src[r0:r0 + 128, :], writes=[hs[t % NB][sub].b])

    load_tile(0)
    for t in range(ntile):
        pb = t % NB
        h = hs[pb]
        for sub in range(2):
            sch.op("act", "activation", dict(out=junk.ap, in_=h[sub].ap, func=AF.Square,
                                             accum_out=ss[pb].ap[:, sub:sub + 1]),
                   reads=[h[sub].b], writes=[junk.b, ss[pb].b])
        P.rstd(rs[pb], ss[pb], D)
        for sub in range(2):
            sch.op("dve", "scalar_tensor_tensor", dict(out=hn[sub].ap, in0=h[sub].ap, scalar=rs[pb].ap[:, sub:sub + 1],
                                                       in1=gin.ap, op0=ALU.mult, op1=ALU.mult),
                   reads=[h[sub].b, rs[pb].b, gin.b], writes=[hn[sub].b])
        for kc in range(8):
            ptb = pt[kc % 2]
            ptv = ptb.ap.bitcast(BF16)
            for sub in range(2):
                sch.op("pe", "transpose", dict(out=ptv[:, sub * 128:(sub + 1) * 128],
                                               in_=hn[sub].ap[:, kc * 128:(kc + 1) * 128], identity=P.ident_bf.ap),
                       reads=[hn[sub].b, P.ident_bf.b], writes=[ptb.b], inc=(sub == 1))
            if kc % 2 == 0:
                sch.op("act", "copy", dict(out=hnT[pb].ap[:, kc * TT:(kc + 1) * TT], in_=ptv[:, 0:TT]),
                       reads=[ptb.b], writes=[hnT[pb].b])
            else:
                sch.op("dve", "tensor_copy", dict(out=hnT[pb].ap[:, kc * TT:(kc + 1) * TT], in_=ptv[:, 0:TT]),
                       reads=[ptb.b], writes=[hnT[pb].b])
        if t + 1 < ntile:
            load_tile(t + 1)

        def mm1(f):
            pmb = pm[f % 2]
            wt = w1[f // 4]
            c0 = (f % 4) * 128
            for kc in range(8):
                sch.op("pe", "matmul", dict(out=pmb.ap[:, 0:TT], lhsT=wt.ap[:, kc * 512 + c0:kc * 512 + c0 + 128],
                                            rhs=hnT[pb].ap[:, kc * TT:(kc + 1) * TT], start=(kc == 0), stop=(kc == 7)),
                       reads=[wt.b, hnT[pb].b], writes=[pmb.b], inc=(kc == 7))
            sch.op("act", "activation", dict(out=rr[f % 2].ap, in_=pmb.ap[:, 0:TT], func=AF.Relu),
                   reads=[pmb.b], writes=[rr[f % 2].b])
            sch.op("dve", "tensor_tensor", dict(out=aT[f % 2].ap, in0=rr[f % 2].ap, in1=rr[f % 2].ap, op=ALU.mult),
                   reads=[rr[f % 2].b], writes=[aT[f % 2].b])

        def mm2(f):
            wt = w2[f // 4]
            o = (f % 4) * 1024
            for sub in range(2):
                for half in range(2):
                    a = acc[sub * 2 + half]
                    sch.op("pe", "matmul", dict(out=a.ap[:, 0:512], lhsT=aT[f % 2].ap[:, sub * 128:(sub + 1) * 128],
                                                rhs=wt.ap[:, o + half * 512:o + (half + 1) * 512],
                                                start=(f == 0), stop=(f == 31)),
                           reads=[aT[f % 2].b, wt.b], writes=[a.b], inc=(f == 31 or (sub == 1 and half == 1)))

        mm1(0)
        for f in range(32):
            if f + 1 < 32:
                mm1(f + 1)
            mm2(f)
        for sub in range(2):
            for half in range(2):
                a = acc[sub * 2 + half]
                sch.op("act", "activation", dict(out=junk.ap[:, 0:512], in_=a.ap[:, 0:512], func=AF.Square,
                                                 accum_out=rs2[pb].ap[:, half:half + 1]),
                       reads=[a.b], writes=[junk.b, rs2[pb].b])
            sch.op("dve", "tensor_tensor", dict(out=rs2[pb].ap[:, 0:1], in0=rs2[pb].ap[:, 0:1],
                                                in1=rs2[pb].ap[:, 1:2], op=ALU.add),
                   reads=[rs2[pb].b], writes=[rs2[pb].b])
            r1 = T(rs2[pb].ap[:, 0:1], rs2[pb].b)
            P.rstd(r1, r1, D)
            for half in range(2):
                a = acc[sub * 2 + half]
                sch.op("dve", "scalar_tensor_tensor", dict(
                    out=yt.ap[:, half * 512:(half + 1) * 512], in0=a.ap[:, 0:512], scalar=rs2[pb].ap[:, 0:1],
                    in1=gout.ap[:, half * 512:(half + 1) * 512], op0=ALU.mult, op1=ALU.mult),
                    reads=[a.b, rs2[pb].b, gout.b], writes=[yt.b])
            sch.op("pool", "tensor_tensor", dict(out=h[sub].ap, in0=yt.ap, in1=h[sub].ap, op=ALU.add),
                   reads=[yt.b, h[sub].b], writes=[h[sub].b])
            r0 = t * TT + sub * 128
            sch.dma("sp", dst[r0:r0 + 128, :], h[sub].ap, reads=[h[sub].b])
    P.phase_end()


G, PST, CH, E = 32, 64, 16, 512
TWO_PI_S = 6.2831845


def s5_host_layout(lam_re, lam_im, log_dt, b_re, b_im, c_re, c_im, d_skip):
    lr = np.concatenate([lam_re.T, lam_re.T], 0)
    li = np.concatenate([lam_im.T, lam_im.T], 0)
    ld = np.broadcast_to(log_dt[None, :], (128, G))
    bre = b_re.transpose(1, 0, 2)
    bim = b_im.transpose(1, 0, 2)
    cre = c_re.transpose(2, 0, 1)
    cim = c_im.transpose(2, 0, 1)
    B1 = np.concatenate([bre, bim], 0).reshape(128, G * CH)
    B2 = np.concatenate([bim, bre], 0).reshape(128, G * CH)
    C1 = np.concatenate([cre, cim], 0).reshape(128, G * CH)
    C2 = np.concatenate([cim, cre], 0).reshape(128, G * CH)
    dcol = np.tile(d_skip.T, (8, 1))
    small = np.concatenate([lr, li, ld, dcol], 1)
    big = np.concatenate([B1, B2, C1, C2], 1)
    return np.ascontiguousarray(small, dtype=np.float32), np.ascontiguousarray(big, dtype=np.float32)


def s5_consts():
    sg = np.ones((128, 2), np.float32)
    sg[64:, 0] = -1.0
    sg[:64, 1] = -1.0
    j = np.arange(128) // 16
    mask = (j[None, :] >= j[:, None]).astype(np.float32)
    return np.concatenate([sg, mask], 1)


def s5_phase(P, src, dst, small_d, big_d, cst_d, w_in_d, w_glu_d, w_out_d, gin_row, gout_row, NI=2):
    sch, ar, S = P.sch, P.ar, P.S
    KP = S // 8
    NM = KP // NI
    NKB = KP // 128
    P.phase_begin()
    small = ar.alloc(128)
    big = ar.alloc(2048)
    cst = ar.alloc(130)
    sch.dma("sp", small.ap, small_d, writes=[small.b])
    sch.dma("sp", big.ap, big_d, writes=[big.b])
    sch.dma("sp", cst.ap, cst_d, writes=[cst.b])
    gin = ar.alloc(D)
    gout = ar.alloc(D)
    sch.dma("sp", gin.ap, gin_row.partition_broadcast(128), writes=[gin.b])
    sch.dma("sp", gout.ap, gout_row.partition_broadcast(128), writes=[gout.b])
    sg = cst.ap[:, 0:1]
    nsg = cst.ap[:, 1:2]
    cmask = cst.ap[:, 2:130]
    lr = small.ap[:, 0:32]
    li = small.ap[:, 32:64]
    ld = small.ap[:, 64:96]
    dcol = small.ap[:, 96:128]
    B1 = big.ap[:, 0:512].rearrange("p (g c) -> p g c", c=CH)
    B2 = big.ap[:, 512:1024].rearrange("p (g c) -> p g c", c=CH)
    C1 = big.ap[:, 1024:1536].rearrange("p (g c) -> p g c", c=CH)
    C2 = big.ap[:, 1536:2048].rearrange("p (g c) -> p g c", c=CH)

    EC = list(range(-7, 8 * NI + 1))
    EB = [8 * (NI - 1 - i) + 7 - j for i in range(NI) for j in range(8)]
    EX = EC + EB
    NE = len(EX)
    NC_ = len(EC)
    tb = T(None)
    def tl(cols, dt=F32):
        t = ar.alloc(cols, dt)
        return t.ap
    Pr = tl(32 * NE); Pi = tl(32 * NE); nsPi = tl(32 * NE); sPi = tl(32 * NE)
    Bb1 = tl(512); Bb2 = tl(512)
    zT = ar.alloc(4 * 8 * KP, BF16)
    zTv = zT.ap.rearrange("p (e j k) -> p e j k", j=8, k=KP)
    off_after_zT = ar.off
    U = ar.alloc(G * KP, BF16)
    Uv = U.ap.rearrange("p (g k) -> p g k", k=KP)
    ar.mark()
    dt_ = tl(32); lrdt = tl(32); lidt = tl(32)
    argm = tl(32 * NE); arga = tl(32 * NE); argi = tl(32 * NE, I32); argf = tl(32 * NE)
    m1 = tl(32 * NE)
    R = [small.b, big.b, cst.b, tb.b]
    W = [tb.b]
    def dve(meth, **kw):
        sch.op("dve", meth, kw, reads=R, writes=W)
    def act(**kw):
        sch.op("act", "activation", kw, reads=R, writes=W)
    act(out=dt_, in_=ld, func=AF.Exp)
    dve("tensor_tensor", out=lrdt, in0=lr, in1=dt_, op=ALU.mult)
    dve("tensor_tensor", out=lidt, in0=li, in1=dt_, op=ALU.mult)
    v3 = lambda a: a.rearrange("p (g n) -> p g n", n=NE)
    for i, n in enumerate(EX):
        dve("tensor_scalar", out=v3(argm)[:, :, i], in0=lrdt, scalar1=float(n), scalar2=None, op0=ALU.mult)
        dve("tensor_scalar", out=v3(arga)[:, :, i], in0=lidt, scalar1=float(n) / (2 * math.pi), scalar2=None, op0=ALU.mult)
    act(out=argm, in_=argm, func=AF.Exp)
    dve("tensor_copy", out=argi, in_=arga)
    dve("tensor_copy", out=argf, in_=argi)
    dve("tensor_tensor", out=arga, in0=arga, in1=argf, op=ALU.subtract)
    def wrap(y):
        dve("tensor_scalar", out=m1, in0=y, scalar1=0.5, scalar2=None, op0=ALU.is_gt)
        dve("tensor_tensor", out=y, in0=y, in1=m1, op=ALU.subtract)
        dve("tensor_scalar", out=m1, in0=y, scalar1=-0.5, scalar2=None, op0=ALU.is_lt)
        dve("tensor_tensor", out=y, in0=y, in1=m1, op=ALU.add)
    wrap(arga)
    act(out=Pi, in_=arga, func=AF.Sin, scale=TWO_PI_S)
    dve("tensor_scalar", out=arga, in0=arga, scalar1=0.25, scalar2=None, op0=ALU.add)
    wrap(arga)
    act(out=Pr, in_=arga, func=AF.Sin, scale=TWO_PI_S)
    dve("tensor_tensor", out=Pr, in0=Pr, in1=argm, op=ALU.mult)
    dve("tensor_tensor", out=Pi, in0=Pi, in1=argm, op=ALU.mult)
    dve("tensor_scalar", out=nsPi, in0=Pi, scalar1=nsg, scalar2=None, op0=ALU.mult)
    dve("tensor_scalar", out=sPi, in0=Pi, scalar1=sg, scalar2=None, op0=ALU.mult)
    i1 = EC.index(1)
    nr = tl(32); den = tl(32); fr = tl(32); fi = tl(32); t32 = tl(32); nsfi = tl(32); sfi = tl(32)
    dve("tensor_scalar", out=nr, in0=v3(Pr)[:, :, i1], scalar1=-1.0, scalar2=None, op0=ALU.add)
    ni_ = v3(Pi)[:, :, i1]
    dve("tensor_tensor", out=den, in0=lr, in1=lr, op=ALU.mult)
    dve("tensor_tensor", out=t32, in0=li, in1=li, op=ALU.mult)
    dve("tensor_tensor", out=den, in0=den, in1=t32, op=ALU.add)
    dve("reciprocal", out=den, in_=den)
    dve("tensor_tensor", out=fr, in0=nr, in1=lr, op=ALU.mult)
    dve("tensor_tensor", out=t32, in0=ni_, in1=li, op=ALU.mult)
    dve("tensor_tensor", out=fr, in0=fr, in1=t32, op=ALU.add)
    dve("tensor_tensor", out=fr, in0=fr, in1=den, op=ALU.mult)
    dve("tensor_tensor", out=fi, in0=ni_, in1=lr, op=ALU.mult)
    dve("tensor_tensor", out=t32, in0=nr, in1=li, op=ALU.mult)
    dve("tensor_tensor", out=fi, in0=fi, in1=t32, op=ALU.subtract)
    dve("tensor_tensor", out=fi, in0=fi, in1=den, op=ALU.mult)
    dve("tensor_scalar", out=nsfi, in0=fi, scalar1=nsg, scalar2=None, op0=ALU.mult)
    dve("tensor_scalar", out=sfi, in0=fi, scalar1=sg, scalar2=None, op0=ALU.mult)
    tB = tl(512)
    g3 = lambda a: a.rearrange("p (g c) -> p g c", c=CH)
    bc = lambda a: a.unsqueeze(2).broadcast_to([128, 32, CH])
    dve("tensor_tensor", out=g3(Bb1), in0=B1, in1=bc(fr), op=ALU.mult)
    dve("tensor_tensor", out=g3(tB), in0=B2, in1=bc(nsfi), op=ALU.mult)
    dve("tensor_tensor", out=Bb1, in0=Bb1, in1=tB, op=ALU.add)
    dve("tensor_tensor", out=g3(Bb2), in0=B2, in1=bc(fr), op=ALU.mult)
    dve("tensor_tensor", out=g3(tB), in0=B1, in1=bc(sfi), op=ALU.mult)
    dve("tensor_tensor", out=Bb2, in0=Bb2, in1=tB, op=ALU.add)
    iA = EC.index(8 * NI)
    A1 = v3(Pr)[:, :, iA]
    A2 = v3(nsPi)[:, :, iA]
    A2w = v3(sPi)[:, :, iA]

    NB8 = NI * 8
    def ba_tables(g, outst, outsw, tmp):
        o4 = lambda a: a.rearrange("p (n c) -> p n c", c=CH)
        prb = v3(Pr)[:, g, NC_:NE].unsqueeze(2).broadcast_to([128, NB8, CH])
        nspb = v3(nsPi)[:, g, NC_:NE].unsqueeze(2).broadcast_to([128, NB8, CH])
        spb = v3(sPi)[:, g, NC_:NE].unsqueeze(2).broadcast_to([128, NB8, CH])
        b1 = g3(Bb1)[:, g, :].unsqueeze(1).broadcast_to([128, NB8, CH])
        b2 = g3(Bb2)[:, g, :].unsqueeze(1).broadcast_to([128, NB8, CH])
        rw = dict(reads=[tb.b], writes=[outst.b, outsw.b, tmp.b])
        sch.op("dve", "tensor_tensor", dict(out=o4(outst.ap), in0=prb, in1=b1, op=ALU.mult), **rw)
        sch.op("dve", "tensor_tensor", dict(out=o4(tmp.ap), in0=nspb, in1=b2, op=ALU.mult), **rw)
        sch.op("dve", "tensor_tensor", dict(out=outst.ap, in0=outst.ap, in1=tmp.ap, op=ALU.add), **rw)
        if outsw is not outst:
            sch.op("dve", "tensor_tensor", dict(out=o4(outsw.ap), in0=prb, in1=b2, op=ALU.mult), **rw)
            sch.op("dve", "tensor_tensor", dict(out=o4(tmp.ap), in0=spb, in1=b1, op=ALU.mult), **rw)
            sch.op("dve", "tensor_tensor", dict(out=outsw.ap, in0=outsw.ap, in1=tmp.ap, op=ALU.add), **rw)

    sch.barrier()
    ar.release()
    ar.mark()
    w_in = ar.alloc(8 * E, BF16)
    sch.dma("pool", w_in.ap.rearrange("p (c e) -> p c e", e=E), w_in_d.rearrange("(c p) e -> p c e", p=128), writes=[w_in.b])
    hblk = ar.alloc(8 * D)
    hnj = [ar.alloc(D, BF16) for _ in range(2)]
    hnT = [ar.alloc(8 * 128, BF16) for _ in range(2)]
    utok = ar.alloc(8 * E, BF16)
    utv = utok.ap.rearrange("p (g j c) -> p g j c", j=8, c=CH)
    ss = ar.alloc(8); rs = ar.alloc(8)
    junk = ar.alloc(D, BF16)
    pt = P.psum[6:8]
    pu = P.psum[4:6]
    for kb in range(NKB):
        r0 = kb * 1024
        sch.dma("sp", hblk.ap, src[r0:r0 + 1024, :].rearrange("(k j) d -> k (j d)", j=8), writes=[hblk.b])
        for j in range(8):
            sch.op("act", "activation", dict(out=junk.ap, in_=hblk.ap[:, j * D:(j + 1) * D], func=AF.Square,
                                             accum_out=ss.ap[:, j:j + 1]), reads=[hblk.b], writes=[junk.b, ss.b])
        P.rstd(rs, ss, D)
        for j in range(8):
            hn = hnj[j % 2]
            sch.op("dve", "scalar_tensor_tensor", dict(out=hn.ap, in0=hblk.ap[:, j * D:(j + 1) * D],
                                                       scalar=rs.ap[:, j:j + 1], in1=gin.ap, op0=ALU.mult, op1=ALU.mult),
                   reads=[hblk.b, rs.b, gin.b], writes=[hn.b])
            hT = hnT[j % 2]
            for kc2 in range(2):
                ptb = pt[kc2]
                ptv = ptb.ap.bitcast(BF16)
                for q in range(4):
                    kc = kc2 * 4 + q
                    sch.op("pe", "transpose", dict(out=ptv[:, q * 128:(q + 1) * 128],
                                                   in_=hn.ap[:, kc * 128:(kc + 1) * 128], identity=P.ident_bf.ap),
                           reads=[hn.b, P.ident_bf.b], writes=[ptb.b], inc=(q == 3))
                if kc2 == 0:
                    sch.op("act", "copy", dict(out=hT.ap[:, 0:512], in_=ptv[:, 0:512]), reads=[ptb.b], writes=[hT.b])
                else:
                    sch.op("dve", "tensor_copy", dict(out=hT.ap[:, 512:1024], in_=ptv[:, 0:512]), reads=[ptb.b], writes=[hT.b])
            pub = pu[j % 2]
            for kc in range(8):
                sch.op("pe", "matmul", dict(out=pub.ap[:, 0:E], lhsT=hT.ap[:, kc * 128:(kc + 1) * 128],
                                            rhs=w_in.ap[:, kc * E:(kc + 1) * E], start=(kc == 0), stop=(kc == 7)),
                       reads=[hT.b, w_in.b], writes=[pub.b], inc=(kc == 7))
            sch.op("act", "copy", dict(out=utv[:, :, j, :], in_=pub.ap[:, 0:E].rearrange("p (g c) -> p g c", c=CH)),
                   reads=[pub.b], writes=[utok.b])
        for g4 in range(8):
            ptb = pt[g4 % 2]
            ptv = ptb.ap.bitcast(BF16)
            for q in range(4):
                g = g4 * 4 + q
                sch.op("pe", "transpose", dict(out=ptv[:, q * 128:(q + 1) * 128], in_=utok.ap[:, g * 128:(g + 1) * 128],
                                               identity=P.ident_bf.ap),
                       reads=[utok.b, P.ident_bf.b], writes=[ptb.b], inc=(q == 3))
            eng = ("act", "copy") if g4 % 2 == 0 else ("dve", "tensor_copy")
            sch.op(eng[0], eng[1], dict(out=Uv[:, g4 * 4:g4 * 4 + 4, kb * 128:(kb + 1) * 128],
                                        in_=ptv[:, 0:512].rearrange("p (g k) -> p g k", k=128)),
                   reads=[ptb.b], writes=[U.b])

    sch.barrier()
    ar.release()
    Sst = ar.alloc(G * NM, BF16)
    Ssw = ar.alloc(G * NM, BF16)
    Sstv = Sst.ap.rearrange("p (g m) -> p g m", m=NM)
    Sswv = Ssw.ap.rearrange("p (g m) -> p g m", m=NM)
    bast = [ar.alloc(NB8 * CH) for _ in range(2)]
    basw = [ar.alloc(NB8 * CH) for _ in range(2)]
    btmp = ar.alloc(NB8 * CH)
    wsst = [ar.alloc(NI * 128, BF16) for _ in range(2)]
    wssw = [ar.alloc(NI * 128, BF16) for _ in range(2)]
    ps_s = P.psum[0:4]
    for g in range(G):
        pb = g % 2
        ba_tables(g, bast[pb], basw[pb], btmp)
        for which, (ba, ws) in enumerate(((bast[pb], wsst[pb]), (basw[pb], wssw[pb]))):
            ptb = pt[which]
            for i in range(NI):
                sch.op("pe", "transpose", dict(out=ptb.ap[:, i * 128:(i + 1) * 128], in_=ba.ap[:, i * 128:(i + 1) * 128],
                                               identity=P.ident_f.ap),
                       reads=[ba.b, P.ident_f.b], writes=[ptb.b], inc=(i == NI - 1))
            sch.op("act", "copy", dict(out=ws.ap, in_=ptb.ap[:, 0:NI * 128]), reads=[ptb.b], writes=[ws.b])
        for which, (ws, Sv, Sb) in enumerate(((wsst[pb], Sstv, Sst), (wssw[pb], Sswv, Ssw))):
            psb = ps_s[(g % 2) * 2 + which]
            for i in range(NI):
                rhs = Uv[:, g, :].rearrange("p (m i) -> p m i", i=NI)[:, :, i]
                sch.op("pe", "matmul", dict(out=psb.ap[:, 0:NM], lhsT=ws.ap[:, i * 128:(i + 1) * 128], rhs=rhs,
                                            start=(i == 0), stop=(i == NI - 1)),
                       reads=[ws.b, U.b], writes=[psb.b], inc=(i == NI - 1))
            sch.op("dve", "tensor_copy", dict(out=Sv[:, g, :], in_=psb.ap[:, 0:NM]), reads=[psb.b], writes=[Sb.b])

    Xs = ar.alloc(32); Xw = ar.alloc(32); t1 = ar.alloc(32); t2 = ar.alloc(32)
    sb = T(None)
    rw = dict(reads=[tb.b, sb.b, Sst.b, Ssw.b], writes=[sb.b])
    sch.op("dve", "memset", dict(ap=Xs.ap, constant=0.0), **rw)
    sch.op("dve", "memset", dict(ap=Xw.ap, constant=0.0), **rw)
    for m in range(NM):
        tt = lambda **kw: sch.op("dve", "tensor_tensor", kw, **rw)
        tt(out=t1.ap, in0=Xs.ap, in1=A1, op=ALU.mult)
        tt(out=t2.ap, in0=Xw.ap, in1=A2, op=ALU.mult)
        tt(out=t1.ap, in0=t1.ap, in1=t2.ap, op=ALU.add)
        tt(out=t2.ap, in0=Xw.ap, in1=A1, op=ALU.mult)
        tt(out=Xw.ap, in0=Xs.ap, in1=A2w, op=ALU.mult)
        tt(out=Xw.ap, in0=Xw.ap, in1=t2.ap, op=ALU.add)
        tt(out=Xw.ap, in0=Xw.ap, in1=Sswv[:, :, m], op=ALU.add)
        tt(out=Xs.ap, in0=t1.ap, in1=Sstv[:, :, m], op=ALU.add)
        sch.op("dve", "tensor_copy", dict(out=Sstv[:, :, m], in_=Xs.ap), reads=[sb.b], writes=[sb.b, Sst.b])

    cast = [ar.alloc((NC_) * CH) for _ in range(2)]
    ctmp = ar.alloc((NC_) * CH)
    L1 = [ar.alloc(128) for _ in range(2)]
    Tg = [ar.alloc(NI * 128, BF16) for _ in range(2)]
    WY = [ar.alloc(NI * 128, BF16) for _ in range(2)]
    tmask = ar.alloc(128)
    zg = [ar.alloc(KP, BF16) for _ in range(2)]
    gx2 = ar.alloc(KP); gt = ar.alloc(KP)
    psy = P.psum[0:2]
    pst = P.psum[2:4]
    for g in range(G):
        pb = g % 2
        ba_tables(g, bast[pb], bast[pb], btmp)
        o4 = lambda a: a.rearrange("p (n c) -> p n c", c=CH)
        prc = v3(Pr)[:, g, 0:NC_].unsqueeze(2).broadcast_to([128, NC_, CH])
        nspc = v3(nsPi)[:, g, 0:NC_].unsqueeze(2).broadcast_to([128, NC_, CH])
        c1 = C1[:, g, :].unsqueeze(1).broadcast_to([128, NC_, CH])
        c2 = C2[:, g, :].unsqueeze(1).broadcast_to([128, NC_, CH])
        rwc = dict(reads=[tb.b, big.b], writes=[cast[pb].b, ctmp.b])
        sch.op("dve", "tensor_tensor", dict(out=o4(cast[pb].ap), in0=prc, in1=c1, op=ALU.mult), **rwc)
        sch.op("dve", "tensor_tensor", dict(out=o4(ctmp.ap), in0=nspc, in1=c2, op=ALU.mult), **rwc)
        sch.op("dve", "tensor_tensor", dict(out=cast[pb].ap, in0=cast[pb].ap, in1=ctmp.ap, op=ALU.add), **rwc)
        sch.op("dve", "tensor_scalar", dict(out=L1[pb].ap, in0=bast[pb].ap[:, (NI - 1) * 128:NI * 128], scalar1=sg, scalar2=None, op0=ALU.mult),
               reads=[bast[pb].b, cst.b], writes=[L1[pb].b])
        ptb = pst[pb]
        for dl in range(NI):
            sch.op("pe", "matmul", dict(out=ptb.ap[:, dl * 128:(dl + 1) * 128], lhsT=L1[pb].ap,
                                        rhs=cast[pb].ap[:, dl * 128:(dl + 1) * 128], start=True, stop=True),
                   reads=[L1[pb].b, cast[pb].b], writes=[ptb.b], inc=(dl == NI - 1))
        sch.op("dve", "tensor_tensor", dict(out=tmask.ap, in0=ptb.ap[:, 0:128], in1=cmask, op=ALU.mult),
               reads=[ptb.b, cst.b], writes=[tmask.b])
        sch.op("dve", "scalar_tensor_tensor", dict(out=Tg[pb].ap[:, 0:128], in0=P.ident_f.ap, scalar=dcol[:, g:g + 1], in1=tmask.ap,
                                                   op0=ALU.mult, op1=ALU.add),
               reads=[tmask.b, P.ident_f.b, small.b], writes=[Tg[pb].b])
        if NI > 1:
            sch.op("act", "copy", dict(out=Tg[pb].ap[:, 128:NI * 128], in_=ptb.ap[:, 128:NI * 128]), reads=[ptb.b], writes=[Tg[pb].b])
        sch.op("dve", "tensor_scalar", dict(out=WY[pb].ap, in0=cast[pb].ap[:, 128:128 + NI * 128], scalar1=sg, scalar2=None, op0=ALU.mult),
               reads=[cast[pb].b, cst.b], writes=[WY[pb].b])
        yb = psy[pb]
        yv = yb.ap[:, 0:KP].rearrange("p (m i) -> p m i", i=NI)
        uv = Uv[:, g, :].rearrange("p (m i) -> p m i", i=NI)
        for dl in range(NI):
            for i in range(dl, NI):
                sch.op("pe", "matmul", dict(out=yv[:, :, i], lhsT=Tg[pb].ap[:, dl * 128:(dl + 1) * 128], rhs=uv[:, :, i - dl],
                                            start=(dl == 0 and i == 0), stop=False),
                       reads=[Tg[pb].b, U.b], writes=[yb.b], inc=False)
        for i in range(NI):
            sch.op("pe", "matmul", dict(out=yv[:, 1:NM, i], lhsT=WY[pb].ap[:, i * 128:(i + 1) * 128], rhs=Sstv[:, g, 0:NM - 1],
                                        start=False, stop=(i == NI - 1)),
                   reads=[WY[pb].b, Sst.b], writes=[yb.b], inc=(i == NI - 1))
        y = yb.ap[:, 0:KP]
        sch.op("act", "activation", dict(out=gx2.ap, in_=y, func=AF.Square), reads=[yb.b], writes=[gx2.b])
        sch.op("dve", "tensor_scalar", dict(out=gx2.ap, in0=gx2.ap, scalar1=0.044715, scalar2=1.0, op0=ALU.mult, op1=ALU.add),
               reads=[gx2.b], writes=[gx2.b])
        sch.op("dve", "tensor_tensor", dict(out=gt.ap, in0=y, in1=gx2.ap, op=ALU.mult), reads=[yb.b, gx2.b], writes=[gt.b])
        sch.op("act", "activation", dict(out=gt.ap, in_=gt.ap, func=AF.Sigmoid, scale=1.5957691216057308), reads=[gt.b], writes=[gt.b])
        sch.op("dve", "tensor_tensor", dict(out=zg[pb].ap, in0=y, in1=gt.ap, op=ALU.mult), reads=[yb.b, gt.b], writes=[zg[pb].b])
        ech, gi = g // 8, g % 8
        for j in range(8):
            sch.dma("sp" if j % 2 == 0 else "pool", zTv[gi * 16:(gi + 1) * 16, ech, j, :], zg[pb].ap[j * 16:(j + 1) * 16, :],
                    reads=[zg[pb].b], writes=[zT.b])

    sch.barrier()
    ar.off = off_after_zT
    w_glu = ar.alloc(4 * E, BF16)
    w_out = ar.alloc(4 * D, BF16)
    sch.dma("pool", w_glu.ap.rearrange("p (c e) -> p c e", e=E), w_glu_d.rearrange("(c p) e -> p c e", p=128), writes=[w_glu.b])
    sch.dma("pool", w_out.ap.rearrange("p (c e) -> p c e", e=D), w_out_d.rearrange("(c p) e -> p c e", p=128), writes=[w_out.b])
    junk = ar.alloc(D, BF16)
    z2 = [ar.alloc(4 * 512, BF16) for _ in range(2)]
    sgm = ar.alloc(512)
    hres = [ar.alloc(D) for _ in range(2)]
    yt = ar.alloc(D)
    rs2 = ar.alloc(2)
    pg = P.psum[4:6]
    po = P.psum[0:4]
    NKC = KP // 512 if KP >= 512 else 1
    KW = min(KP, 512)
    cnt = 0
    for j in range(8):
        for kc5 in range(NKC):
            k0 = kc5 * KW
            zz = z2[cnt % 2]
            for eo in range(4):
                pgb = pg[eo % 2]
                for ei in range(4):
                    sch.op("pe", "matmul", dict(out=pgb.ap[:, 0:KW], lhsT=w_glu.ap[:, ei * E + eo * 128:ei * E + (eo + 1) * 128],
                                                rhs=zTv[:, ei, j, k0:k0 + KW], start=(ei == 0), stop=(ei == 3)),
                           reads=[w_glu.b, zT.b], writes=[pgb.b], inc=(ei == 3))
                sch.op("act", "activation", dict(out=sgm.ap[:, 0:KW], in_=pgb.ap[:, 0:KW], func=AF.Sigmoid), reads=[pgb.b], writes=[sgm.b])
                sch.op("dve", "tensor_tensor", dict(out=zz.ap[:, eo * 512:eo * 512 + KW], in0=sgm.ap[:, 0:KW], in1=zTv[:, eo, j, k0:k0 + KW], op=ALU.mult),
                       reads=[sgm.b, zT.b], writes=[zz.b])
            for kq in range(KW // 128):
                kk = k0 + kq * 128
                hb = hres[kq % 2]
                rows = src.rearrange("(k j) d -> j k d", j=8)[j, kk:kk + 128, :]
                orows = dst.rearrange("(k j) d -> j k d", j=8)[j, kk:kk + 128, :]
                sch.dma("sp", hb.ap, rows, writes=[hb.b])
                for half in range(2):
                    pob = po[(kq % 2) * 2 + half]
                    for ei in range(4):
                        sch.op("pe", "matmul", dict(out=pob.ap[:, 0:512], lhsT=zz.ap[:, ei * 512 + kq * 128:ei * 512 + (kq + 1) * 128],
                                                    rhs=w_out.ap[:, ei * D + half * 512:ei * D + (half + 1) * 512],
                                                    start=(ei == 0), stop=(ei == 3)),
                               reads=[zz.b, w_out.b], writes=[pob.b], inc=(ei == 3))
                    sch.op("act", "activation", dict(out=junk.ap[:, 0:512], in_=pob.ap[:, 0:512], func=AF.Square,
                                                     accum_out=rs2.ap[:, half:half + 1]), reads=[pob.b], writes=[junk.b, rs2.b])
                sch.op("dve", "tensor_tensor", dict(out=rs2.ap[:, 0:1], in0=rs2.ap[:, 0:1], in1=rs2.ap[:, 1:2], op=ALU.add),
                       reads=[rs2.b], writes=[rs2.b])
                r1 = T(rs2.ap[:, 0:1], rs2.b)
                P.rstd(r1, r1, D)
                for half in range(2):
                    pob = po[(kq % 2) * 2 + half]
                    sch.op("dve", "scalar_tensor_tensor", dict(out=yt.ap[:, half * 512:(half + 1) * 512], in0=pob.ap[:, 0:512],
                                                               scalar=rs2.ap[:, 0:1], in1=gout.ap[:, half * 512:(half + 1) * 512],
                                                               op0=ALU.mult, op1=ALU.mult),
                           reads=[pob.b, rs2.b, gout.b], writes=[yt.b])
                sch.op("pool", "tensor_tensor", dict(out=hb.ap, in0=yt.ap, in1=hb.ap, op=ALU.add), reads=[yt.b, hb.b], writes=[hb.b])
                sch.dma("sp", orows, hb.ap, reads=[hb.b])
            cnt += 1
    P.phase_end()


HP = [0, 4, 1, 5, 2, 6, 3, 7, 8, 12, 9, 13, 10, 14, 11, 15]
QO, KO, QIO, KIO, VO, WIO, NCOL = 0, 1024, 1280, 1792, 1920, 2176, 2184
NEG = -1.0e30
NBIS = 22
MASK_BIG = 29952.0
import os
NO_INTERLEAVE = bool(int(os.environ.get('NO_INTERLEAVE', '0')))
DVE_SHARE = float(os.environ.get('DVE_SHARE', '0.5'))
FRONT_PRIO = float(os.environ.get('FRONT_PRIO', '1.0'))
NO_POOL = bool(int(os.environ.get('NO_POOL', '1')))


def dsa_host_layout(w_in, w_out):
    q = w_in[:, 0:1024].reshape(1024, 16, 64)[:, HP, :].reshape(1024, 1024)
    k = w_in[:, 1024:1280]
    v = w_in[:, 1280:1536]
    qi = w_in[:, 1536:2048]
    ki = w_in[:, 2048:2112]
    wi = w_in[:, 2112:2120]
    wip = np.concatenate([q, k, qi, ki, ki, v, wi], 1)
    wop = w_out.reshape(16, 64, 1024)[HP].reshape(1024, 1024)
    return np.ascontiguousarray(wip, dtype=np.float32), np.ascontiguousarray(wop, dtype=np.float32)


def dsa_consts():
    r = np.arange(128)
    negm = np.where(r[None, :] > r[:, None], NEG, 0.0).astype(np.float32)
    return negm


def dsa_phase(P, src, dst, w_in_d, w_out_d, negm_d, gin_row, gout_row):
    sch, ar, S = P.sch, P.ar, P.S
    NT = S // 128
    KSEL = min(256, S // 4)
    P.phase_begin()
    w_in = ar.alloc(8 * NCOL, BF16)
    w_out = ar.alloc(8 * D, BF16)
    wiv = w_in.ap.rearrange("p (c e) -> p c e", e=NCOL)
    wsrc = w_in_d.rearrange("(c p) e -> p c e", p=128)
    for c0 in range(0, NCOL, 512):
        c1 = min(NCOL, c0 + 512)
        sch.dma("pool", wiv[:, :, c0:c1], wsrc[:, :, c0:c1], writes=[w_in.b])
    sch.dma("pool", w_out.ap.rearrange("p (c e) -> p c e", e=D), w_out_d.rearrange("(c p) e -> p c e", p=128), writes=[w_out.b])
    gin = ar.alloc(D); gout = ar.alloc(D); negm = ar.alloc(128)
    sch.dma("sp", gin.ap, gin_row.partition_broadcast(128), writes=[gin.b])
    sch.dma("sp", gout.ap, gout_row.partition_broadcast(128), writes=[gout.b])
    sch.dma("sp", negm.ap, negm_d, writes=[negm.b])
    cosT = ar.alloc(NT * 32); sinT = ar.alloc(NT * 32)
    ar.mark()
    tb = T(None)
    posi = ar.alloc(NT, I32); posf = ar.alloc(NT); fi_ = ar.alloc(32, I32); invf = ar.alloc(32)
    ang = ar.alloc(NT * 32); angi = ar.alloc(NT * 32, I32); angf = ar.alloc(NT * 32); m1 = ar.alloc(NT * 32)
    rw = dict(reads=[tb.b], writes=[tb.b])
    sch.op("pool", "iota", dict(out=posi.ap, pattern=[[128, NT]], base=0, channel_multiplier=1), **rw)
    sch.op("pool", "iota", dict(out=fi_.ap, pattern=[[1, 32]], base=0, channel_multiplier=0), **rw)
    dve = lambda meth, **kw: sch.op("dve", meth, kw, **rw)
    dve("tensor_copy", out=posf.ap, in_=posi.ap)
    dve("tensor_copy", out=invf.ap, in_=fi_.ap)
    sch.op("act", "activation", dict(out=invf.ap, in_=invf.ap, func=AF.Exp, scale=-math.log(10000.0) / 32.0), **rw)
    dve("tensor_scalar", out=invf.ap, in0=invf.ap, scalar1=1.0 / (2 * math.pi), scalar2=None, op0=ALU.mult)
    a3 = lambda t: t.ap.rearrange("p (q i) -> p q i", i=32)
    dve("tensor_tensor", out=a3(ang), in0=posf.ap.unsqueeze(2).broadcast_to([128, NT, 32]),
        in1=invf.ap.unsqueeze(1).broadcast_to([128, NT, 32]), op=ALU.mult)
    dve("tensor_copy", out=angi.ap, in_=ang.ap)
    dve("tensor_copy", out=angf.ap, in_=angi.ap)
    dve("tensor_tensor", out=ang.ap, in0=ang.ap, in1=angf.ap, op=ALU.subtract)
    def wrap():
        dve("tensor_scalar", out=m1.ap, in0=ang.ap, scalar1=0.5, scalar2=None, op0=ALU.is_gt)
        dve("tensor_tensor", out=ang.ap, in0=ang.ap, in1=m1.ap, op=ALU.subtract)
        dve("tensor_scalar", out=m1.ap, in0=ang.ap, scalar1=-0.5, scalar2=None, op0=ALU.is_lt)
        dve("tensor_tensor", out=ang.ap, in0=ang.ap, in1=m1.ap, op=ALU.add)
    wrap()
    sch.op("act", "activation", dict(out=sinT.ap, in_=ang.ap, func=AF.Sin, scale=TWO_PI_S), reads=[tb.b], writes=[tb.b, sinT.b])
    dve("tensor_scalar", out=ang.ap, in0=ang.ap, scalar1=0.25, scalar2=None, op0=ALU.add)
    wrap()
    sch.op("act", "activation", dict(out=cosT.ap, in_=ang.ap, func=AF.Sin, scale=TWO_PI_S), reads=[tb.b], writes=[tb.b, cosT.b])
    sch.barrier()
    ar.release()
    cos3 = cosT.ap.rearrange("p (q i) -> p q i", i=32)
    sin3 = sinT.ap.rearrange("p (q i) -> p q i", i=32)

    kT = ar.alloc(2 * S, BF16)
    kTv = kT.ap.rearrange("p (b s) -> p b s", s=S)
    kiT = ar.alloc(S, BF16)
    V = ar.alloc(NT * 4 * 65, BF16)
    Vv = V.ap.rearrange("p (t n e) -> p t n e", n=4, e=65)
    kT_b = [Buf() for _ in range(NT)]
    kiT_b = [Buf() for _ in range(NT)]
    V_b = [Buf() for _ in range(NT)]
    sch.op("pool", "memset", dict(ap=V.ap, constant=1.0), writes=V_b)
    acc = ar.alloc(S)
    mask = ar.alloc(S, BF16)
    maskT = [ar.alloc(S, BF16) for _ in range(2)]
    negbig = ar.alloc(1)
    sch.op("pool", "memset", dict(ap=negbig.ap, constant=-MASK_BIG), writes=[negbig.b])
    hq = [ar.alloc(D) for _ in range(2)]
    qT = [ar.alloc(8 * 128, BF16) for _ in range(2)]
    hn = ar.alloc(D, BF16)
    hnT = ar.alloc(8 * 128, BF16)
    rp = ar.alloc(1920, BF16)
    tr = [ar.alloc(256) for _ in range(4)]
    wsb = ar.alloc(8); absw = ar.alloc(8); sgnw = ar.alloc(8)
    qiT = ar.alloc(4 * 128, BF16)
    rr = [ar.alloc(512) for _ in range(2)]
    pT = [ar.alloc(512, BF16) for _ in range(4)]
    osb = ar.alloc(D, BF16)
    oT = ar.alloc(8 * 128, BF16)
    yt = ar.alloc(D)
    junk = ar.alloc(D, BF16)
    junk2 = ar.alloc(512, BF16)
    ss = ar.alloc(1); rs = ar.alloc(1); rs2 = ar.alloc(2)
    lo = ar.alloc(1); hi = ar.alloc(1); mid = ar.alloc(1); cnt = ar.alloc(1); ge = ar.alloc(1); dd = ar.alloc(1)
    rden = ar.alloc(4)
    wt = ar.alloc(NBIS + 2); pow2 = ar.alloc(NBIS + 2); cntB = ar.alloc(1)
    junkA = ar.alloc(S - (int(S * DVE_SHARE) // 64) * 64 + 64, BF16)
    for k in range(NBIS + 2):
        sch.op("pool", "memset", dict(ap=pow2.ap[:, k:k + 1], constant=2.0 ** (-k)), writes=[pow2.b])
    pqk = P.psum[0:2]
    ppv = P.psum[2:4]
    pout = P.psum[0:2]
    pf = P.psum[4:7]
    ptf = P.psum[7]

    def rstd_ln(out, ssq):
        sch.op("act", "activation", dict(out=out.ap, in_=ssq.ap, func=AF.Ln, bias=P.eps.ap, scale=1.0 / D), reads=[ssq.b, P.eps.b], writes=[out.b])
        sch.op("act", "activation", dict(out=out.ap, in_=out.ap, func=AF.Exp, scale=-0.5), reads=[out.b], writes=[out.b])

    def front(qt):
        r0 = qt * 128
        L = (qt + 1) * 128
        h = hq[qt % 2]
        qTq = qT[qt % 2]
        mT = maskT[qt % 2]
        sch.dma("sp", h.ap, src[r0:r0 + 128, :], writes=[h.b])
        sch.op("act", "activation", dict(out=junk.ap, in_=h.ap, func=AF.Square, accum_out=ss.ap), reads=[h.b], writes=[junk.b, ss.b])
        rstd_ln(rs, ss)
        sch.op("dve", "scalar_tensor_tensor", dict(out=hn.ap, in0=h.ap, scalar=rs.ap, in1=gin.ap, op0=ALU.mult, op1=ALU.mult),
               reads=[h.b, rs.b, gin.b], writes=[hn.b])
        yield
        ptv = ptf.ap.bitcast(BF16)
        for kc2 in range(2):
            for q in range(4):
                kc = kc2 * 4 + q
                sch.op("pe", "transpose", dict(out=ptv[:, q * 128:(q + 1) * 128], in_=hn.ap[:, kc * 128:(kc + 1) * 128], identity=P.ident_bf.ap),
                       reads=[hn.b, P.ident_bf.b], writes=[ptf.b], inc=(q == 3))
            sch.op("act", "copy", dict(out=hnT.ap[:, kc2 * 512:(kc2 + 1) * 512], in_=ptv[:, 0:512]), reads=[ptf.b], writes=[hnT.b])
            yield
        cq = cos3[:, qt, :]
        sq = sin3[:, qt, :]
        for nb in range(5):
            pjb = pf[nb % 3]
            c0 = nb * 512
            c1 = min(NCOL, c0 + 512)
            for kc in range(8):
                sch.op("pe", "matmul", dict(out=pjb.ap[:, 0:c1 - c0], lhsT=hnT.ap[:, kc * 128:(kc + 1) * 128], rhs=wiv[:, kc, c0:c1],
                                            start=(kc == 0), stop=(kc == 7)),
                       reads=[hnT.b, w_in.b], writes=[pjb.b], inc=(kc == 7))
            yield
            if nb < 4:
                nh = 8 if nb < 3 else 6
                x = pjb.ap[:, 0:nh * 64].rearrange("p (h t i) -> p h t i", t=2, i=32)
                o = rp.ap[:, nb * 512:nb * 512 + nh * 64].rearrange("p (h t i) -> p h t i", t=2, i=32)
                cb = cq.unsqueeze(1).broadcast_to([128, nh, 32])
                sb_ = sq.unsqueeze(1).broadcast_to([128, nh, 32])
                tv = [t_.ap[:, 0:nh * 32].rearrange("p (h i) -> p h i", i=32) for t_ in tr]
                R_ = [pjb.b, cosT.b, sinT.b]
                sch.op("dve", "tensor_tensor", dict(out=tv[0], in0=x[:, :, 0, :], in1=cb, op=ALU.mult), reads=R_, writes=[tr[0].b])
                sch.op("dve", "tensor_tensor", dict(out=tv[1], in0=x[:, :, 1, :], in1=sb_, op=ALU.mult), reads=R_, writes=[tr[1].b])
                sch.op("dve", "tensor_tensor", dict(out=tv[2], in0=x[:, :, 1, :], in1=cb, op=ALU.mult), reads=R_, writes=[tr[2].b])
                sch.op("dve", "tensor_tensor", dict(out=tv[3], in0=x[:, :, 0, :], in1=sb_, op=ALU.mult), reads=R_, writes=[tr[3].b])
                sch.op("pool", "tensor_tensor", dict(out=o[:, :, 0, :], in0=tv[0], in1=tv[1], op=ALU.subtract),
                       reads=[tr[0].b, tr[1].b], writes=[rp.b])
                sch.op("pool", "tensor_tensor", dict(out=o[:, :, 1, :], in0=tv[2], in1=tv[3], op=ALU.add),
                       reads=[tr[2].b, tr[3].b], writes=[rp.b])
            if nb == 3:
                sch.op("act", "copy", dict(out=Vv[:, qt, 0:2, 0:64], in_=pjb.ap[:, 384:512].rearrange("p (n e) -> p n e", e=64)),
                       reads=[pjb.b], writes=[V_b[qt]])
            if nb == 4:
                sch.op("act", "copy", dict(out=Vv[:, qt, 2:4, 0:64], in_=pjb.ap[:, 0:128].rearrange("p (n e) -> p n e", e=64)),
                       reads=[pjb.b], writes=[V_b[qt]])
                sch.op("dve", "tensor_copy", dict(out=wsb.ap, in_=pjb.ap[:, 128:136]), reads=[pjb.b], writes=[wsb.b])
            yield
        sch.op("dve", "tensor_scalar", dict(out=sgnw.ap, in0=wsb.ap, scalar1=0.0, scalar2=None, op0=ALU.is_ge), reads=[wsb.b], writes=[sgnw.b])
        sch.op("dve", "tensor_scalar", dict(out=sgnw.ap, in0=sgnw.ap, scalar1=2.0, scalar2=-1.0, op0=ALU.mult, op1=ALU.add), reads=[sgnw.b], writes=[sgnw.b])
        sch.op("dve", "tensor_tensor", dict(out=absw.ap, in0=wsb.ap, in1=sgnw.ap, op=ALU.mult), reads=[wsb.b, sgnw.b], writes=[absw.b])
        qiv = rp.ap[:, QIO:QIO + 512].rearrange("p (h e) -> p h e", e=64)
        sch.op("dve", "tensor_tensor", dict(out=qiv, in0=qiv, in1=absw.ap.unsqueeze(2).broadcast_to([128, 8, 64]), op=ALU.mult),
               reads=[rp.b, absw.b], writes=[rp.b])
        yield

        def tr4(cols):
            for q, co in enumerate(cols):
                sch.op("pe", "transpose", dict(out=ptv[:, q * 128:(q + 1) * 128], in_=rp.ap[:, co:co + 128], identity=P.ident_bf.ap),
                       reads=[rp.b, P.ident_bf.b], writes=[ptf.b], inc=(q == len(cols) - 1))
        for i0 in (0, 4):
            tr4([QO + (i0 + b) * 128 for b in range(4)])
            sch.op("act", "copy", dict(out=qTq.ap[:, i0 * 128:(i0 + 4) * 128], in_=ptv[:, 0:512]), reads=[ptf.b], writes=[qTq.b])
            yield
        tr4([QIO + b * 128 for b in range(4)])
        sch.op("act", "copy", dict(out=qiT.ap[:, 0:512], in_=ptv[:, 0:512]), reads=[ptf.b], writes=[qiT.b])
        yield
        tr4([KO, KO + 128, KIO])
        sch.op("dve", "tensor_copy", dict(out=kTv[:, :, r0:r0 + 128], in_=ptv[:, 0:256].rearrange("p (b s) -> p b s", s=128)),
               reads=[ptf.b], writes=[kT_b[qt]])
        sch.op("dve", "tensor_copy", dict(out=kiT.ap[:, r0:r0 + 128], in_=ptv[:, 256:384]), reads=[ptf.b], writes=[kiT_b[qt]])
        yield
        nch = (L + 511) // 512
        ci = 0
        for c in range(nch):
            s0 = c * 512
            wdt = min(512, L - s0)
            kb_ = kiT_b[4 * c:min(NT, 4 * c + 4)]
            for hh in range(8):
                pb = pf[ci % 3]
                ci += 1
                half = (hh % 2) * 64
                sch.op("pe", "matmul", dict(out=pb.ap[:, 0:wdt], lhsT=qiT.ap[half:half + 64, (hh // 2) * 128:(hh // 2 + 1) * 128],
                                            rhs=kiT.ap[half:half + 64, s0:s0 + wdt], start=True, stop=True),
                       reads=[qiT.b] + kb_, writes=[pb.b])
                r = rr[hh % 2]
                sch.op("act", "activation", dict(out=r.ap[:, 0:wdt], in_=pb.ap[:, 0:wdt], func=AF.Relu), reads=[pb.b], writes=[r.b])
                if hh == 0:
                    sch.op("dve", "tensor_scalar", dict(out=acc.ap[:, s0:s0 + wdt], in0=r.ap[:, 0:wdt], scalar1=sgnw.ap[:, 0:1], scalar2=None, op0=ALU.mult),
                           reads=[r.b, sgnw.b], writes=[acc.b])
                else:
                    sch.op("dve", "scalar_tensor_tensor", dict(out=acc.ap[:, s0:s0 + wdt], in0=r.ap[:, 0:wdt], scalar=sgnw.ap[:, hh:hh + 1],
                                                               in1=acc.ap[:, s0:s0 + wdt], op0=ALU.mult, op1=ALU.add),
                           reads=[r.b, sgnw.b, acc.b], writes=[acc.b])
                if hh % 2 == 1:
                    yield
        if L > KSEL:
            sch.op("dve", "tensor_reduce", dict(out=lo.ap, in_=acc.ap[:, 0:L], axis=AX.X, op=ALU.min), reads=[acc.b], writes=[lo.b])
        sch.op("dve", "tensor_tensor", dict(out=acc.ap[:, r0:L], in0=acc.ap[:, r0:L], in1=negm.ap, op=ALU.add), reads=[acc.b, negm.b], writes=[acc.b])
        if L > KSEL:
            sch.op("dve", "tensor_reduce", dict(out=hi.ap, in_=acc.ap[:, 0:L], axis=AX.X, op=ALU.max), reads=[acc.b], writes=[hi.b])
            bb = [lo.b, hi.b, mid.b, cnt.b, ge.b, dd.b, wt.b]
            sch.op("dve", "tensor_tensor", dict(out=dd.ap, in0=hi.ap, in1=lo.ap, op=ALU.subtract), reads=bb, writes=[dd.b])
            sch.op("dve", "tensor_scalar", dict(out=wt.ap, in0=pow2.ap, scalar1=dd.ap, scalar2=None, op0=ALU.mult), reads=bb + [pow2.b], writes=[wt.b])
            sch.op("dve", "tensor_tensor", dict(out=mid.ap, in0=lo.ap, in1=wt.ap[:, 1:2], op=ALU.add), reads=bb, writes=[mid.b])
            yield
            LA = (int(L * DVE_SHARE) // 64) * 64
            nB = L - LA
            for it in range(NBIS):
                sch.op("act", "activation", dict(out=junkA.ap[:, 0:nB], in_=acc.ap[:, LA:L], func=AF.Sign, bias=mid.ap, scale=-1.0,
                                                 accum_out=cntB.ap), reads=[acc.b, mid.b], writes=[junkA.b, cntB.b])
                sch.op("dve", "tensor_scalar", dict(out=mask.ap[:, 0:LA], in0=acc.ap[:, 0:LA], scalar1=mid.ap, scalar2=None, op0=ALU.is_ge, op1=ALU.add,
                                                    accum_out=cnt.ap), reads=[acc.b, mid.b], writes=[mask.b, cnt.b])
                sch.op("dve", "scalar_tensor_tensor", dict(out=ge.ap, in0=cntB.ap, scalar=-0.5, in1=cnt.ap, op0=ALU.mult, op1=ALU.add),
                       reads=[cntB.b, cnt.b], writes=[ge.b])
                sch.op("dve", "scalar_tensor_tensor", dict(out=ge.ap, in0=ge.ap, scalar=float(KSEL) - 0.5 - nB / 2.0, in1=wt.ap[:, it + 1:it + 2],
                                                           op0=ALU.is_ge, op1=ALU.mult), reads=[ge.b, wt.b], writes=[ge.b])
                sch.op("dve", "scalar_tensor_tensor", dict(out=mid.ap, in0=ge.ap, scalar=wt.ap[:, it + 2:it + 3], in1=mid.ap,
                                                           op0=ALU.subtract, op1=ALU.add), reads=[ge.b, wt.b, mid.b], writes=[mid.b])
                yield
            sch.op("dve", "tensor_tensor", dict(out=lo.ap, in0=mid.ap, in1=wt.ap[:, NBIS + 1:NBIS + 2], op=ALU.subtract), reads=bb, writes=[lo.b])
            sch.op("dve", "tensor_scalar", dict(out=mask.ap[:, 0:L], in0=acc.ap[:, 0:L], scalar1=lo.ap, scalar2=None, op0=ALU.is_ge),
                   reads=[acc.b, lo.b], writes=[mask.b])
        else:
            sch.op("dve", "tensor_scalar", dict(out=mask.ap[:, 0:L], in0=acc.ap[:, 0:L], scalar1=-1.0e29, scalar2=None, op0=ALU.is_ge),
                   reads=[acc.b], writes=[mask.b])
        yield
        for i0 in range(0, qt + 1, 4):
            n = min(4, qt + 1 - i0)
            for q in range(n):
                st = i0 + q
                sch.op("pe", "transpose", dict(out=ptv[:, q * 128:(q + 1) * 128], in_=mask.ap[:, st * 128:(st + 1) * 128], identity=P.ident_bf.ap),
                       reads=[mask.b, P.ident_bf.b], writes=[ptf.b], inc=(q == n - 1))
            sch.op("act", "activation", dict(out=mT.ap[:, i0 * 128:(i0 + n) * 128], in_=ptv[:, 0:n * 128], func=AF.Identity,
                                             scale=MASK_BIG, bias=negbig.ap), reads=[ptf.b, negbig.b], writes=[mT.b])
            yield

    def back(qt):
        r0 = qt * 128
        h = hq[qt % 2]
        qTq = qT[qt % 2]
        mTv = maskT[qt % 2].ap.rearrange("p (t q) -> p t q", q=128)
        mTb = maskT[qt % 2].b
        cnt_p = 0
        for n in range(4):
            half = (n % 2) * 64
            a = n // 2
            pob = ppv[n % 2]
            for st in range(qt + 1):
                pq = pqk[cnt_p % 2]
                p_ = pT[cnt_p % 4]
                sch.op("pe", "matmul", dict(out=pq.ap[:, 0:512], lhsT=kTv[half:half + 64, a, st * 128:(st + 1) * 128],
                                            rhs=qTq.ap[half:half + 64, a * 512:(a + 1) * 512], start=True, stop=False),
                       reads=[kT_b[st], qTq.b], writes=[pq.b], inc=False)
                sch.op("pe", "matmul", dict(out=pq.ap[:, 0:512], lhsT=P.ident_bf.ap,
                                            rhs=mTv[:, st, :].unsqueeze(1).broadcast_to([128, 4, 128]), start=False, stop=True),
                       reads=[mTb, P.ident_bf.b], writes=[pq.b])
                sch.op("act", "activation", dict(out=p_.ap, in_=pq.ap[:, 0:512], func=AF.Exp, scale=0.125), reads=[pq.b], writes=[p_.b])
                for g in range(4):
                    sch.op("pe", "matmul", dict(out=pob.ap[:, g * 65:(g + 1) * 65], lhsT=p_.ap[:, g * 128:(g + 1) * 128], rhs=Vv[:, st, n, :],
                                                start=(st == 0 and g == 0), stop=(st == qt and g == 3)),
                           reads=[p_.b, V_b[st]], writes=[pob.b], inc=(g == 3))
                cnt_p += 1
                yield
            po3 = pob.ap[:, 0:260].rearrange("p (g e) -> p g e", e=65)
            sch.op("dve", "reciprocal", dict(out=rden.ap, in_=po3[:, :, 64]), reads=[pob.b], writes=[rden.b])
            ov = osb.ap[:, a * 512:(a + 1) * 512].rearrange("p (g two e) -> p g two e", two=2, e=64)[:, :, n % 2, :]
            sch.op("dve", "tensor_tensor", dict(out=ov, in0=po3[:, :, 0:64], in1=rden.ap.unsqueeze(2).broadcast_to([128, 4, 64]), op=ALU.mult),
                   reads=[pob.b, rden.b], writes=[osb.b])
        yield
        for kc2 in range(2):
            ptb = ppv[kc2]
            ptv = ptb.ap.bitcast(BF16)
            for q in range(4):
                kc = kc2 * 4 + q
                sch.op("pe", "transpose", dict(out=ptv[:, q * 128:(q + 1) * 128], in_=osb.ap[:, kc * 128:(kc + 1) * 128], identity=P.ident_bf.ap),
                       reads=[osb.b, P.ident_bf.b], writes=[ptb.b], inc=(q == 3))
            sch.op("act", "copy", dict(out=oT.ap[:, kc2 * 512:(kc2 + 1) * 512], in_=ptv[:, 0:512]), reads=[ptb.b], writes=[oT.b])
        yield
        for half in range(2):
            pob = pout[half]
            for kc in range(8):
                sch.op("pe", "matmul", dict(out=pob.ap[:, 0:512], lhsT=oT.ap[:, kc * 128:(kc + 1) * 128],
                                            rhs=w_out.ap[:, kc * D + half * 512:kc * D + (half + 1) * 512], start=(kc == 0), stop=(kc == 7)),
                       reads=[oT.b, w_out.b], writes=[pob.b], inc=(kc == 7))
            sch.op("act", "activation", dict(out=junk2.ap[:, 0:512], in_=pob.ap[:, 0:512], func=AF.Square, accum_out=rs2.ap[:, half:half + 1]),
                   reads=[pob.b], writes=[junk2.b, rs2.b])
            yield
        sch.op("dve", "tensor_tensor", dict(out=rs2.ap[:, 0:1], in0=rs2.ap[:, 0:1], in1=rs2.ap[:, 1:2], op=ALU.add), reads=[rs2.b], writes=[rs2.b])
        r1 = T(rs2.ap[:, 0:1], rs2.b)
        rstd_ln(r1, r1)
        for half in range(2):
            sch.op("dve", "scalar_tensor_tensor", dict(out=yt.ap[:, half * 512:(half + 1) * 512], in0=pout[half].ap[:, 0:512], scalar=rs2.ap[:, 0:1],
                                                       in1=gout.ap[:, half * 512:(half + 1) * 512], op0=ALU.mult, op1=ALU.mult),
                   reads=[pout[half].b, rs2.b, gout.b], writes=[yt.b])
        sch.op("pool", "tensor_tensor", dict(out=yt.ap, in0=yt.ap, in1=h.ap, op=ALU.add), reads=[yt.b, h.b], writes=[yt.b])
        sch.dma("sp", dst[r0:r0 + 128, :], yt.ap, reads=[yt.b])
        yield

    def count_units(gen_fn, qt):
        return None

    def n_front(qt):
        L = (qt + 1) * 128
        nch = (L + 511) // 512
        return 1 + 2 + 10 + 1 + 4 + 4 * nch + (2 + NBIS if L > KSEL else 1) + (qt + 4) // 4
    def n_back(qt):
        return 4 * (qt + 1) + 1 + 1 + 2 + 1

    def run_all(g):
        for _ in g:
            pass

    run_all(front(0))
    for qt in range(NT):
        gb = back(qt)
        if qt + 1 < NT:
            gf = front(qt + 1)
            nb_, nf_ = n_back(qt), n_front(qt + 1)
            ib = i_f = 0
            bdone = fdone = False
            while not (bdone and fdone):
                if not bdone and (fdone or NO_INTERLEAVE or ib * nf_ * FRONT_PRIO <= i_f * nb_):
                    try:
                        next(gb); ib += 1
                    except StopIteration:
                        bdone = True
                elif not fdone:
                    try:
                        next(gf); i_f += 1
                    except StopIteration:
                        fdone = True
        else:
            run_all(gb)
    P.phase_end()


SEQ = 4096
DEPTH = 4
S5_NI = 4


def build_program(S=SEQ, depth=DEPTH):
    P = Prog(S)
    x = P.din("x", [S, D])
    out = P.dout("out", [S, D])
    hbuf = P.dscratch("hbuf", [S, D])
    g = P.din("g", [depth * 4, D])
    s5cst = P.din("s5cst", [128, 130])
    negm = P.din("negm", [128, 128])
    for i in range(depth):
        j = i // 2
        src = x if i == 0 else hbuf
        grow = lambda k: g[i * 4 + k:i * 4 + k + 1, :]
        if i % 2 == 0:
            s5_phase(P, src, hbuf, P.din("s5small_%d" % j, [128, 128]), P.din("s5big_%d" % j, [128, 2048]), s5cst,
                     P.din("s5win_%d" % j, [D, E]), P.din("s5wglu_%d" % j, [E, E]), P.din("s5wout_%d" % j, [E, D]),
                     grow(0), grow(1), NI=S5_NI)
        else:
            dsa_phase(P, src, hbuf, P.din("awin_%d" % j, [D, NCOL]), P.din("awout_%d" % j, [D, D]), negm, grow(0), grow(1))
        dst = out if i == depth - 1 else hbuf
        mlp_phase(P, hbuf, dst, P.din("w1_%d" % i, [D, DFF]), P.din("w2_%d" % i, [DFF, D]), grow(2), grow(3))
    P.sch.barrier()
    P.sch.emit()
    return P


_CACHE = {}


def host_inputs(inputs, depth=DEPTH):
    f = lambda a: np.ascontiguousarray(np.asarray(a), dtype=np.float32)
    shared = {"ident": np.eye(128, dtype=np.float32), "s5cst": s5_consts(), "negm": dsa_consts(),
              "g": f(inputs["norm_g"]).reshape(-1, D)[:depth * 4]}
    for i in range(depth):
        j = i // 2
        shared["w1_%d" % i] = f(inputs["mlp_w1"][i])
        shared["w2_%d" % i] = f(inputs["mlp_w2"][i])
        if i % 2 == 0:
            small, big = s5_host_layout(*[np.asarray(inputs["ssm_" + n][j]) for n in
                                          ["lam_re", "lam_im", "log_dt", "b_re", "b_im", "c_re", "c_im", "d"]])
            shared["s5small_%d" % j] = small
            shared["s5big_%d" % j] = big
            shared["s5win_%d" % j] = f(inputs["ssm_w_in"][j])
            shared["s5wglu_%d" % j] = f(inputs["ssm_w_glu"][j])
            shared["s5wout_%d" % j] = f(inputs["ssm_w_out"][j])
        else:
            wip, wop = dsa_host_layout(np.asarray(inputs["att_w_in"][j]), np.asarray(inputs["att_w_out"][j]))
            shared["awin_%d" % j] = wip
            shared["awout_%d" % j] = wop
    return shared


def kernel(**inputs):
    x = np.asarray(inputs["x"], dtype=np.float32)
    B, S, _ = x.shape
    key = (S, DEPTH)
    if key not in _CACHE:
        _CACHE[key] = build_program(S, DEPTH)
    P = _CACHE[key]
    shared = host_inputs(inputs)
    in_maps = []
    for b in range(B):
        m = dict(shared)
        m["x"] = np.ascontiguousarray(x[b])
        in_maps.append(m)
    res = run_bass_kernel_spmd(P.nc, in_maps, core_ids=list(range(B)))
    return np.stack([np.asarray(r["out"]) for r in res.results], 0).astype(np.float32)
```
